# Optimizing a Trainium2 kernel written in Bass

```python
import math
import jax, jax.numpy as jnp
from jax import lax
import numpy as np

D_MODEL = 1024
BATCH = 32
SEQ = 2048
DEPTH = 1

CTX_LEN = 256
GRID_W = 64
S5_WIDTH = D_MODEL // 4
S5_CH_PER_GROUP = 16
S5_GROUPS = S5_WIDTH // S5_CH_PER_GROUP
S5_STATE = 64
CONV_HEAD_DIM = 64
CONV_WIDTH = D_MODEL - S5_WIDTH
CONV_HEADS = CONV_WIDTH // CONV_HEAD_DIM
CONV_ROW_WIDTH = CONV_WIDTH // 2
CONV_TAPS = 3
D_MIX = S5_WIDTH + CONV_WIDTH
D_IN_PROJ = S5_WIDTH + 3 * CONV_WIDTH
N_EXPERT_GROUPS = 4
EXPERTS_PER_GROUP = 4
N_EXPERTS = N_EXPERT_GROUPS * EXPERTS_PER_GROUP
TOP_K_IN_GROUP = 2
D_EXPERT = D_MODEL // 2
N_MOD = 6
RMS_EPS = 1e-6
DT_MIN = 1e-3
DT_MAX = 1e-1

kernel_name = 'hybrid_s5_shortconv_hmoe_prefix_dit'


def rmsnorm(x, g):
    xf = x.astype(jnp.float32)
    y = xf * lax.rsqrt(jnp.mean(xf * xf, axis=-1, keepdims=True) + RMS_EPS)
    return (y * g.astype(jnp.float32)).astype(x.dtype)


def ada_mod(cond, w_mod, b_mod):
    m = jax.nn.silu(cond) @ w_mod + b_mod
    return jnp.split(m[:, None, :], N_MOD, axis=-1)


def modulate(x, g, shift, scale):
    return rmsnorm(x, g) * (1.0 + scale) + shift


def s5_discretize(lam_re, lam_im, log_dt, b_re, b_im):
    lam = lax.complex(lam_re.astype(jnp.float32), lam_im.astype(jnp.float32))
    dt = jnp.exp(log_dt.astype(jnp.float32))[:, None]
    a_bar = jnp.exp(lam * dt)
    b = lax.complex(b_re.astype(jnp.float32), b_im.astype(jnp.float32))
    b_bar = ((a_bar - 1.0) / lam)[..., None] * b
    return a_bar, b_bar


def _linear_recurrence(e1, e2):
    a1, b1 = e1
    a2, b2 = e2
    return a1 * a2, a2 * b1 + b2


def s5_states(u, a_bar, b_bar, h0, reverse):
    bu = jnp.einsum('blgh,gph->blgp', u.astype(jnp.complex64), b_bar)
    if h0 is not None:
        first = -1 if reverse else 0
        bu = bu.at[:, first].add(a_bar * h0)
    a = jnp.broadcast_to(a_bar, (1, bu.shape[1]) + a_bar.shape)
    _, states = lax.associative_scan(_linear_recurrence, (a, bu), reverse=reverse, axis=1)
    return states


def s5_readout(u, st_f, st_b, c_f, c_b, d, w_glu, b_glu, dtype):
    bsz, n = u.shape[0], u.shape[1]
    y = (jnp.einsum('blgp,ghp->blgh', st_f, c_f).real
         + jnp.einsum('blgp,ghp->blgh', st_b, c_b).real
         + d.astype(jnp.float32).reshape(S5_GROUPS, S5_CH_PER_GROUP) * u)
    g = jax.nn.gelu(y.reshape(bsz, n, S5_WIDTH).astype(dtype))
    return g * jax.nn.sigmoid(g @ w_glu + b_glu)


def dwconv3(x, w, axis):
    n = x.shape[axis]
    pad = [(0, 0)] * x.ndim
    pad[axis] = (1, 1)
    xp = jnp.pad(x, pad)
    return (lax.slice_in_dim(xp, 0, n, axis=axis) * w[0]
            + lax.slice_in_dim(xp, 1, n + 1, axis=axis) * w[1]
            + lax.slice_in_dim(xp, 2, n + 2, axis=axis) * w[2])


def short_conv_latent(z, conv_w):
    b_g, c_g, v = jnp.split(z, 3, axis=-1)
    cv = c_g * v
    bsz, n, _ = cv.shape
    rows = n // GRID_W
    grid = cv.reshape(bsz, rows, GRID_W, CONV_WIDTH)
    row_part = dwconv3(grid[..., :CONV_ROW_WIDTH], conv_w[:, :CONV_ROW_WIDTH], axis=2)
    col_part = dwconv3(grid[..., CONV_ROW_WIDTH:], conv_w[:, CONV_ROW_WIDTH:], axis=1)
    conv = jnp.concatenate([row_part, col_part], axis=-1).reshape(bsz, n, CONV_WIDTH)
    return b_g * conv


def short_conv_context(z, conv_w):
    b_g, c_g, v = jnp.split(z, 3, axis=-1)
    return b_g * dwconv3(c_g * v, conv_w, axis=1)


def hier_moe(h, wg, bg, we, be, w1, w3, w2):
    shp = h.shape
    t = h.reshape(-1, shp[-1])
    g_prob = jax.nn.softmax((t @ wg + bg).astype(jnp.float32), axis=-1)
    g_p, g_idx = lax.top_k(g_prob, 1)
    e_logits = (t @ we + be).astype(jnp.float32).reshape(-1, N_EXPERT_GROUPS, EXPERTS_PER_GROUP)
    e_in = jnp.take_along_axis(e_logits, g_idx[:, :, None], axis=1)[:, 0]
    top_v, top_i = lax.top_k(e_in, TOP_K_IN_GROUP)
    weights = jax.nn.softmax(top_v, axis=-1) * g_p
    e_idx = g_idx * EXPERTS_PER_GROUP + top_i
    gates = jnp.einsum('tk,tke->te', weights,
                       jax.nn.one_hot(e_idx, N_EXPERTS, dtype=jnp.float32)).astype(h.dtype)
    out = jnp.zeros_like(t)
    for e in range(N_EXPERTS):
        he = jax.nn.silu(t @ w1[e]) * (t @ w3[e])
        out = out + gates[:, e:e + 1] * (he @ w2[e])
    return out.reshape(shp)


def setup_inputs(seed: int = 0) -> dict:
    key = jax.random.key(seed)
    ks = jax.random.split(key, 32)
    f32 = jnp.float32
    nrm = lambda k, shape, s: jax.random.normal(k, shape, f32) * s
    G, P, H = S5_GROUPS, S5_STATE, S5_CH_PER_GROUP
    lam_im = jnp.broadcast_to(jnp.pi * jnp.arange(P, dtype=f32), (DEPTH, 2, G, P))
    return {
        'x': nrm(ks[0], (BATCH, SEQ, D_MODEL), 1.0),
        'c': nrm(ks[1], (BATCH, D_MODEL), 1.0),
        'ctx': nrm(ks[2], (BATCH, CTX_LEN, D_MODEL), 1.0),
        'c_ctx': nrm(ks[3], (D_MODEL,), 1.0),
        'w_mod': nrm(ks[4], (DEPTH, D_MODEL, N_MOD * D_MODEL), 0.5 * D_MODEL ** -0.5),
        'b_mod': nrm(ks[5], (DEPTH, N_MOD * D_MODEL), 0.02),
        'norm1_g': 1.0 + nrm(ks[6], (DEPTH, D_MODEL), 0.05),
        'norm2_g': 1.0 + nrm(ks[7], (DEPTH, D_MODEL), 0.05),
        'w_in': nrm(ks[8], (DEPTH, D_MODEL, D_IN_PROJ), D_MODEL ** -0.5),
        's5_lambda_re': -0.5 + nrm(ks[9], (DEPTH, 2, G, P), 0.01),
        's5_lambda_im': lam_im + nrm(ks[10], (DEPTH, 2, G, P), 0.01),
        's5_log_dt': jax.random.uniform(ks[11], (DEPTH, 2, G), f32, math.log(DT_MIN), math.log(DT_MAX)),
        's5_b_re': nrm(ks[12], (DEPTH, 2, G, P, H), (2 * H) ** -0.5),
        's5_b_im': nrm(ks[13], (DEPTH, 2, G, P, H), (2 * H) ** -0.5),
        's5_c_re': nrm(ks[14], (DEPTH, 2, G, H, P), P ** -0.5),
        's5_c_im': nrm(ks[15], (DEPTH, 2, G, H, P), P ** -0.5),
        's5_d': nrm(ks[16], (DEPTH, S5_WIDTH), 1.0),
        'w_glu': nrm(ks[17], (DEPTH, S5_WIDTH, S5_WIDTH), S5_WIDTH ** -0.5),
        'b_glu': nrm(ks[18], (DEPTH, S5_WIDTH), 0.02),
        'conv_w': nrm(ks[19], (DEPTH, CONV_TAPS, CONV_WIDTH), CONV_TAPS ** -0.5),
        'w_out': nrm(ks[20], (DEPTH, D_MIX, D_MODEL), D_MIX ** -0.5),
        'router_group_w': nrm(ks[21], (DEPTH, D_MODEL, N_EXPERT_GROUPS), D_MODEL ** -0.5),
        'router_group_b': nrm(ks[22], (DEPTH, N_EXPERT_GROUPS), 0.01),
        'router_expert_w': nrm(ks[23], (DEPTH, D_MODEL, N_EXPERTS), D_MODEL ** -0.5),
        'router_expert_b': nrm(ks[24], (DEPTH, N_EXPERTS), 0.01),
        'expert_w1': nrm(ks[25], (DEPTH, N_EXPERTS, D_MODEL, D_EXPERT), D_MODEL ** -0.5),
        'expert_w3': nrm(ks[26], (DEPTH, N_EXPERTS, D_MODEL, D_EXPERT), D_MODEL ** -0.5),
        'expert_w2': nrm(ks[27], (DEPTH, N_EXPERTS, D_EXPERT, D_MODEL), D_EXPERT ** -0.5),
        'final_g': 1.0 + nrm(ks[28], (D_MODEL,), 0.05),
    }


def reference(x, c, ctx, c_ctx, w_mod, b_mod, norm1_g, norm2_g, w_in, s5_lambda_re, s5_lambda_im,
              s5_log_dt, s5_b_re, s5_b_im, s5_c_re, s5_c_im, s5_d, w_glu, b_glu, conv_w, w_out,
              router_group_w, router_group_b, router_expert_w, router_expert_b,
              expert_w1, expert_w3, expert_w2, final_g):
    f32 = jnp.float32
    bsz, n_tok, _ = x.shape
    n_ctx = ctx.shape[1]
    for l in range(DEPTH):
        last = l == DEPTH - 1
        mx = ada_mod(c, w_mod[l], b_mod[l])
        mc = ada_mod(c_ctx[None, :], w_mod[l], b_mod[l])
        a_f, bb_f = s5_discretize(s5_lambda_re[l, 0], s5_lambda_im[l, 0], s5_log_dt[l, 0],
                                  s5_b_re[l, 0], s5_b_im[l, 0])
        a_b, bb_b = s5_discretize(s5_lambda_re[l, 1], s5_lambda_im[l, 1], s5_log_dt[l, 1],
                                  s5_b_re[l, 1], s5_b_im[l, 1])
        cm_f = lax.complex(s5_c_re[l, 0].astype(f32), s5_c_im[l, 0].astype(f32))
        cm_b = lax.complex(s5_c_re[l, 1].astype(f32), s5_c_im[l, 1].astype(f32))

        hc = modulate(ctx, norm1_g[l], mc[0], mc[1])
        uc = (hc @ w_in[l][:, :S5_WIDTH]).astype(f32).reshape(bsz, n_ctx, S5_GROUPS, S5_CH_PER_GROUP)
        sc_f = s5_states(uc, a_f, bb_f, None, False)
        sc_b = s5_states(uc, a_b, bb_b, None, True)

        hx = modulate(x, norm1_g[l], mx[0], mx[1])
        zx = hx @ w_in[l]
        ux = zx[..., :S5_WIDTH].astype(f32).reshape(bsz, n_tok, S5_GROUPS, S5_CH_PER_GROUP)
        sx_f = s5_states(ux, a_f, bb_f, sc_f[:, -1], False)
        sx_b = s5_states(ux, a_b, bb_b, sc_b[:, 0], True)
        y_s5 = s5_readout(ux, sx_f, sx_b, cm_f, cm_b, s5_d[l], w_glu[l], b_glu[l], x.dtype)
        y_conv = short_conv_latent(zx[..., S5_WIDTH:], conv_w[l])
        yx = jnp.concatenate([y_s5, y_conv], axis=-1) @ w_out[l]
        x = x + mx[2] * yx
        x = x + mx[5] * hier_moe(modulate(x, norm2_g[l], mx[3], mx[4]), router_group_w[l],
                                 router_group_b[l], router_expert_w[l], router_expert_b[l],
                                 expert_w1[l], expert_w3[l], expert_w2[l])

        if not last:
            yc_s5 = s5_readout(uc, sc_f, sc_b, cm_f, cm_b, s5_d[l], w_glu[l], b_glu[l], ctx.dtype)
            yc_conv = short_conv_context(hc @ w_in[l][:, S5_WIDTH:], conv_w[l])
            ctx = ctx + mc[2] * (jnp.concatenate([yc_s5, yc_conv], axis=-1) @ w_out[l])
            ctx = ctx + mc[5] * hier_moe(modulate(ctx, norm2_g[l], mc[3], mc[4]), router_group_w[l],
                                         router_group_b[l], router_expert_w[l], router_expert_b[l],
                                         expert_w1[l], expert_w3[l], expert_w2[l])
    return rmsnorm(x, final_g)
```

```python
import contextlib
import math
import numpy as np
import concourse.bass as bass
import concourse.mybir as mybir
from concourse.bass_utils import run_bass_kernel_spmd

F32 = mybir.dt.float32
BF16 = mybir.dt.bfloat16
AF = mybir.ActivationFunctionType
ALU = mybir.AluOpType
AX = mybir.AxisListType
ENGS = ("pe", "act", "dve", "pool", "sp")
NB = 4
SEQ = 2048
NCTX = 256
D = 1024
LTOT = SEQ + NCTX
NLEV = 9
NBLK = 288
NX = 320
TAIL = 47104
TSZ = 512
NST = 19
NSLOT = NST * TSZ
ROWW = 1040
I32 = mybir.dt.int32
ARB_N = 65536
ARF_N = 9472


class Buf:
    __slots__ = ("name", "writers", "readers", "sem", "dma_total", "_par", "_epoch")

    def __init__(self, name):
        self.name = name
        self.writers = []
        self.readers = []
        self.sem = None
        self.dma_total = 0
        self._par = False
        self._epoch = []


class Op:
    __slots__ = ("eng", "fn", "deps", "needs_inc", "count", "dma_buf", "dma_count")

    def __init__(self, eng, fn):
        self.eng = eng
        self.fn = fn
        self.deps = []
        self.needs_inc = False
        self.count = None
        self.dma_buf = None
        self.dma_count = None


class Sched:
    def __init__(self, nc, stack):
        self.nc = nc
        self.stack = stack
        self.ops = {e: [] for e in ENGS}
        self.bufs = []
        self.final_waits = []

    def buf(self, name):
        b = Buf(name)
        self.bufs.append(b)
        return b

    def _add(self, eng, fn, reads, writes, dma_buf=None, par=False):
        op = Op(eng, fn)
        seen = set()
        for b in reads:
            for d in b.writers:
                if id(d) not in seen:
                    seen.add(id(d)); op.deps.append(d)
        for b in writes:
            if par:
                if b.readers or not getattr(b, "_par", False):
                    b._epoch = b.writers + b.readers
                    b.writers = []
                    b.readers = []
                    b._par = True
                dl = b._epoch
            else:
                dl = b.writers + b.readers
                b._par = False
            for d in dl:
                if id(d) not in seen:
                    seen.add(id(d)); op.deps.append(d)
        for b in reads:
            b.readers.append(op)
        for b in writes:
            if par:
                b.writers.append(op)
            else:
                b.writers = [op]
                b.readers = []
        if dma_buf is not None:
            op.dma_buf = dma_buf
            if dma_buf.sem is None:
                dma_buf.sem = self.stack.enter_context(self.nc.semaphore("s_" + dma_buf.name))
            dma_buf.dma_total += 16
            op.dma_count = dma_buf.dma_total
        self.ops[eng].append(op)
        return op

    def op(self, eng, fn, reads=(), writes=()):
        return self._add(eng, fn, list(reads), list(writes))

    def dma(self, eng, fn, reads, write, par=False):
        return self._add(eng, fn, list(reads), [write], dma_buf=write, par=par)

    def barrier(self):
        nc = self.nc
        pend = []
        seen = set()
        for b in self.bufs:
            if b.name.endswith("_d") or "_d" in b.name and b.name.split("_d")[-1].isdigit() or b.name in ("wb_scr", "h2s_zero"):
                continue
            for d in b.writers + b.readers:
                if id(d) not in seen:
                    seen.add(id(d)); pend.append(d)
        engobj = {"pe": nc.tensor, "act": nc.scalar, "dve": nc.vector, "pool": nc.gpsimd, "sp": nc.sync}
        for e in ENGS:
            op = Op(e, (lambda e=e: engobj[e].nop()))
            op.deps = list(pend)
            self.ops[e].append(op)

    def emit(self):
        nc = self.nc
        for e in ENGS:
            for op in self.ops[e]:
                for d in op.deps:
                    if d.dma_buf is None and not (d.eng == "pe" and op.eng == "pe" and op.dma_buf is None):
                        d.needs_inc = True
        sems = {e: self.stack.enter_context(nc.semaphore("eng_" + e)) for e in ENGS}
        for e in ENGS:
            c = 0
            for op in self.ops[e]:
                if op.dma_buf is None and op.needs_inc:
                    c += 1
                    op.count = c
        final_waits = self.final_waits

        def run(e, eng):
            waited = {}
            for op in self.ops[e]:
                need = {}
                for d in op.deps:
                    if d.dma_buf is not None:
                        key = ("d", id(d.dma_buf)); v = d.dma_count; sem = d.dma_buf.sem
                    else:
                        if d.eng == "pe" and e == "pe" and op.dma_buf is None:
                            continue
                        key = ("e", d.eng); v = d.count; sem = sems[d.eng]
                    if key not in need or need[key][1] < v:
                        need[key] = (sem, v)
                for key, (sem, v) in need.items():
                    if waited.get(key, 0) >= v:
                        continue
                    waited[key] = v
                    eng.wait_ge(sem, v)
                ins = op.fn()
                if op.dma_buf is not None:
                    ins.then_inc(op.dma_buf.sem, 16)
                elif op.needs_inc:
                    ins.then_inc(sems[e], 1)
            if e == "sp":
                for b in final_waits:
                    eng.wait_ge(b.sem, b.dma_total)

        with nc.Block() as block:
            @block.tensor
            def _(eng):
                run("pe", eng)

            @block.scalar
            def _(eng):
                run("act", eng)

            @block.vector
            def _(eng):
                run("dve", eng)

            @block.gpsimd
            def _(eng):
                run("pool", eng)

            @block.sync
            def _(eng):
                run("sp", eng)


def make_consts():
    c = {}
    c["ident"] = np.eye(128, dtype=np.float32)
    isw = np.zeros((128, 128), np.float32)
    for p in range(128):
        isw[p, (p + 64) % 128] = 1.0
    c["iswap"] = isw
    c["ones"] = np.ones((128, 128), np.float32)
    sg = np.ones((128, 1), np.float32); sg[64:] = -1.0
    c["sgn"] = sg
    rm = np.zeros((128, 8), np.float32)
    for p in range(128):
        rm[p, p // 16] = 1.0
    c["rowmask"] = rm
    s8i = np.arange(128)[:, None] // 16
    t8i = np.arange(128)[None, :] // 16
    c["maskf"] = (s8i <= t8i).astype(np.float32)
    c["maskb"] = (s8i >= t8i).astype(np.float32)
    c["eps"] = np.full((128, 1), 1e-6, np.float32)
    c["lst"] = (np.arange(128)[:, None] < np.arange(128)[None, :]).astype(np.float32)
    c["thr"] = np.tile((np.arange(NST, dtype=np.float32) * float(TSZ))[None, :], (128, 1))
    c["iotap"] = np.arange(128, dtype=np.float32)[:, None]
    order = ["ident", "iswap", "ones", "sgn", "rowmask", "maskf", "maskb", "eps", "lst", "thr", "iotap"]
    offs = {}
    o = 0
    for k in order:
        offs[k] = (o, c[k].shape[1]); o += c[k].shape[1]
    return np.concatenate([c[k] for k in order], axis=1), offs


CONSTS, COFF = make_consts()


def make_gsel():
    g = np.zeros((128, 8, 240), np.float32)
    for g8 in range(8):
        for h in range(16):
            g[16 * g8 + h, g8, 112 + h] = 1.0
    return g.reshape(128, 8 * 240)


GSEL = make_gsel()


def build():
    nc = bass.Bass("TRN2", target_bir_lowering=False, dynamic_dma_scratch_size=8192)

    def din(name, shape):
        return nc.dram_tensor(name, list(shape), F32, kind="ExternalInput").ap()

    x_d = din("x", [NB * SEQ, D]); ctx_d = din("ctx", [NB * NCTX, D]); cond_d = din("cond", [NB + 1, D])
    wmod_d = din("w_mod", [D, 6 * D]); bmod_d = din("b_mod", [6 * D])
    n1g_d = din("norm1_g", [D]); n2g_d = din("norm2_g", [D]); win_d = din("w_in", [D, 2560])
    lre_d = din("lam_re", [2, 16, 64]); lim_d = din("lam_im", [2, 16, 64]); ldt_d = din("log_dt", [32])
    bre_d = din("b_re", [2, 16, 64, 16]); bim_d = din("b_im", [2, 16, 64, 16])
    cre_d = din("c_re", [512, 64]); cim_d = din("c_im", [512, 64])
    s5d_d = din("s5_d", [256]); wglu_d = din("w_glu", [256, 256]); bglu_d = din("b_glu", [256])
    cw_d = din("conv_w", [3, 768]); wout_d = din("w_out", [D, D])
    rgw_d = din("rgw", [D, 4]); rgb_d = din("rgb", [4]); rew_d = din("rew", [D, 16]); reb_d = din("reb", [16])
    w1_d = din("w1", [16, D, 512]); w3_d = din("w3", [16, D, 512]); w2_d = din("w2", [16, 512, D])
    fg_d = din("final_g", [D]); consts_d = din("consts", list(CONSTS.shape)); gsel_d = din("gsel", [128, 1920])
    out_d = nc.dram_tensor("out", [NB * SEQ, D], F32, kind="ExternalOutput").ap()
    ycat_d = nc.dram_tensor("ycat_scr", [NB, 128, 8, SEQ], BF16, kind="Internal").ap()
    x1_d = nc.dram_tensor("x1_scr", [NB * SEQ, D], F32, kind="Internal").ap()
    modrow_d = nc.dram_tensor("modrow_scr", [NB + 1, 6 * D], F32, kind="Internal").ap()
    a2row_d = nc.dram_tensor("a2row_scr", [NB + 1, D], F32, kind="Internal").ap()
    rot_d = nc.dram_tensor("rot_scr", [16, 128, 2 * NLEV, 128], BF16, kind="Internal").ap()
    w1b_d = nc.dram_tensor("w1b_scr", [16, 128, 8, 512], BF16, kind="Internal").ap()
    w3b_d = nc.dram_tensor("w3b_scr", [16, 128, 8, 512], BF16, kind="Internal").ap()
    w2b_d = nc.dram_tensor("w2b_scr", [16, 128, 4, D], BF16, kind="Internal").ap()
    h2_d = nc.dram_tensor("h2_scr", [NB * SEQ, D], BF16, kind="Internal").ap()
    h2s_d = nc.dram_tensor("h2s_scr", [NSLOT, ROWW], BF16, kind="Internal").ap()
    moe_d = nc.dram_tensor("moe_scr", [NSLOT, D], BF16, kind="Internal").ap()

    with contextlib.ExitStack() as st:
        S = Sched(nc, st)

        def sb(name, shape, dt=F32):
            return st.enter_context(nc.sbuf_tensor(name, list(shape), dt))

        def ps(name, shape, dt=F32):
            return st.enter_context(nc.psum_tensor(name, list(shape), dt))

        V, A, P, T = nc.vector, nc.scalar, nc.gpsimd, nc.tensor
        NCQ = dict(allow_slow_non_contiguous=True)

        cst = sb("cst", list(CONSTS.shape)); b_cst = S.buf("cst")
        identf = cst[:, COFF["ident"][0]:COFF["ident"][0] + 128]
        iswapf = cst[:, COFF["iswap"][0]:COFF["iswap"][0] + 128]
        onesf = cst[:, COFF["ones"][0]:COFF["ones"][0] + 128]
        sgn = cst[:, COFF["sgn"][0]:COFF["sgn"][0] + 1]
        rowmask = cst[:, COFF["rowmask"][0]:COFF["rowmask"][0] + 8]
        maskf = cst[:, COFF["maskf"][0]:COFF["maskf"][0] + 128]
        maskb = cst[:, COFF["maskb"][0]:COFF["maskb"][0] + 128]
        epsc = cst[:, COFF["eps"][0]:COFF["eps"][0] + 1]
        lstf = cst[:, COFF["lst"][0]:COFF["lst"][0] + 128]
        thrf = cst[:, COFF["thr"][0]:COFF["thr"][0] + NST]
        iotap = cst[:, COFF["iotap"][0]:COFF["iotap"][0] + 1]
        posi = sb("posi", [128, 64], I32); b_posi = S.buf("posi")
        widx = sb("widx", [128, 4], I32); b_widx = [S.buf("widx0"), S.buf("widx1"), S.buf("widx2")]
        Gsel = sb("Gsel", [128, 8, 240], BF16); b_Gsel = S.buf("Gsel")
        identb = sb("identb", [128, 128], BF16); b_identb = S.buf("identb")
        modT = sb("modT", [128, 48, 8]); b_modT = S.buf("modT")
        A1T = sb("A1T", [128, 8, 8]); b_A1T = S.buf("A1T")
        A2T = sb("A2T", [128, 8, 8]); b_A2T = S.buf("A2T")
        gT12 = sb("gT12", [128, 2, 8]); b_gT12 = S.buf("gT12")
        rbias = sb("rbias", [128, 20]); b_rbias = S.buf("rbias")
        wr = sb("wr", [128, 8, 20], BF16); b_wr = S.buf("wr")
        cw = sb("cw", [128, 6, 3]); b_cw = S.buf("cw")
        dcol = sb("dcol", [128, 2]); b_dcol = S.buf("dcol")
        bglu = sb("bglu", [128, 2]); b_bglu = S.buf("bglu")
        wglu = sb("wglu", [128, 2, 256], BF16); b_wglu = S.buf("wglu")
        c1t = sb("c1t", [128, 32, NLEV]); b_c1t = S.buf("c1t")
        c2t = sb("c2t", [128, 32, NLEV]); b_c2t = S.buf("c2t")
        ss = sb("ss", [128, 4]); rstd = sb("rstd", [128, 4])
        b_ss = [S.buf("ss%d" % i) for i in range(4)]; b_rstd = [S.buf("rstd%d" % i) for i in range(4)]

        ARB = sb("arb", [128, ARB_N], BF16)
        ARF = sb("arf", [128, ARF_N], F32)

        class Carver:
            def __init__(self, t, n):
                self.t = t; self.n = n; self.o = 0

            def take(self, *shape):
                n = int(np.prod(shape))
                assert self.o + n <= self.n, ("arena overflow", self.o + n, self.n)
                ap = self.t[:, self.o:self.o + n]
                self.o += n
                if len(shape) == 2:
                    ap = ap.rearrange("p (a b) -> p a b", a=shape[0])
                elif len(shape) == 3:
                    ap = ap.rearrange("p (a b c) -> p a b c", a=shape[0], b=shape[1])
                return ap

        pbank = [ps("pb%d" % i, [128, 512]) for i in range(7)]
        b_pb = [S.buf("pb%d" % i) for i in range(7)]
        pT = ps("pT", [128, 8, 128], BF16); b_pT = S.buf("pT")

        S.dma("sp", lambda: nc.sync.dma_start(out=cst[:], in_=consts_d), [], b_cst)
        S.dma("pool", lambda: P.dma_start(out=identb[:], in_=consts_d[:, 0:128]), [], b_identb)
        S.dma("pool", lambda: P.dma_start(out=Gsel[:], in_=gsel_d.rearrange("p (a b) -> p a b", a=8)), [], b_Gsel)
        S.dma("sp", lambda: nc.sync.dma_start(out=rbias[:, 0:4], in_=rgb_d.partition_broadcast(128)), [], b_rbias)
        S.dma("sp", lambda: nc.sync.dma_start(out=rbias[:, 4:20], in_=reb_d.partition_broadcast(128)), [], b_rbias)
        S.dma("pool", lambda: P.dma_start(out=wr[:, :, 0:4], in_=rgw_d.rearrange("(k p) n -> p k n", p=128)), [], b_wr)
        S.dma("pool", lambda: P.dma_start(out=wr[:, :, 4:20], in_=rew_d.rearrange("(k p) n -> p k n", p=128)), [], b_wr)
        for tp in range(3):
            S.dma("sp", (lambda tp=tp: nc.sync.dma_start(out=cw[:, :, tp], in_=cw_d[tp].rearrange("(i p) -> p i", p=128), **NCQ)), [], b_cw)
        S.dma("sp", lambda: nc.sync.dma_start(out=dcol[:], in_=s5d_d.rearrange("(f p) -> p f", p=128), **NCQ), [], b_dcol)
        S.dma("sp", lambda: nc.sync.dma_start(out=bglu[:], in_=bglu_d.rearrange("(f p) -> p f", p=128), **NCQ), [], b_bglu)
        S.dma("pool", lambda: P.dma_start(out=wglu[:], in_=wglu_d.rearrange("(k p) n -> p k n", p=128)), [], b_wglu)
        S.dma("sp", lambda: nc.sync.dma_start(out=gT12[:, 0, :], in_=n1g_d.rearrange("(k p) -> p k", p=128), **NCQ), [], b_gT12)
        S.dma("sp", lambda: nc.sync.dma_start(out=gT12[:, 1, :], in_=n2g_d.rearrange("(k p) -> p k", p=128), **NCQ), [], b_gT12)

        cb = Carver(ARB, ARB_N); cf = Carver(ARF, ARF_N)
        win = cb.take(8, 2560); b_win = S.buf("win")
        S.dma("pool", lambda: P.dma_start(out=win[:, :, 0:1280], in_=win_d[:, 0:1280].rearrange("(k p) n -> p k n", p=128)), [], b_win, par=True)
        S.dma("pool", lambda: P.dma_start(out=win[:, :, 1280:2560], in_=win_d[:, 1280:2560].rearrange("(k p) n -> p k n", p=128)), [], b_win, par=True)
        wm = [cb.take(8, 512).rearrange("p k n -> p (k n)").bitcast(F32).rearrange("p (k n) -> p k n", k=8), cb.take(8, 512).rearrange("p k n -> p (k n)").bitcast(F32).rearrange("p (k n) -> p k n", k=8)]
        b_wm = [S.buf("wm0"), S.buf("wm1")]
        LFa = cb.take(16, 8, 16); LFb = cb.take(16, 8, 16); b_LFa = S.buf("LFa"); b_LFb = S.buf("LFb")
        LF = cb.take(32, 128); b_LF = S.buf("LF")
        LF4 = LF.rearrange("p a (b c) -> p a b c", b=8)
        RCa = cb.take(16, 16, 16); RCb = cb.take(16, 16, 16); b_RCa = S.buf("RCa"); b_RCb = S.buf("RCb")
        assert cb.o <= TAIL
        cb.o = TAIL
        Wt = cb.take(32, 128); b_Wt = S.buf("Wt")
        RC = cb.take(32, 256); b_RC = S.buf("RC")
        RC4 = RC.rearrange("p a (b c) -> p a b c", b=16)
        M0 = cb.take(16, 128); b_M0 = S.buf("M0")
        condT = cf.take(8, 8); b_condT = S.buf("condT")
        scT = cf.take(8, 8); b_scT = S.buf("scT")
        bmodT = cf.take(48); b_bmodT = S.buf("bmodT")
        NSM = 30
        sm = cf.take(NSM, 32); b_sm = [S.buf("sm%d" % i) for i in range(NSM)]
        Bm1 = cf.take(32, 16); Bm2 = cf.take(32, 16); b_Bm1 = S.buf("Bm1"); b_Bm2 = S.buf("Bm2")
        CIN = cf.take(4, 128); b_CIN = S.buf("CIN")
        CIN2 = cf.take(4, 128); b_CIN2 = S.buf("CIN2")
        Cm1 = cf.take(512); b_Cm1 = S.buf("Cm1")
        Cm2 = cf.take(512); b_Cm2 = S.buf("Cm2")
        Pre = cf.take(32, 16); Pim = cf.take(32, 16); b_P = [S.buf("P%d" % i) for i in range(16)]
        PEre = cf.take(32, 8); PEim = cf.take(32, 8); b_PE = S.buf("PE")
        PQre = cf.take(32, 8); PQim = cf.take(32, 8); PQt = cf.take(32, 8)
        b_PQ = S.buf("PQ"); b_PQi = S.buf("PQi"); b_PQt = S.buf("PQt")
        PRre = cf.take(32, 16); PRim = cf.take(32, 16); b_PR = S.buf("PR")
        Mt1 = cf.take(4, 128); Mt2 = cf.take(4, 128); b_Mt1 = S.buf("Mt1"); b_Mt2 = S.buf("Mt2")

        (LRE, LIM, DT, XR, XI, MAG, SN, CS, ARE, AIM, T1, T2, T3, NR, D2, RD, QRE, QIM, QB, T4) = range(20)

        def smv(i):
            return sm[:, i, :]

        for half in range(2):
            S.dma("sp", (lambda half=half: nc.sync.dma_start(out=sm[half * 64:(half + 1) * 64, LRE, :], in_=lre_d.rearrange("d g p -> p (d g)"), **NCQ)), [], b_sm[LRE])
            S.dma("sp", (lambda half=half: nc.sync.dma_start(out=sm[half * 64:(half + 1) * 64, LIM, :], in_=lim_d.rearrange("d g p -> p (d g)"), **NCQ)), [], b_sm[LIM])
        S.dma("sp", lambda: nc.sync.dma_start(out=smv(DT), in_=ldt_d.partition_broadcast(128)), [], b_sm[DT])
        S.dma("sp", lambda: nc.sync.dma_start(out=Bm1[0:64], in_=bre_d.rearrange("d g p h -> p (d g) h")), [], b_Bm1)
        S.dma("sp", lambda: nc.sync.dma_start(out=Bm1[64:128], in_=bim_d.rearrange("d g p h -> p (d g) h")), [], b_Bm1)
        S.dma("sp", lambda: nc.sync.dma_start(out=Bm2[0:64], in_=bim_d.rearrange("d g p h -> p (d g) h")), [], b_Bm2)
        S.dma("sp", lambda: nc.sync.dma_start(out=Bm2[64:128], in_=bre_d.rearrange("d g p h -> p (d g) h")), [], b_Bm2)
        S.dma("sp", lambda: nc.sync.dma_start(out=CIN[:, :, 0:64], in_=cre_d.rearrange("(r q) p -> q r p", q=128)), [], b_CIN)
        S.dma("sp", lambda: nc.sync.dma_start(out=CIN[:, :, 64:128], in_=cim_d.rearrange("(r q) p -> q r p", q=128)), [], b_CIN)
        S.dma("sp", lambda: nc.sync.dma_start(out=CIN2[:, :, 0:64], in_=cim_d.rearrange("(r q) p -> q r p", q=128)), [], b_CIN2)
        S.dma("sp", lambda: nc.sync.dma_start(out=CIN2[:, :, 64:128], in_=cre_d.rearrange("(r q) p -> q r p", q=128)), [], b_CIN2)
        for bb in range(5):
            S.dma("sp", (lambda bb=bb: nc.sync.dma_start(out=condT[:, :, bb], in_=cond_d[bb].rearrange("(k p) -> p k", p=128), **NCQ)), [], b_condT)
        S.op("act", lambda: A.activation(out=scT[:, :, 0:5], in_=condT[:, :, 0:5], func=AF.Silu), [b_condT], [b_scT])
        S.dma("sp", lambda: nc.sync.dma_start(out=bmodT, in_=bmod_d.rearrange("(t p) -> p t", p=128), **NCQ), [], b_bmodT)
        pmod = pbank[0][:, 0:384].rearrange("p (t e) -> p t e", e=8)
        for j in range(24):
            s_ = j % 2
            S.dma("sp", (lambda j=j, s_=s_: nc.sync.dma_start(out=wm[s_], in_=wmod_d[:, j * 256:(j + 1) * 256].rearrange("(k p) n -> p k n", p=128))), [], b_wm[s_])
            for f in range(2):
                t = j * 2 + f
                for k in range(8):
                    S.op("pe", (lambda t=t, f=f, k=k, s_=s_: T.matmul(pmod[:, t, 0:5], lhsT=wm[s_][:, k, f * 128:(f + 1) * 128], rhs=scT[:, k, 0:5], start=(k == 0), stop=(k == 7))), [b_wm[s_], b_scT], [b_pb[0]])
        for b in range(5):
            S.op("dve", (lambda b=b: V.tensor_tensor(out=modT[:, :, b], in0=pmod[:, :, b], in1=bmodT, op=ALU.add)), [b_pb[0], b_bmodT], [b_modT])
        S.op("dve", lambda: V.tensor_scalar(out=A1T[:, :, 0:5], in0=modT[:, 8:16, 0:5], scalar1=1.0, scalar2=None, op0=ALU.add), [b_modT], [b_A1T])
        S.op("dve", lambda: V.tensor_scalar(out=A2T[:, :, 0:5], in0=modT[:, 32:40, 0:5], scalar1=1.0, scalar2=None, op0=ALU.add), [b_modT], [b_A2T])
        for b in range(5):
            S.op("dve", (lambda b=b: V.tensor_tensor(out=A1T[:, :, b], in0=A1T[:, :, b], in1=gT12[:, 0, :], op=ALU.mult)), [b_A1T, b_gT12], [b_A1T])
            S.op("dve", (lambda b=b: V.tensor_tensor(out=A2T[:, :, b], in0=A2T[:, :, b], in1=gT12[:, 1, :], op=ALU.mult)), [b_A2T, b_gT12], [b_A2T])

        b_modrow = S.buf("modrow_d")
        for bb in range(NB):
            S.dma("sp", (lambda bb=bb: nc.sync.dma_start(out=modrow_d[bb].rearrange("(t p) -> p t", p=128), in_=modT[:, :, bb], **NCQ)), [b_modT], b_modrow, par=True)
            S.dma("sp", (lambda bb=bb: nc.sync.dma_start(out=a2row_d[bb].rearrange("(k p) -> p k", p=128), in_=A2T[:, :, bb], **NCQ)), [b_A2T], b_modrow, par=True)
        (LRE, LIM, DT, XR, XI, MAG, SN, CS, ARE, AIM, T1, T2, T3, NR, D2, RD, QRE, QIM, QB, T4) = range(20)

        def smv(i):
            return sm[:, i, :]

        def dv(fn, reads, writes):
            S.op("dve", fn, [b_sm[i] for i in reads], [b_sm[i] for i in writes])

        def tt(o, a, b, op):
            dv((lambda: V.tensor_tensor(out=smv(o), in0=smv(a), in1=smv(b), op=op)), [a, b], [o])

        S.op("act", lambda: A.activation(out=smv(DT), in_=smv(DT), func=AF.Exp), [b_sm[DT]], [b_sm[DT]])
        tt(XR, LRE, DT, ALU.mult); tt(XI, LIM, DT, ALU.mult)
        S.op("act", lambda: A.activation(out=smv(MAG), in_=smv(XR), func=AF.Exp, scale=1.0 / 16), [b_sm[XR]], [b_sm[MAG]])
        S.op("act", lambda: A.activation(out=smv(SN), in_=smv(XI), func=AF.Sin, scale=1.0 / 16), [b_sm[XI]], [b_sm[SN]])
        dv((lambda: V.tensor_scalar(out=smv(T4), in0=smv(XI), scalar1=1.0 / 16, scalar2=math.pi / 2, op0=ALU.mult, op1=ALU.add)), [XI], [T4])
        S.op("act", lambda: A.activation(out=smv(CS), in_=smv(T4), func=AF.Sin), [b_sm[T4]], [b_sm[CS]])
        tt(ARE, MAG, CS, ALU.mult); tt(AIM, MAG, SN, ALU.mult)

        def csq(re, im):
            tt(T1, re, re, ALU.mult); tt(T2, im, im, ALU.mult); tt(T3, re, im, ALU.mult)
            tt(re, T1, T2, ALU.subtract)
            dv((lambda: V.tensor_scalar(out=smv(im), in0=smv(T3), scalar1=2.0, scalar2=None, op0=ALU.mult)), [T3], [im])

        for _ in range(4):
            csq(ARE, AIM)
        dv((lambda: V.tensor_scalar(out=smv(NR), in0=smv(ARE), scalar1=-1.0, scalar2=None, op0=ALU.add)), [ARE], [NR])
        tt(T1, LRE, LRE, ALU.mult); tt(T2, LIM, LIM, ALU.mult); tt(D2, T1, T2, ALU.add)
        dv((lambda: V.reciprocal(out=smv(RD), in_=smv(D2))), [D2], [RD])
        tt(T1, NR, LRE, ALU.mult); tt(T2, AIM, LIM, ALU.mult); tt(T3, T1, T2, ALU.add); tt(QRE, T3, RD, ALU.mult)
        tt(T1, AIM, LRE, ALU.mult); tt(T2, NR, LIM, ALU.mult); tt(T3, T1, T2, ALU.subtract); tt(QIM, T3, RD, ALU.mult)
        S.op("dve", lambda: V.tensor_scalar(out=smv(QB), in0=smv(QIM), scalar1=sgn, scalar2=-1.0, op0=ALU.mult, op1=ALU.mult), [b_sm[QIM], b_cst], [b_sm[QB]])
        def cmulP(o_re, o_im, bo, x_re, x_im, bx, y_re, y_im, by):
            bxy = list(bx) + list(by)
            S.op("dve", (lambda: V.tensor_tensor(out=smv(T1), in0=x_re, in1=y_re, op=ALU.mult)), bxy, [b_sm[T1]])
            S.op("dve", (lambda: V.tensor_tensor(out=smv(T2), in0=x_im, in1=y_im, op=ALU.mult)), bxy, [b_sm[T2]])
            S.op("dve", (lambda: V.tensor_tensor(out=o_re, in0=smv(T1), in1=smv(T2), op=ALU.subtract)), [b_sm[T1], b_sm[T2]], [bo])
            S.op("dve", (lambda: V.tensor_tensor(out=smv(T1), in0=x_re, in1=y_im, op=ALU.mult)), bxy, [b_sm[T1]])
            S.op("dve", (lambda: V.tensor_tensor(out=smv(T2), in0=x_im, in1=y_re, op=ALU.mult)), bxy, [b_sm[T2]])
            S.op("dve", (lambda: V.tensor_tensor(out=o_im, in0=smv(T1), in1=smv(T2), op=ALU.add)), [b_sm[T1], b_sm[T2]], [bo])

        AIRE, AIIM, A8RE, A8IM = 20, 21, 22, 23
        b_A = S.buf("a_pair")
        S.op("dve", lambda: V.memset(Pre[:, :, 7], 1.0), [], [b_P[7]])
        S.op("dve", lambda: V.memset(Pim[:, :, 7], 0.0), [], [b_P[7]])
        S.op("dve", lambda: V.tensor_copy(out=Pre[:, :, 8], in_=smv(ARE)), [b_sm[ARE], b_sm[AIM]], [b_P[8]])
        S.op("dve", lambda: V.tensor_copy(out=Pim[:, :, 8], in_=smv(AIM)), [b_sm[AIM]], [b_P[8]])
        for m in range(8, 15):
            cmulP(Pre[:, :, m + 1], Pim[:, :, m + 1], b_P[m + 1], Pre[:, :, m], Pim[:, :, m], [b_P[m]], smv(ARE), smv(AIM), [b_sm[ARE], b_sm[AIM]])
        tt(T1, ARE, ARE, ALU.mult); tt(T2, AIM, AIM, ALU.mult); tt(T3, T1, T2, ALU.add)
        dv((lambda: V.reciprocal(out=smv(T4), in_=smv(T3))), [T3], [T4])
        tt(AIRE, ARE, T4, ALU.mult)
        dv((lambda: V.scalar_tensor_tensor(out=smv(AIIM), in0=smv(AIM), scalar=-1.0, in1=smv(T4), op0=ALU.mult, op1=ALU.mult)), [AIM, T4], [AIIM])
        b_AI = S.buf("ainv_pair")
        for m in range(7, 0, -1):
            cmulP(Pre[:, :, m - 1], Pim[:, :, m - 1], b_P[m - 1], Pre[:, :, m], Pim[:, :, m], [b_P[m]], smv(AIRE), smv(AIIM), [b_sm[AIRE], b_sm[AIIM]])
        S.op("dve", lambda: V.tensor_copy(out=smv(A8RE), in_=Pre[:, :, 15]), [b_P[15]], [b_sm[A8RE]])
        S.op("dve", lambda: V.tensor_copy(out=smv(A8IM), in_=Pim[:, :, 15]), [b_P[15]], [b_sm[A8IM]])
        for m in range(NLEV):
            S.op("dve", (lambda m=m: V.tensor_copy(out=c1t[:, :, m], in_=smv(A8RE))), [b_sm[A8RE]], [b_c1t])
            S.op("dve", (lambda m=m: V.tensor_scalar(out=c2t[:, :, m], in0=smv(A8IM), scalar1=sgn, scalar2=None, op0=ALU.mult)), [b_sm[A8IM], b_cst], [b_c2t])
            if m < NLEV - 1:
                csq(A8RE, A8IM)
        b_rotds = [S.buf("rot_d0"), S.buf("rot_d1")]
        b_Pall = b_P
        for s8 in range(8):
            S.op("dve", (lambda s8=s8: V.tensor_copy(out=PEre[:, 0:16, s8], in_=Pre[:, 0:16, 14 - s8])), [b_P[14 - s8]], [b_PE])
            S.op("dve", (lambda s8=s8: V.tensor_copy(out=PEim[:, 0:16, s8], in_=Pim[:, 0:16, 14 - s8])), [b_P[14 - s8]], [b_PE])
            S.op("dve", (lambda s8=s8: V.tensor_copy(out=PEre[:, 16:32, s8], in_=Pre[:, 16:32, 7 + s8])), [b_P[7 + s8]], [b_PE])
            S.op("dve", (lambda s8=s8: V.tensor_copy(out=PEim[:, 16:32, s8], in_=Pim[:, 16:32, 7 + s8])), [b_P[7 + s8]], [b_PE])
        qre_b = smv(QRE).unsqueeze(2).broadcast_to([128, 32, 8])
        qim_b = smv(QIM).unsqueeze(2).broadcast_to([128, 32, 8])
        S.op("dve", lambda: V.tensor_tensor(out=PQre, in0=PEre, in1=qre_b, op=ALU.mult), [b_PE, b_sm[QRE]], [b_PQ])
        S.op("dve", lambda: V.tensor_tensor(out=PQt, in0=PEim, in1=qim_b, op=ALU.mult), [b_PE, b_sm[QIM]], [b_PQt])
        S.op("dve", lambda: V.tensor_tensor(out=PQre, in0=PQre, in1=PQt, op=ALU.subtract), [b_PQ, b_PQt], [b_PQ])
        S.op("dve", lambda: V.tensor_tensor(out=PQim, in0=PEre, in1=qim_b, op=ALU.mult), [b_PE, b_sm[QIM]], [b_PQi])
        S.op("dve", lambda: V.tensor_tensor(out=PQt, in0=PEim, in1=qre_b, op=ALU.mult), [b_PE, b_sm[QRE], b_PQ], [b_PQt])
        S.op("dve", lambda: V.tensor_tensor(out=PQim, in0=PQim, in1=PQt, op=ALU.add), [b_PQi, b_PQt], [b_PQi])
        S.op("dve", lambda: V.tensor_scalar(out=PQim, in0=PQim, scalar1=sgn, scalar2=-1.0, op0=ALU.mult, op1=ALU.mult), [b_PQi, b_cst], [b_PQi])
        for d in range(2):
            dsl = slice(d * 16, (d + 1) * 16)
            S.op("dve", (lambda dsl=dsl: V.tensor_tensor(out=LFa, in0=PQre[:, dsl, :].unsqueeze(3).broadcast_to([128, 16, 8, 16]), in1=Bm1[:, dsl, :].unsqueeze(2).broadcast_to([128, 16, 8, 16]), op=ALU.mult)), [b_PQ, b_Bm1], [b_LFa])
            S.op("dve", (lambda dsl=dsl: V.tensor_tensor(out=LFb, in0=PQim[:, dsl, :].unsqueeze(3).broadcast_to([128, 16, 8, 16]), in1=Bm2[:, dsl, :].unsqueeze(2).broadcast_to([128, 16, 8, 16]), op=ALU.mult)), [b_PQi, b_Bm2], [b_LFb])
            S.op("dve", (lambda dsl=dsl: V.tensor_tensor(out=LF4[:, dsl], in0=LFa, in1=LFb, op=ALU.add)), [b_LFa, b_LFb], [b_LF])
        for r in range(4):
            for q8 in range(8):
                dg = r * 8 + q8
                S.op("pe", (lambda dg=dg, q8=q8: T.transpose(out=pT[:, q8, :], in_=LF[:, dg, :], identity=identb[:])), [b_LF, b_identb], [b_pT])
            S.op("act", (lambda r=r: A.copy(out=Wt[:, r * 8:(r + 1) * 8, :], in_=pT[:, :, :])), [b_pT], [b_Wt])
        for r in range(4):
            S.op("pe", (lambda r=r: T.transpose(out=pbank[1][:, r * 128:(r + 1) * 128], in_=CIN[:, r, :], identity=identf)), [b_CIN, b_cst], [b_pb[1]])
        S.op("dve", lambda: V.tensor_scalar(out=Cm1, in0=pbank[1][:, :], scalar1=sgn, scalar2=None, op0=ALU.mult), [b_pb[1], b_cst], [b_Cm1])
        for r in range(4):
            S.op("pe", (lambda r=r: T.transpose(out=pbank[2][:, r * 128:(r + 1) * 128], in_=CIN2[:, r, :], identity=identf)), [b_CIN2, b_cst], [b_pb[2]])
        S.op("dve", lambda: V.tensor_scalar(out=Cm2, in0=pbank[2][:, :], scalar1=sgn, scalar2=None, op0=ALU.mult), [b_pb[2], b_cst], [b_Cm2])
        S.op("dve", lambda: V.tensor_copy(out=PRre[:, 0:16, :], in_=Pre[:, 0:16, :]), b_P, [b_PR])
        S.op("dve", lambda: V.tensor_copy(out=PRim[:, 0:16, :], in_=Pim[:, 0:16, :]), b_P, [b_PR])
        for m in range(16):
            S.op("dve", (lambda m=m: V.tensor_copy(out=PRre[:, 16:32, m], in_=Pre[:, 16:32, 15 - m])), [b_P[15 - m]], [b_PR])
            S.op("dve", (lambda m=m: V.tensor_copy(out=PRim[:, 16:32, m], in_=Pim[:, 16:32, 15 - m])), [b_P[15 - m]], [b_PR])
        S.op("dve", lambda: V.tensor_scalar(out=PRim, in0=PRim, scalar1=sgn, scalar2=-1.0, op0=ALU.mult, op1=ALU.mult), [b_PR, b_cst], [b_PR])
        Cm1v = Cm1.rearrange("p (a b) -> p a b", a=32)
        Cm2v = Cm2.rearrange("p (a b) -> p a b", a=32)
        for d in range(2):
            dsl = slice(d * 16, (d + 1) * 16)
            S.op("dve", (lambda dsl=dsl: V.tensor_tensor(out=RCa, in0=PRre[:, dsl, :].unsqueeze(3).broadcast_to([128, 16, 16, 16]), in1=Cm1v[:, dsl, :].unsqueeze(2).broadcast_to([128, 16, 16, 16]), op=ALU.mult)), [b_PR, b_Cm1], [b_RCa])
            S.op("dve", (lambda dsl=dsl: V.tensor_tensor(out=RCb, in0=PRim[:, dsl, :].unsqueeze(3).broadcast_to([128, 16, 16, 16]), in1=Cm2v[:, dsl, :].unsqueeze(2).broadcast_to([128, 16, 16, 16]), op=ALU.mult)), [b_PR, b_Cm2], [b_RCb])
            S.op("dve", (lambda dsl=dsl: V.tensor_tensor(out=RC4[:, dsl], in0=RCa, in1=RCb, op=ALU.add)), [b_RCa, b_RCb], [b_RC])
        for g4 in range(4):
            for gi in range(4):
                g = g4 * 4 + gi
                S.op("pe", (lambda g=g, gi=gi: T.matmul(pbank[3][:, gi * 128:(gi + 1) * 128], lhsT=LF[:, g, :], rhs=RC[:, g, 0:128], start=True, stop=True)), [b_LF, b_RC], [b_pb[3]])
                S.op("pe", (lambda g=g, gi=gi: T.matmul(pbank[4][:, gi * 128:(gi + 1) * 128], lhsT=LF[:, 16 + g, :], rhs=RC[:, 16 + g, 128:256], start=True, stop=True)), [b_LF, b_RC], [b_pb[4]])
            S.op("dve", lambda: V.tensor_tensor(out=Mt1, in0=pbank[3][:, :].rearrange("p (a b) -> p a b", a=4), in1=maskf.unsqueeze(1).broadcast_to([128, 4, 128]), op=ALU.mult), [b_pb[3], b_cst], [b_Mt1])
            S.op("dve", lambda: V.tensor_tensor(out=Mt2, in0=pbank[4][:, :].rearrange("p (a b) -> p a b", a=4), in1=maskb.unsqueeze(1).broadcast_to([128, 4, 128]), op=ALU.mult), [b_pb[4], b_cst], [b_Mt2])
            S.op("dve", (lambda g4=g4: V.tensor_tensor(out=M0[:, g4 * 4:(g4 + 1) * 4, :], in0=Mt1, in1=Mt2, op=ALU.add)), [b_Mt1, b_Mt2], [b_M0])

        S.barrier()

        cb = Carver(ARB, ARB_N); cf = Carver(ARF, ARF_N)
        cb.take(8, 2560)
        UT = cb.take(2, 2560); b_UT = [S.buf("UT0"), S.buf("UT1")]
        o_alias = cb.o
        cvcol = cb.take(3, SEQ); b_cvcol = [S.buf("cvcol%d" % i) for i in range(3)]
        bcol = cb.take(3, SEQ); b_bcol = [S.buf("bcol%d" % i) for i in range(3)]
        o_end = cb.o
        cb.o = o_alias
        Hs2 = [[cb.take(NBLK), cb.take(NBLK)], [cb.take(NBLK), cb.take(NBLK)]]
        b_Hs2 = [[S.buf("Hs00"), S.buf("Hs01")], [S.buf("Hs10"), S.buf("Hs11")]]
        rot = [cb.take(2 * NLEV, 128), cb.take(2 * NLEV, 128)]; b_rot = [S.buf("rot0"), S.buf("rot1")]
        gTt = cb.take(2, SEQ); b_gTt = [S.buf("gT0"), S.buf("gT1")]
        Yall = cb.take(8, 256); b_Yall = S.buf("Yall")
        assert cb.o <= o_end
        cb.o = o_end
        hT0 = cb.take(8, 512)
        junk = cb.take(D); b_junk = S.buf("junk")
        xn = cb.take(D); b_xn = S.buf("xn")
        yst = [cb.take(512), cb.take(512)]; b_yst = [S.buf("yst0"), S.buf("yst1")]
        ycolst = cb.take(SEQ); b_ycolst = S.buf("ycolst")
        Xg = [ycolst[:, 0:NX], ycolst[:, 1024:1024 + NX]]; b_Xg = [S.buf("Xg0"), S.buf("Xg1")]
        assert cb.o <= TAIL, cb.o
        cb.o = TAIL + 14336
        hT1 = cb.take(8, 512)
        hTs = [hT0, hT1]; b_hTs = [S.buf("hT0"), S.buf("hT1")]
        hcnt = {"i": 0}
        xt = [cf.take(D), cf.take(D)]; b_xt = [S.buf("xt0"), S.buf("xt1")]
        ysacc = cf.take(SEQ); b_ysacc = S.buf("ysacc")
        Csb2 = [cf.take(512), cf.take(512)]; b_Csb2 = [S.buf("Csb0"), S.buf("Csb1")]
        cvt2 = [cf.take(512), cf.take(512)]; b_cvt2 = [S.buf("cvt0"), S.buf("cvt1")]
        cacc2 = [cf.take(512), cf.take(512)]; b_cacc2 = [S.buf("cacc0"), S.buf("cacc1")]
        ge1 = cf.take(512); ge2 = cf.take(512); b_ge1 = S.buf("ge1"); b_ge2 = S.buf("ge2")
        rt1 = cf.take(128); rt2 = cf.take(128); b_rt1 = S.buf("rt1"); b_rt2 = S.buf("rt2")
        colacc = cf.take(SEQ) if False else None


        b_wb = S.buf("wb_scr")
        precast = []
        for e in range(16):
            precast.append(lambda e=e: S.dma("pool", (lambda: P.dma_start(out=w1b_d[e], in_=w1_d[e].rearrange("(k p) n -> p k n", p=128))), [], b_wb))
            precast.append(lambda e=e: S.dma("pool", (lambda: P.dma_start(out=w3b_d[e], in_=w3_d[e].rearrange("(k p) n -> p k n", p=128))), [], b_wb))
            precast.append(lambda e=e: S.dma("pool", (lambda: P.dma_start(out=w2b_d[e], in_=w2_d[e].rearrange("(k p) n -> p k n", p=128))), [], b_wb))
        b_ycats = [S.buf("ycat_d%d" % i) for i in range(4)]
        b_x1ds = [S.buf("x1_d%d" % i) for i in range(4)]
        b_outs = [S.buf("out_d%d" % i) for i in range(4)]
        rr = {"yc": 0, "out": 0}

        def nyc():
            rr["yc"] += 1
            return b_ycats[rr["yc"] % 4]
        cnt = {"xt": 0, "ev": 0, "ss": 0}

        def norm_transpose(nb, src_rows, AT, ST, bA, bS, idx, dst, b_dst, eps=1e-6, src_sb=None, b_src=None):
            junk, xn, b_junk, b_xn, xt, b_xt = nb
            if src_sb is None:
                i = cnt["xt"] % 2; cnt["xt"] += 1
                xs = xt[i]; bx = b_xt[i]
                S.dma("sp", (lambda: nc.sync.dma_start(out=xs, in_=src_rows)), [], bx)
            else:
                xs = src_sb; bx = b_src
            j = cnt["ss"] % 4; cnt["ss"] += 1
            S.op("act", (lambda: A.activation(out=junk, in_=xs, func=AF.Square, accum_out=ss[:, j:j + 1])), [bx], [b_junk, b_ss[j]])
            S.op("act", (lambda: A.activation(out=rstd[:, j:j + 1], in_=ss[:, j:j + 1], func=AF.Sqrt, scale=1.0 / D, bias=epsc)), [b_ss[j], b_cst], [b_rstd[j]])
            S.op("dve", (lambda: V.reciprocal(out=rstd[:, j:j + 1], in_=rstd[:, j:j + 1])), [b_rstd[j]], [b_rstd[j]])
            S.op("act", (lambda: A.activation(out=xn, in_=xs, func=AF.Copy, scale=rstd[:, j:j + 1])), [bx, b_rstd[j]], [b_xn])
            for k in range(8):
                S.op("pe", (lambda k=k: T.transpose(out=pT[:, k, :], in_=xn[:, k * 128:(k + 1) * 128], identity=identb[:])), [b_xn, b_identb], [b_pT])
            for k in range(8):
                if k % 2 == 0:
                    S.op("dve", (lambda k=k: V.tensor_scalar(out=dst[:, k, :], in0=pT[:, k, :], scalar1=AT[:, k, idx:idx + 1], scalar2=ST[:, k, idx:idx + 1], op0=ALU.mult, op1=ALU.add)), [b_pT, bA, bS], [b_dst])
                else:
                    S.op("act", (lambda k=k: A.activation(out=dst[:, k, :], in_=pT[:, k, :], func=AF.Identity, scale=AT[:, k, idx:idx + 1], bias=ST[:, k, idx:idx + 1])), [b_pT, bA, bS], [b_dst])
            return j

        S1T = modT[:, 0:8, :]
        nbB = (junk, xn, b_junk, b_xn, xt, b_xt)
        S2T = modT[:, 24:32, :]
        pz = {"i": 0}

        def zbank():
            i = 1 + (pz["i"] % 6); pz["i"] += 1
            return pbank[i], b_pb[i]

        def ctx_norms(bb):
            hT_ = hTs[hcnt["i"] % 2]; b_hT_ = b_hTs[hcnt["i"] % 2]; hcnt["i"] += 1
            for ti in range(2):
                norm_transpose(nbB, ctx_d[bb * NCTX + ti * 128: bb * NCTX + (ti + 1) * 128, :], A1T, S1T, b_A1T, b_modT, 4, hT_[:, :, ti * 128:(ti + 1) * 128], b_hT_)
            return hT_, b_hT_

        def chunk_norms(bb, c_):
            t0_ = bb * SEQ + c_ * 512
            hT_ = hTs[hcnt["i"] % 2]; b_hT_ = b_hTs[hcnt["i"] % 2]; hcnt["i"] += 1
            for ti in range(4):
                norm_transpose(nbB, x_d[t0_ + ti * 128: t0_ + (ti + 1) * 128, :], A1T, S1T, b_A1T, b_modT, bb, hT_[:, :, ti * 128:(ti + 1) * 128], b_hT_)
            return hT_, b_hT_

        def chunk_norm_alloc():
            hT_ = hTs[hcnt["i"] % 2]; b_hT_ = b_hTs[hcnt["i"] % 2]; hcnt["i"] += 1
            return hT_, b_hT_

        def chunk_norm_tile(bb, c_, ti, hT_, b_hT_):
            t0_ = bb * SEQ + c_ * 512
            norm_transpose(nbB, x_d[t0_ + ti * 128: t0_ + (ti + 1) * 128, :], A1T, S1T, b_A1T, b_modT, bb, hT_[:, :, ti * 128:(ti + 1) * 128], b_hT_)

        pref = {}
        for b in range(NB):
            if b in pref:
                (hT, b_hT), pre_c0 = pref[b]
            else:
                hT, b_hT = ctx_norms(b)
                pre_c0 = None
            for ft in range(2):
                pb_, bpb_ = zbank()
                for k in range(8):
                    S.op("pe", (lambda k=k, ft=ft, pb_=pb_, hT=hT: T.matmul(pb_[:, 0:256], lhsT=win[:, k, ft * 128:(ft + 1) * 128], rhs=hT[:, k, 0:256], start=(k == 0), stop=(k == 7))), [b_win, b_hT], [bpb_])
                S.op("act", (lambda ft=ft, pb_=pb_: A.copy(out=UT[:, ft, 0:256], in_=pb_[:, 0:256])), [bpb_], [b_UT[ft]])
                S.op("dve", (lambda ft=ft, pb_=pb_: V.tensor_copy(out=UT[:, ft, 2304:2560], in_=pb_[:, 0:256])), [bpb_], [b_UT[ft]])
            def do_norms(c_):
                return chunk_norms(b, c_)

            nxt_h = pre_c0 if pre_c0 is not None else do_norms(0)
            for c in range(4):
                t0 = b * SEQ + c * 512
                hT, b_hT = nxt_h

                def zx(col0, hT=hT, b_hT=b_hT):
                    pb_, bpb_ = zbank()
                    for k in range(8):
                        S.op("pe", (lambda k=k, pb_=pb_: T.matmul(pb_[:, :], lhsT=win[:, k, col0:col0 + 128], rhs=hT[:, k, :], start=(k == 0), stop=(k == 7))), [b_win, b_hT], [bpb_])
                    return pb_, bpb_

                for ft in range(2):
                    pb_, bpb_ = zx(ft * 128)
                    S.op("act", (lambda ft=ft, pb_=pb_, c=c: A.copy(out=UT[:, ft, 256 + c * 512: 256 + (c + 1) * 512], in_=pb_[:, :])), [bpb_], [b_UT[ft]])
                if c + 1 < 4:
                    nxt_h = chunk_norm_alloc()
                for i in range(6):
                    if c + 1 < 4 and 1 <= i <= 4:
                        chunk_norm_tile(b, c + 1, i - 1, nxt_h[0], nxt_h[1])
                    q = i % 2
                    Csb_, cvt_, cacc_ = Csb2[q], cvt2[q], cacc2[q]
                    bCsb_, bcvt_, bcacc_ = b_Csb2[q], b_cvt2[q], b_cacc2[q]
                    pC, bC = zx(1024 + i * 128)
                    S.op("act", (lambda pC=pC, Csb_=Csb_: A.copy(out=Csb_, in_=pC[:, :])), [bC], [bCsb_])
                    pV, bV = zx(1792 + i * 128)
                    if i < 3:
                        S.op("dve", (lambda pV=pV, Csb_=Csb_, cvt_=cvt_: V.tensor_tensor(out=cvt_, in0=Csb_, in1=pV[:, :], op=ALU.mult)), [bCsb_, bV], [bcvt_])
                        pB, bB = zx(256 + i * 128)
                        S.op("act", (lambda i=i, cvt_=cvt_, cacc_=cacc_: A.activation(out=cacc_, in_=cvt_, func=AF.Copy, scale=cw[:, i, 1:2])), [bcvt_, b_cw], [bcacc_])
                        c3 = cvt_.rearrange("p (r w) -> p r w", w=64)
                        a3 = cacc_.rearrange("p (r w) -> p r w", w=64)
                        S.op("dve", (lambda i=i, c3=c3, a3=a3: V.scalar_tensor_tensor(out=a3[:, :, 1:64], in0=c3[:, :, 0:63], scalar=cw[:, i, 0:1], in1=a3[:, :, 1:64], op0=ALU.mult, op1=ALU.add)), [bcvt_, bcacc_, b_cw], [bcacc_])
                        S.op("dve", (lambda i=i, c3=c3, a3=a3: V.scalar_tensor_tensor(out=a3[:, :, 0:63], in0=c3[:, :, 1:64], scalar=cw[:, i, 2:3], in1=a3[:, :, 0:63], op0=ALU.mult, op1=ALU.add)), [bcvt_, bcacc_, b_cw], [bcacc_])
                        yi = (c * 6 + i) % 2
                        S.op("dve", (lambda pB=pB, yi=yi, cacc_=cacc_: V.tensor_tensor(out=yst[yi], in0=cacc_, in1=pB[:, :], op=ALU.mult)), [bcacc_, bB], [b_yst[yi]])
                        S.dma("pool", (lambda b=b, i=i, c=c, yi=yi: P.dma_start(out=ycat_d[b, :, 2 + i, c * 512:(c + 1) * 512], in_=yst[yi])), [b_yst[yi]], nyc())
                    else:
                        j = i - 3
                        S.op("dve", (lambda pV=pV, j=j, c=c, Csb_=Csb_: V.tensor_tensor(out=cvcol[:, j, c * 512:(c + 1) * 512], in0=Csb_, in1=pV[:, :], op=ALU.mult)), [bCsb_, bV], [b_cvcol[j]])
                        pB, bB = zx(256 + i * 128)
                        S.op("act", (lambda pB=pB, j=j, c=c: A.copy(out=bcol[:, j, c * 512:(c + 1) * 512], in_=pB[:, :])), [bB], [b_bcol[j]])
            for j in range(3):
                i = 3 + j
                S.op("act", (lambda j=j, i=i: A.activation(out=ysacc, in_=cvcol[:, j, :], func=AF.Copy, scale=cw[:, i, 1:2])), [b_cvcol[j], b_cw], [b_ysacc])
                S.op("dve", (lambda j=j, i=i: V.scalar_tensor_tensor(out=ysacc[:, 64:SEQ], in0=cvcol[:, j, 0:SEQ - 64], scalar=cw[:, i, 0:1], in1=ysacc[:, 64:SEQ], op0=ALU.mult, op1=ALU.add)), [b_cvcol[j], b_ysacc, b_cw], [b_ysacc])
                S.op("dve", (lambda j=j, i=i: V.scalar_tensor_tensor(out=ysacc[:, 0:SEQ - 64], in0=cvcol[:, j, 64:SEQ], scalar=cw[:, i, 2:3], in1=ysacc[:, 0:SEQ - 64], op0=ALU.mult, op1=ALU.add)), [b_cvcol[j], b_ysacc, b_cw], [b_ysacc])
                S.op("pool", (lambda j=j: P.tensor_tensor(out=ycolst, in0=ysacc, in1=bcol[:, j, :], op=ALU.mult)), [b_ysacc, b_bcol[j]], [b_ycolst])
                S.dma("pool", (lambda b=b, j=j: P.dma_start(out=ycat_d[b, :, 5 + j, :], in_=ycolst)), [b_ycolst], nyc())
            S.barrier()
            for _ in range(16):
                if precast:
                    precast.pop(0)()
            if b + 1 < NB:
                pref[b + 1] = (ctx_norms(b + 1), chunk_norms(b + 1, 0))
            for ft in range(2):
                for gp in range(4):
                    gl = [(ft * 8 + gp * 2 + u, gp * 2 + u, u) for u in range(2)]
                    for (g, g8, ri) in gl:
                        if b == 0:
                            for d in range(2):
                                dg = d * 16 + g
                                for m in range(NLEV):
                                    S.op("dve", (lambda dg=dg, m=m: V.tensor_scalar(out=rt1, in0=identf, scalar1=c1t[:, dg, m:m + 1], scalar2=None, op0=ALU.mult)), [b_cst, b_c1t], [b_rt1])
                                    S.op("dve", (lambda d=d, dg=dg, m=m, ri=ri: V.scalar_tensor_tensor(out=rot[ri][:, d * NLEV + m, :], in0=iswapf, scalar=c2t[:, dg, m:m + 1], in1=rt1, op0=ALU.mult, op1=ALU.add)), [b_cst, b_c2t, b_rt1], [b_rot[ri]])
                            S.dma("pool", (lambda g=g, ri=ri: P.dma_start(out=rot_d[g], in_=rot[ri])), [b_rot[ri]], b_rotds[ri])
                        else:
                            S.dma("sp", (lambda g=g, ri=ri: nc.sync.dma_start(out=rot[ri], in_=rot_d[g])), b_rotds, b_rot[ri])
                        pb_, bpb_ = zbank()
                        for s8 in range(8):
                            S.op("pe", (lambda pb_=pb_, s8=s8, g8=g8, ft=ft: T.matmul(pb_[:, 0:NX], lhsT=Gsel[:, g8, 112 - 16 * s8: 240 - 16 * s8], rhs=UT[:, ft, s8:2560:8], start=(s8 == 0), stop=(s8 == 7))), [b_Gsel, b_UT[ft]], [bpb_])
                        S.op("act", (lambda pb_=pb_, ri=ri: A.copy(out=Xg[ri], in_=pb_[:, 0:NX])), [bpb_], [b_Xg[ri]])
                    for (g, g8, ri) in gl:
                        for d in range(2):
                            dg = d * 16 + g
                            off = 0 if d == 0 else 32
                            pb_, bpb_ = zbank()
                            S.op("pe", (lambda pb_=pb_, dg=dg, off=off, ri=ri: T.matmul(pb_[:, 0:NBLK], lhsT=Wt[:, dg, :], rhs=Xg[ri][:, off:off + NBLK], start=True, stop=True)), [b_Wt, b_Xg[ri]], [bpb_])
                            if d == 0:
                                S.op("act", (lambda pb_=pb_, d=d, ri=ri: A.copy(out=Hs2[ri][d], in_=pb_[:, 0:NBLK])), [bpb_], [b_Hs2[ri][d]])
                            else:
                                S.op("dve", (lambda pb_=pb_, d=d, ri=ri: V.tensor_copy(out=Hs2[ri][d], in_=pb_[:, 0:NBLK])), [bpb_], [b_Hs2[ri][d]])
                    for m in range(NLEV):
                        s_ = 1 << m
                        n = NBLK - s_
                        for (g, g8, ri) in gl:
                            for d in range(2):
                                lo, rlo = (s_, 0) if d == 0 else (0, s_)
                                pb_, bpb_ = zbank()
                                S.op("pe", (lambda pb_=pb_, d=d, m=m, rlo=rlo, n=n, ri=ri: T.matmul(pb_[:, 0:n], lhsT=rot[ri][:, d * NLEV + m, :], rhs=Hs2[ri][d][:, rlo:rlo + n], start=True, stop=True)), [b_rot[ri], b_Hs2[ri][d]], [bpb_])
                                S.op("dve", (lambda pb_=pb_, d=d, lo=lo, n=n, ri=ri: V.tensor_tensor(out=Hs2[ri][d][:, lo:lo + n], in0=pb_[:, 0:n], in1=Hs2[ri][d][:, lo:lo + n], op=ALU.add)), [bpb_, b_Hs2[ri][d]], [b_Hs2[ri][d]])
                    for (g, g8, ri) in gl:
                        pb_, bpb_ = zbank()
                        S.op("pe", (lambda pb_=pb_, g=g, ri=ri: T.matmul(pb_[:, 0:256], lhsT=M0[:, g, :], rhs=Xg[ri][:, 32:288], start=True, stop=False)), [b_M0, b_Xg[ri]], [bpb_])
                        S.op("pe", (lambda pb_=pb_, g=g, ri=ri: T.matmul(pb_[:, 0:256], lhsT=RC[:, g, 128:256], rhs=Hs2[ri][0][:, 31:287], start=False, stop=False)), [b_RC, b_Hs2[ri][0]], [bpb_])
                        S.op("pe", (lambda pb_=pb_, g=g, ri=ri: T.matmul(pb_[:, 0:256], lhsT=RC[:, 16 + g, 0:128], rhs=Hs2[ri][1][:, 1:257], start=False, stop=True)), [b_RC, b_Hs2[ri][1]], [bpb_])
                        S.op("act", (lambda pb_=pb_, g8=g8: A.copy(out=Yall[:, g8, :], in_=pb_[:, 0:256])), [bpb_], [b_Yall])
                for q in range(4):
                    pb_, bpb_ = zbank()
                    for t8 in range(8):
                        for g8 in range(8):
                            S.op("pe", (lambda pb_=pb_, t8=t8, g8=g8, q=q: T.matmul(pb_[:, t8:512:8], lhsT=Gsel[:, t8, 112 - 16 * g8: 240 - 16 * g8], rhs=Yall[:, g8, q * 64:(q + 1) * 64], start=(g8 == 0), stop=(g8 == 7))), [b_Gsel, b_Yall], [bpb_])
                    S.op("dve", (lambda pb_=pb_, q=q, ft=ft: V.scalar_tensor_tensor(out=ysacc[:, q * 512:(q + 1) * 512], in0=UT[:, ft, 256 + q * 512: 256 + (q + 1) * 512], scalar=dcol[:, ft:ft + 1], in1=pb_[:, :], op0=ALU.mult, op1=ALU.add)), [b_UT[ft], b_dcol, bpb_], [b_ysacc])
                for c in range(4):
                    xs_ = ysacc[:, c * 512:(c + 1) * 512]
                    S.op("act", (lambda xs_=xs_: A.activation(out=ge1, in_=xs_, func=AF.Square)), [b_ysacc], [b_ge1])
                    S.op("dve", (lambda: V.tensor_scalar(out=ge1, in0=ge1, scalar1=0.044715, scalar2=1.0, op0=ALU.mult, op1=ALU.add)), [b_ge1], [b_ge1])
                    S.op("dve", (lambda xs_=xs_: V.tensor_tensor(out=ge2, in0=ge1, in1=xs_, op=ALU.mult)), [b_ge1, b_ysacc], [b_ge2])
                    S.op("act", (lambda: A.activation(out=ge1, in_=ge2, func=AF.Sigmoid, scale=1.5957691216057308)), [b_ge2, b_ge1], [b_ge1])
                    S.op("dve", (lambda xs_=xs_, ft=ft, c=c: V.tensor_tensor(out=gTt[:, ft, c * 512:(c + 1) * 512], in0=ge1, in1=xs_, op=ALU.mult)), [b_ge1, b_ysacc], [b_gTt[ft]])
            for c in range(4):
                for f2 in range(2):
                    pb_, bpb_ = zbank()
                    for k in range(2):
                        S.op("pe", (lambda pb_=pb_, k=k, f2=f2, c=c: T.matmul(pb_[:, :], lhsT=wglu[:, k, f2 * 128:(f2 + 1) * 128], rhs=gTt[:, k, c * 512:(c + 1) * 512], start=(k == 0), stop=(k == 1))), [b_wglu, b_gTt[k]], [bpb_])
                    S.op("act", (lambda pb_=pb_, f2=f2: A.activation(out=ge1, in_=pb_[:, :], func=AF.Sigmoid, bias=bglu[:, f2:f2 + 1])), [bpb_, b_bglu], [b_ge1])
                    yi = (c * 2 + f2) % 2
                    S.op("dve", (lambda f2=f2, c=c, yi=yi: V.tensor_tensor(out=yst[yi], in0=ge1, in1=gTt[:, f2, c * 512:(c + 1) * 512], op=ALU.mult)), [b_ge1, b_gTt[f2]], [b_yst[yi]])
                    S.dma("pool", (lambda b=b, f2=f2, c=c, yi=yi: P.dma_start(out=ycat_d[b, :, f2, c * 512:(c + 1) * 512], in_=yst[yi])), [b_yst[yi]], nyc())
            S.barrier()

        S.barrier()

        while precast:
            precast.pop(0)()
        cf = Carver(ARF, ARF_N)
        OH_all = cf.take(64, 4); b_OH = S.buf("OH_all")
        GI_all = cf.take(64, 4); b_GI = S.buf("GI_all")
        POSf = cf.take(64); b_POSf = S.buf("POSf")
        GIDf = cf.take(NST); b_GIDf = S.buf("GIDf")
        ssC = cf.take(4); sdC = cf.take(4); b_ssC = [S.buf("ssC%d" % i) for i in range(4)]; b_sdC = [S.buf("sdC%d" % i) for i in range(4)]
        ef = cf.take(4); b_ef = [S.buf("ef0"), S.buf("ef1"), S.buf("ef2")]
        rdiag = cf.take(128); b_rdiag = S.buf("rdiag")
        gsl2 = [cf.take(8, 4), cf.take(8, 4)]; b_gsl2 = [S.buf("gsl0"), S.buf("gsl1")]
        f_persist = cf.o
        cntC = {"x": 0, "s": 0, "y": 0, "x3": 0, "r": 0}
        b_h2d = [S.buf("h2_d%d" % i) for i in range(4)]
        b_h2ss = [S.buf("h2s_d0"), S.buf("h2s_d1")]
        b_h2z = S.buf("h2s_zero")
        b_moe = S.buf("moe_d")

        cb = Carver(ARB, ARB_N)
        wout = cb.take(8, D); b_wout = S.buf("wout")
        yc = [cb.take(8, 512), cb.take(8, 512)]; b_yc = [S.buf("yc0"), S.buf("yc1")]
        xnC = [cb.take(D), cb.take(D)]; b_xnC = [S.buf("xnC0"), S.buf("xnC1")]
        xnT = [cb.take(8, 128), cb.take(8, 128)]; b_xnT = [S.buf("xnT0"), S.buf("xnT1")]
        rowbuf = [cb.take(ROWW), cb.take(ROWW)]; b_rowbuf = [S.buf("rowbuf0"), S.buf("rowbuf1")]
        wrb = cb.take(8, 20); b_wrb = S.buf("wrb")
        swr = cb.take(8, 20); b_swr = S.buf("swr")
        onesb = cb.take(128); b_onesb = S.buf("onesb")
        zrow = cb.take(8, ROWW); b_zrow = S.buf("zrow")
        xtC = [cf.take(D), cf.take(D)]; b_xtC = [S.buf("xtC0"), S.buf("xtC1")]
        A2b = cf.take(D); b_A2b = S.buf("A2b")
        S2b = cf.take(D); b_S2b = S.buf("S2b")
        G1b = cf.take(D); b_G1b = S.buf("G1b")
        rbb = cf.take(20); b_rbb = S.buf("rbb")
        lg = cf.take(8, 20); b_lg = S.buf("lg")
        NR_ = 18
        rsm = cf.take(NR_, 8, 4); b_rsm = [S.buf("rsm%d" % i) for i in range(NR_)]
        lgp = pbank[0][:, 0:160].rearrange("p (t e) -> p t e", e=20)
        b_lgp = b_pb[0]

        S.op("dve", lambda: V.memset(zrow, 0.0), [], [b_zrow])
        for s_ in range(NST):
            S.dma("sp", (lambda s_=s_: nc.sync.dma_start(out=h2s_d[s_ * TSZ:(s_ + 1) * TSZ, :].rearrange("(a p) c -> p a c", p=128), in_=zrow[:, 0:4, :])), [b_zrow], b_h2z, par=True)
        S.op("dve", lambda: V.tensor_copy(out=onesb, in_=onesf), [b_cst], [b_onesb])

        (GM, GE, GS, GP, OHG, EIN, ET, M1, MK1, E2, M2, MK2, DL, W1, W2, GI, GT) = range(17)

        def rs(i, n=4):
            return rsm[:, i, :, 0:n]

        def rop(fn, reads, writes, extra_r=(), extra_w=()):
            S.op("dve", fn, [b_rsm[i] for i in reads] + list(extra_r), [b_rsm[i] for i in writes] + list(extra_w))

        def bc4(ap1):
            return ap1.broadcast_to([128, 8, 4])

        def router_batch(sc):
            T0 = sc * 8
            S.op("dve", (lambda: V.tensor_tensor(out=lg, in0=lgp, in1=rbb.unsqueeze(1).broadcast_to([128, 8, 20]), op=ALU.add)), [b_lgp, b_rbb], [b_lg])
            rop((lambda: V.tensor_reduce(out=rs(GM, 1), in_=lg[:, :, 0:4], axis=AX.X, op=ALU.max)), [], [GM], [b_lg])
            rop((lambda: V.tensor_tensor(out=rs(GE), in0=lg[:, :, 0:4], in1=bc4(rs(GM, 1)), op=ALU.subtract)), [GM], [GE], [b_lg])
            S.op("act", (lambda: A.activation(out=rs(GE), in_=rs(GE), func=AF.Exp)), [b_rsm[GE]], [b_rsm[GE]])
            rop((lambda: V.tensor_reduce(out=rs(GS, 1), in_=rs(GE), axis=AX.X, op=ALU.add)), [GE], [GS])
            rop((lambda: V.reciprocal(out=rs(GP, 1), in_=rs(GS, 1))), [GS], [GP])
            rop((lambda: V.tensor_tensor(out=OH_all[:, T0:T0 + 8, :], in0=lg[:, :, 0:4], in1=bc4(rs(GM, 1)), op=ALU.is_equal)), [GM], [], [b_lg], [b_OH])
            ohv = OH_all[:, T0:T0 + 8, :]
            rop((lambda: V.tensor_tensor(out=rs(EIN), in0=lg[:, :, 4:8], in1=bc4(ohv[:, :, 0:1]), op=ALU.mult)), [], [EIN], [b_lg, b_OH])
            for g in range(1, 4):
                rop((lambda g=g: V.tensor_tensor(out=rs(ET), in0=lg[:, :, 4 + 4 * g: 8 + 4 * g], in1=bc4(ohv[:, :, g:g + 1]), op=ALU.mult)), [], [ET], [b_lg, b_OH])
                rop((lambda: V.tensor_tensor(out=rs(EIN), in0=rs(EIN), in1=rs(ET), op=ALU.add)), [EIN, ET], [EIN])
            rop((lambda: V.tensor_reduce(out=rs(M1, 1), in_=rs(EIN), axis=AX.X, op=ALU.max)), [EIN], [M1])
            rop((lambda: V.tensor_tensor(out=rs(MK1), in0=rs(EIN), in1=bc4(rs(M1, 1)), op=ALU.is_equal)), [EIN, M1], [MK1])
            rop((lambda: V.scalar_tensor_tensor(out=rs(E2), in0=rs(MK1), scalar=-1e30, in1=rs(EIN), op0=ALU.mult, op1=ALU.add)), [MK1, EIN], [E2])
            rop((lambda: V.tensor_reduce(out=rs(M2, 1), in_=rs(E2), axis=AX.X, op=ALU.max)), [E2], [M2])
            rop((lambda: V.tensor_tensor(out=rs(MK2), in0=rs(E2), in1=bc4(rs(M2, 1)), op=ALU.is_equal)), [E2, M2], [MK2])
            rop((lambda: V.tensor_tensor(out=rs(DL, 1), in0=rs(M2, 1), in1=rs(M1, 1), op=ALU.subtract)), [M1, M2], [DL])
            S.op("act", (lambda: A.activation(out=rs(DL, 1), in_=rs(DL, 1), func=AF.Exp)), [b_rsm[DL]], [b_rsm[DL]])
            rop((lambda: V.tensor_scalar(out=rs(W1, 1), in0=rs(DL, 1), scalar1=1.0, scalar2=None, op0=ALU.add)), [DL], [W1])
            rop((lambda: V.reciprocal(out=rs(W1, 1), in_=rs(W1, 1))), [W1], [W1])
            rop((lambda: V.tensor_tensor(out=rs(W2, 1), in0=rs(DL, 1), in1=rs(W1, 1), op=ALU.mult)), [DL, W1], [W2])
            rop((lambda: V.tensor_tensor(out=rs(W1, 1), in0=rs(W1, 1), in1=rs(GP, 1), op=ALU.mult)), [W1, GP], [W1])
            rop((lambda: V.tensor_tensor(out=rs(W2, 1), in0=rs(W2, 1), in1=rs(GP, 1), op=ALU.mult)), [W2, GP], [W2])
            rop((lambda: V.tensor_tensor(out=rs(GI), in0=rs(MK1), in1=bc4(rs(W1, 1)), op=ALU.mult)), [MK1, W1], [GI])
            rop((lambda: V.tensor_tensor(out=rs(GT), in0=rs(MK2), in1=bc4(rs(W2, 1)), op=ALU.mult)), [MK2, W2], [GT])
            rop((lambda: V.tensor_tensor(out=GI_all[:, T0:T0 + 8, :], in0=rs(GI), in1=rs(GT), op=ALU.add)), [GI, GT], [], (), [b_GI])

        def bcast_rows(gt0, b, Gd, bG):
            for k in range(8):
                S.op("dve", (lambda gt0=gt0, k=k, b=b: V.tensor_scalar(out=rdiag, in0=identf, scalar1=modT[:, gt0 + k, b:b + 1], scalar2=None, op0=ALU.mult)), [b_cst, b_modT], [b_rdiag])
                pb_, bpb_ = zbank()
                S.op("pe", (lambda pb_=pb_: T.matmul(pb_[:, 0:128], lhsT=onesf, rhs=rdiag, start=True, stop=True)), [b_cst, b_rdiag], [bpb_])
                S.op("act", (lambda pb_=pb_, Gd=Gd, k=k: A.copy(out=Gd[:, k * 128:(k + 1) * 128], in_=pb_[:, 0:128])), [bpb_], [bG])

        def bcast_rows_tab(tab, b_tab, b, Gd, bG):
            for k in range(8):
                S.op("dve", (lambda k=k, b=b: V.tensor_scalar(out=rdiag, in0=identf, scalar1=tab[:, k, b:b + 1], scalar2=None, op0=ALU.mult)), [b_cst, b_tab], [b_rdiag])
                pb_, bpb_ = zbank()
                S.op("pe", (lambda pb_=pb_: T.matmul(pb_[:, 0:128], lhsT=onesf, rhs=rdiag, start=True, stop=True)), [b_cst, b_rdiag], [bpb_])
                S.op("act", (lambda pb_=pb_, Gd=Gd, k=k: A.copy(out=Gd[:, k * 128:(k + 1) * 128], in_=pb_[:, 0:128])), [bpb_], [bG])

        def batch_prep(b):
            S.dma("sp", (lambda b=b: nc.sync.dma_start(out=G1b, in_=modrow_d[b, 2 * D:3 * D].partition_broadcast(128))), [b_modrow], b_G1b)
            S.dma("sp", (lambda b=b: nc.sync.dma_start(out=A2b, in_=a2row_d[b].partition_broadcast(128))), [b_modrow], b_A2b)
            S.dma("sp", (lambda b=b: nc.sync.dma_start(out=S2b, in_=modrow_d[b, 3 * D:4 * D].partition_broadcast(128))), [b_modrow], b_S2b)
            S.dma("pool", lambda: P.dma_start(out=wout, in_=wout_d.rearrange("(k p) n -> p k n", p=128)), [], b_wout)
            for k in range(8):
                S.op("dve", (lambda k=k: V.tensor_tensor(out=wout[:, k, :], in0=wout[:, k, :], in1=G1b, op=ALU.mult)), [b_wout, b_G1b], [b_wout])
            for k in range(8):
                S.op("dve", (lambda k=k, b=b: V.tensor_scalar(out=wrb[:, k, :], in0=wr[:, k, :], scalar1=A2T[:, k, b:b + 1], scalar2=None, op0=ALU.mult)), [b_wr, b_A2T], [b_wrb])
                S.op("dve", (lambda k=k, b=b: V.tensor_scalar(out=swr[:, k, :], in0=wr[:, k, :], scalar1=S2T[:, k, b:b + 1], scalar2=None, op0=ALU.mult)), [b_wr, b_modT], [b_swr])
            pb_, bpb_ = zbank()
            for k in range(8):
                S.op("pe", (lambda pb_=pb_, k=k: T.matmul(pb_[:, 0:20], lhsT=onesb, rhs=swr[:, k, :], start=(k == 0), stop=(k == 7))), [b_onesb, b_swr], [bpb_])
            S.op("dve", (lambda pb_=pb_: V.tensor_tensor(out=rbb, in0=pb_[:, 0:20], in1=rbias[:, :], op=ALU.add)), [bpb_, b_rbias], [b_rbb])

        def part1A(sc, t):
            b = sc // 2; half = sc % 2
            tok0 = sc * 1024 + t * 128
            col = half * 1024 + t * 128
            if t % 4 == 0:
                cntC["y"] += 1
                yi = cntC["y"] % 2
                S.dma("sp", (lambda b=b, col=col, yi=yi: nc.sync.dma_start(out=yc[yi], in_=ycat_d[b, :, :, col:col + 512])), b_ycats, b_yc[yi])
            yi = cntC["y"] % 2
            tc_ = (t % 4) * 128
            i = cntC["x"] % 2; cntC["x"] += 1
            S.dma("sp", (lambda i=i, tok0=tok0: nc.sync.dma_start(out=xtC[i], in_=x_d[tok0:tok0 + 128, :])), [], b_xtC[i])
            for hf in range(2):
                pb_, bpb_ = zbank()
                for k in range(8):
                    S.op("pe", (lambda pb_=pb_, k=k, hf=hf, yi=yi, tc_=tc_: T.matmul(pb_[:, :], lhsT=yc[yi][:, k, tc_:tc_ + 128], rhs=wout[:, k, hf * 512:(hf + 1) * 512], start=(k == 0), stop=(k == 7))), [b_yc[yi], b_wout], [bpb_])
                S.op("dve", (lambda pb_=pb_, hf=hf, i=i: V.tensor_tensor(out=xtC[i][:, hf * 512:(hf + 1) * 512], in0=pb_[:, :], in1=xtC[i][:, hf * 512:(hf + 1) * 512], op=ALU.add)), [bpb_, b_xtC[i]], [b_xtC[i]])
            S.dma("pool", (lambda i=i, tok0=tok0: P.dma_start(out=x1_d[tok0:tok0 + 128, :], in_=xtC[i])), [b_xtC[i]], b_x1ds[t % 4])
            return (i, tok0)

        def part1A2(sc, t, st1):
            i, tok0 = st1
            j = cntC["s"] % 4; cntC["s"] += 1
            xq = xnC[i]; bxq = b_xnC[i]
            S.op("act", (lambda i=i, j=j, xq=xq: A.activation(out=xq, in_=xtC[i], func=AF.Square, accum_out=ssC[:, j:j + 1])), [b_xtC[i]], [bxq, b_ssC[j]])
            S.op("act", (lambda j=j: A.activation(out=sdC[:, j:j + 1], in_=ssC[:, j:j + 1], func=AF.Sqrt, scale=1.0 / D, bias=epsc)), [b_ssC[j], b_cst], [b_sdC[j]])
            S.op("dve", (lambda j=j: V.reciprocal(out=sdC[:, j:j + 1], in_=sdC[:, j:j + 1])), [b_sdC[j]], [b_sdC[j]])
            S.op("act", (lambda i=i, j=j, xq=xq: A.activation(out=xq, in_=xtC[i], func=AF.Copy, scale=sdC[:, j:j + 1])), [b_xtC[i], b_sdC[j]], [bxq])
            rb_ = rowbuf[i]; brb_ = b_rowbuf[i]
            S.op("dve", (lambda xq=xq, rb_=rb_: V.tensor_tensor(out=rb_[:, 0:D], in0=xq, in1=A2b, op=ALU.mult)), [bxq, b_A2b], [brb_])
            S.op("pool", (lambda rb_=rb_: P.tensor_tensor(out=rb_[:, 0:D], in0=rb_[:, 0:D], in1=S2b, op=ALU.add)), [brb_, b_S2b], [brb_])
            S.dma("pool", (lambda rb_=rb_, tok0=tok0: P.dma_start(out=h2_d[tok0:tok0 + 128, :], in_=rb_[:, 0:D])), [brb_], b_h2d[t % 4])
            return (i, xq, bxq)

        def part1B(sc, t, st_):
            i, xq, bxq = st_
            for k in range(8):
                S.op("pe", (lambda k=k, xq=xq: T.transpose(out=pT[:, k, :], in_=xq[:, k * 128:(k + 1) * 128], identity=identb[:])), [bxq, b_identb], [b_pT])
            S.op("act", (lambda i=i: A.copy(out=xnT[i], in_=pT[:, :, :])), [b_pT], [b_xnT[i]])
            for k in range(8):
                S.op("pe", (lambda k=k, t=t, i=i: T.matmul(lgp[:, t, :], lhsT=xnT[i][:, k, :], rhs=wrb[:, k, :], start=(k == 0), stop=(k == 7))), [b_xnT[i], b_wrb], [b_lgp])

        NSC = 2 * NB
        for sc in range(NSC):
            if sc % 2 == 0:
                batch_prep(sc // 2)
            prev = None
            for t in range(8):
                st1 = part1A(sc, t)
                if prev is not None:
                    part1B(sc, t - 1, prev)
                prev = part1A2(sc, t, st1)
            part1B(sc, 7, prev)
            router_batch(sc)

        WITH = cf.take(64, 4); b_WITH = S.buf("WITH")
        TOT = cf.take(64, 4); b_TOT = S.buf("TOT")
        CUM = cf.take(64, 4); b_CUM = S.buf("CUM")
        ones64 = cf.take(64); b_ones64 = S.buf("ones64")
        tmpN = cf.take(NST); b_tmpN = S.buf("tmpN")
        NT = cf.take(4); b_NT = S.buf("NT")
        EE = cf.take(4); b_EE = S.buf("EE")
        BASE = cf.take(4); b_BASE = S.buf("BASE")
        OHf = OH_all.rearrange("p a b -> p (a b)")
        S.op("pe", lambda: T.matmul(pbank[1][:, 0:256], lhsT=lstf, rhs=OHf, start=True, stop=True), [b_cst, b_OH], [b_pb[1]])
        S.op("pe", lambda: T.matmul(pbank[2][:, 0:256], lhsT=onesf, rhs=OHf, start=True, stop=True), [b_cst, b_OH], [b_pb[2]])
        S.op("dve", lambda: V.tensor_copy(out=WITH.rearrange("p a b -> p (a b)"), in_=pbank[1][:, 0:256]), [b_pb[1]], [b_WITH])
        S.op("dve", lambda: V.tensor_copy(out=TOT.rearrange("p a b -> p (a b)"), in_=pbank[2][:, 0:256]), [b_pb[2]], [b_TOT])
        S.op("dve", lambda: V.memset(ones64, 1.0), [], [b_ones64])
        for g in range(4):
            S.op("dve", (lambda g=g: V.tensor_tensor_scan(out=CUM[:, :, g], data0=ones64, data1=TOT[:, :, g], initial=0.0, op0=ALU.mult, op1=ALU.add)), [b_ones64, b_TOT], [b_CUM])
        for g in range(4):
            S.op("dve", (lambda g=g: V.tensor_scalar(out=tmpN, in0=thrf, scalar1=CUM[:, 63, g:g + 1], scalar2=None, op0=ALU.is_lt)), [b_cst, b_CUM], [b_tmpN])
            S.op("dve", (lambda g=g: V.tensor_reduce(out=NT[:, g:g + 1], in_=tmpN, axis=AX.X, op=ALU.add)), [b_tmpN], [b_NT])
        S.op("dve", lambda: V.tensor_scalar(out=EE[:, 0:1], in0=NT[:, 0:1], scalar1=float(TSZ), scalar2=None, op0=ALU.mult), [b_NT], [b_EE])
        for g in range(1, 4):
            S.op("dve", (lambda g=g: V.scalar_tensor_tensor(out=EE[:, g:g + 1], in0=NT[:, g:g + 1], scalar=float(TSZ), in1=EE[:, g - 1:g], op0=ALU.mult, op1=ALU.add)), [b_NT, b_EE], [b_EE])
        S.op("dve", lambda: V.memset(BASE[:, 0:1], 0.0), [], [b_BASE])
        S.op("dve", lambda: V.tensor_copy(out=BASE[:, 1:4], in_=EE[:, 0:3]), [b_EE], [b_BASE])
        S.op("dve", lambda: V.tensor_tensor(out=CUM, in0=CUM, in1=TOT, op=ALU.subtract), [b_CUM, b_TOT], [b_CUM])
        S.op("dve", lambda: V.tensor_tensor(out=CUM, in0=CUM, in1=WITH, op=ALU.add), [b_CUM, b_WITH], [b_CUM])
        S.op("dve", lambda: V.tensor_tensor(out=CUM, in0=CUM, in1=BASE.unsqueeze(1).broadcast_to([128, 64, 4]), op=ALU.add), [b_CUM, b_BASE], [b_CUM])
        S.op("dve", lambda: V.tensor_tensor(out=CUM, in0=CUM, in1=OH_all, op=ALU.mult), [b_CUM, b_OH], [b_CUM])
        S.op("dve", lambda: V.tensor_reduce(out=POSf, in_=CUM, axis=AX.X, op=ALU.add), [b_CUM], [b_POSf])
        S.op("dve", lambda: V.tensor_copy(out=posi[:, :], in_=POSf), [b_POSf], [b_posi])
        S.op("dve", lambda: V.tensor_scalar(out=GIDf, in0=thrf, scalar1=EE[:, 0:1], scalar2=None, op0=ALU.is_ge), [b_cst, b_EE], [b_GIDf])
        for g in range(1, 3):
            S.op("dve", (lambda g=g: V.tensor_scalar(out=tmpN, in0=thrf, scalar1=EE[:, g:g + 1], scalar2=None, op0=ALU.is_ge)), [b_cst, b_EE], [b_tmpN])
            S.op("dve", lambda: V.tensor_tensor(out=GIDf, in0=GIDf, in1=tmpN, op=ALU.add), [b_GIDf, b_tmpN], [b_GIDf])

        for Tt in range(64):
            i = Tt % 2
            rb_ = rowbuf[i]; brb_ = b_rowbuf[i]
            S.dma("sp", (lambda rb_=rb_, Tt=Tt: nc.sync.dma_start(out=rb_[:, 0:D], in_=h2_d[Tt * 128:(Tt + 1) * 128, :])), b_h2d, brb_)
            S.op("dve", (lambda rb_=rb_, Tt=Tt: V.tensor_copy(out=rb_[:, D:D + 4], in_=GI_all[:, Tt, :])), [b_GI, brb_], [brb_])
            S.dma("pool", (lambda rb_=rb_, Tt=Tt: P.indirect_dma_start(out=h2s_d[:, :], out_offset=bass.IndirectOffsetOnAxis(ap=posi[:, Tt:Tt + 1], axis=0), in_=rb_, in_offset=None)), [brb_, b_posi, b_h2z], b_h2ss[i])

        S.barrier()

        cb = Carver(ARB, ARB_N); cf = Carver(ARF, ARF_N); cf.o = f_persist
        rows8 = cb.take(4, ROWW); b_rows8 = S.buf("rows8")
        h2sT = [cb.take(8, TSZ), cb.take(8, TSZ)]
        b_h2sT = [[S.buf("h2sT%d_%d" % (p, i)) for i in range(4)] for p in range(2)]
        acc = cb.take(4, D); b_acc = [S.buf("acc_%d" % i) for i in range(4)]
        w1s = [cb.take(8, 512) for _ in range(3)]; w3s = [cb.take(8, 512) for _ in range(3)]; w2s = [cb.take(4, D) for _ in range(3)]
        b_w1s = [S.buf("w1s%d" % u) for u in range(3)]; b_w3s = [S.buf("w3s%d" % u) for u in range(3)]; b_w2s = [S.buf("w2s%d" % u) for u in range(3)]
        heT2 = [cb.take(4, 512), cb.take(4, 512)]; b_heT2 = [S.buf("heT0"), S.buf("heT1")]
        sil = [cb.take(512), cb.take(512)]; b_sil = [S.buf("sil0"), S.buf("sil1")]
        w1rows = w1b_d.rearrange("e p k n -> (e p) (k n)")
        w3rows = w3b_d.rearrange("e p k n -> (e p) (k n)")
        w2rows = w2b_d.rearrange("e p k n -> (e p) (k n)")

        def load_w(s_, i_):
            wi = (s_ * 4 + i_) % 3
            S.op("dve", (lambda s_=s_, i_=i_, wi=wi: V.tensor_scalar(out=ef[:, wi:wi + 1], in0=GIDf[:, s_:s_ + 1], scalar1=512.0, scalar2=float(i_ * 128), op0=ALU.mult, op1=ALU.add)), [b_GIDf], [b_ef[wi]])
            S.op("dve", (lambda wi=wi: V.tensor_tensor(out=ef[:, wi:wi + 1], in0=ef[:, wi:wi + 1], in1=iotap, op=ALU.add)), [b_ef[wi], b_cst], [b_ef[wi]])
            S.op("dve", (lambda wi=wi: V.tensor_copy(out=widx[:, wi:wi + 1], in_=ef[:, wi:wi + 1])), [b_ef[wi]], [b_widx[wi]])
            S.dma("pool", (lambda wi=wi: P.indirect_dma_start(out=w1s[wi].rearrange("p k n -> p (k n)"), out_offset=None, in_=w1rows, in_offset=bass.IndirectOffsetOnAxis(ap=widx[:, wi:wi + 1], axis=0))), [b_widx[wi], b_wb], b_w1s[wi])
            S.dma("pool", (lambda wi=wi: P.indirect_dma_start(out=w3s[wi].rearrange("p k n -> p (k n)"), out_offset=None, in_=w3rows, in_offset=bass.IndirectOffsetOnAxis(ap=widx[:, wi:wi + 1], axis=0))), [b_widx[wi], b_wb], b_w3s[wi])
            S.dma("pool", (lambda wi=wi: P.indirect_dma_start(out=w2s[wi].rearrange("p k n -> p (k n)"), out_offset=None, in_=w2rows, in_offset=bass.IndirectOffsetOnAxis(ap=widx[:, wi:wi + 1], axis=0))), [b_widx[wi], b_wb], b_w2s[wi])

        def load_rows(s_):
            pp = s_ % 2
            S.dma("sp", (lambda s_=s_: nc.sync.dma_start(out=rows8, in_=h2s_d[s_ * TSZ:(s_ + 1) * TSZ, :].rearrange("(a p) c -> p a c", p=128))), b_h2ss, b_rows8)
            S.op("dve", (lambda pp=pp: V.tensor_copy(out=gsl2[pp][:, 0:4, :], in_=rows8[:, :, D:D + 4])), [b_rows8], [b_gsl2[pp]])
            for sub in range(4):
                for k in range(8):
                    S.op("pe", (lambda sub=sub, k=k: T.transpose(out=pT[:, k, :], in_=rows8[:, sub, k * 128:(k + 1) * 128], identity=identb[:])), [b_rows8, b_identb], [b_pT])
                if sub % 2 == 0:
                    S.op("act", (lambda sub=sub, pp=pp: A.copy(out=h2sT[pp][:, :, sub * 128:(sub + 1) * 128], in_=pT[:, :, :])), [b_pT], [b_h2sT[pp][sub]])
                else:
                    S.op("dve", (lambda sub=sub, pp=pp: V.tensor_copy(out=h2sT[pp][:, :, sub * 128:(sub + 1) * 128], in_=pT[:, :, :])), [b_pT], [b_h2sT[pp][sub]])

        def h_part(n, dds):
            s_, i_ = divmod(n, 4)
            pp = s_ % 2
            wi = n % 3
            hh = n % 2
            for dd in dds:
                p1, bp1 = zbank()
                p3, bp3 = zbank()
                for k in range(8):
                    S.op("pe", (lambda p1=p1, k=k, dd=dd, wi=wi, pp=pp: T.matmul(p1[:, :], lhsT=w1s[wi][:, k, dd * 128:(dd + 1) * 128], rhs=h2sT[pp][:, k, 0:512], start=(k == 0), stop=(k == 7))), [b_w1s[wi]] + b_h2sT[pp], [bp1])
                for k in range(8):
                    S.op("pe", (lambda p3=p3, k=k, dd=dd, wi=wi, pp=pp: T.matmul(p3[:, :], lhsT=w3s[wi][:, k, dd * 128:(dd + 1) * 128], rhs=h2sT[pp][:, k, 0:512], start=(k == 0), stop=(k == 7))), [b_w3s[wi]] + b_h2sT[pp], [bp3])
                si = dd % 2
                S.op("act", (lambda p1=p1, si=si: A.activation(out=sil[si], in_=p1[:, :], func=AF.Silu)), [bp1], [b_sil[si]])
                S.op("dve", (lambda p3=p3, si=si, dd=dd, hh=hh: V.tensor_tensor(out=heT2[hh][:, dd, :], in0=sil[si], in1=p3[:, :], op=ALU.mult)), [b_sil[si], bp3], [b_heT2[hh]])

        def out_part(n):
            s_, i_ = divmod(n, 4)
            pp = s_ % 2
            wi = n % 3
            hh = n % 2
            for tt_ in range(4):
                t = tt_
                for hf in range(2):
                    po, bpo = zbank()
                    for dd in range(4):
                        S.op("pe", (lambda po=po, dd=dd, tt_=tt_, hf=hf, wi=wi, hh=hh: T.matmul(po[:, :], lhsT=heT2[hh][:, dd, tt_ * 128:(tt_ + 1) * 128], rhs=w2s[wi][:, dd, hf * 512:(hf + 1) * 512], start=(dd == 0), stop=(dd == 3))), [b_heT2[hh], b_w2s[wi]], [bpo])
                    if i_ == 0:
                        S.op("dve", (lambda po=po, t=t, hf=hf, i_=i_, pp=pp: V.tensor_scalar(out=acc[:, t, hf * 512:(hf + 1) * 512], in0=po[:, :], scalar1=gsl2[pp][:, t, i_:i_ + 1], scalar2=None, op0=ALU.mult)), [bpo, b_gsl2[pp]], [b_acc[t]])
                    else:
                        S.op("dve", (lambda po=po, t=t, hf=hf, i_=i_, pp=pp: V.scalar_tensor_tensor(out=acc[:, t, hf * 512:(hf + 1) * 512], in0=po[:, :], scalar=gsl2[pp][:, t, i_:i_ + 1], in1=acc[:, t, hf * 512:(hf + 1) * 512], op0=ALU.mult, op1=ALU.add)), [bpo, b_gsl2[pp], b_acc[t]], [b_acc[t]])

        NEXP = NST * 4
        load_w(0, 0)
        load_w(0, 1)
        load_rows(0)
        h_part(0, range(4))
        for n in range(NEXP):
            s_, i_ = divmod(n, 4)
            if n + 1 < NEXP:
                h_part(n + 1, [0])
            out_part(n)
            if n + 2 < NEXP:
                load_w((n + 2) // 4, (n + 2) % 4)
            if n + 1 < NEXP:
                h_part(n + 1, [1, 2, 3])
            if i_ == 1 and s_ + 1 < NST:
                load_rows(s_ + 1)
            if i_ == 3:
                S.dma("sp", (lambda s_=s_: nc.sync.dma_start(out=moe_d[s_ * TSZ:(s_ + 1) * TSZ, :].rearrange("(a p) c -> p a c", p=128), in_=acc)), b_acc, b_moe)

        S.barrier()

        cb = Carver(ARB, ARB_N); cf = Carver(ARF, ARF_N); cf.o = f_persist
        mrow = [cb.take(D), cb.take(D), cb.take(D)]; b_mrow = [S.buf("mrow0"), S.buf("mrow1"), S.buf("mrow2")]
        x3t = [cf.take(D), cf.take(D), cf.take(D)]; b_x3t = [S.buf("x3t0"), S.buf("x3t1"), S.buf("x3t2")]
        t1s = [cf.take(D), cf.take(D)]; b_t1s = [S.buf("t1_0"), S.buf("t1_1")]
        G2b = cf.take(D); b_G2b = S.buf("G2b")
        FGb = cf.take(D); b_FGb = S.buf("FGb")
        S.dma("sp", lambda: nc.sync.dma_start(out=FGb, in_=fg_d.partition_broadcast(128)), [], b_FGb)
        def c5_fetch(Tt):
            i = Tt % 3
            tok0 = Tt * 128
            S.dma("pool", (lambda i=i, Tt=Tt: P.indirect_dma_start(out=mrow[i], out_offset=None, in_=moe_d[:, :], in_offset=bass.IndirectOffsetOnAxis(ap=posi[:, Tt:Tt + 1], axis=0))), [b_moe, b_posi], b_mrow[i])
            S.dma("sp", (lambda i=i, tok0=tok0: nc.sync.dma_start(out=x3t[i], in_=x1_d[tok0:tok0 + 128, :])), b_x1ds, b_x3t[i])

        G2bs = [G2b, t1s[0]]
        c5_fetch(0)
        for Tt in range(64):
            b = Tt // 16
            if Tt % 16 == 0:
                S.dma("sp", (lambda b=b: nc.sync.dma_start(out=G2b, in_=modrow_d[b, 5 * D:6 * D].partition_broadcast(128))), [b_modrow], b_G2b)
            if Tt + 1 < 64:
                c5_fetch(Tt + 1)
            tok0 = Tt * 128
            i = Tt % 3
            t1 = t1s[Tt % 2]; b_t1 = b_t1s[Tt % 2]
            S.op("dve", (lambda i=i, t1=t1: V.tensor_tensor(out=t1, in0=mrow[i], in1=G2b, op=ALU.mult)), [b_mrow[i], b_G2b], [b_t1])
            S.op("dve", (lambda i=i, t1=t1: V.tensor_tensor(out=x3t[i], in0=t1, in1=x3t[i], op=ALU.add)), [b_t1, b_x3t[i]], [b_x3t[i]])
            j = cntC["s"] % 4; cntC["s"] += 1
            S.op("act", (lambda i=i, j=j, t1=t1: A.activation(out=t1, in_=x3t[i], func=AF.Square, accum_out=ssC[:, j:j + 1])), [b_x3t[i]], [b_t1, b_ssC[j]])
            S.op("act", (lambda j=j: A.activation(out=sdC[:, j:j + 1], in_=ssC[:, j:j + 1], func=AF.Sqrt, scale=1.0 / D, bias=epsc)), [b_ssC[j], b_cst], [b_sdC[j]])
            S.op("dve", (lambda j=j: V.reciprocal(out=sdC[:, j:j + 1], in_=sdC[:, j:j + 1])), [b_sdC[j]], [b_sdC[j]])
            S.op("act", (lambda i=i, j=j, t1=t1: A.activation(out=t1, in_=x3t[i], func=AF.Copy, scale=sdC[:, j:j + 1])), [b_x3t[i], b_sdC[j]], [b_t1])
            S.op("dve", (lambda i=i, t1=t1: V.tensor_tensor(out=x3t[i], in0=t1, in1=FGb, op=ALU.mult)), [b_t1, b_FGb], [b_x3t[i]])
            S.dma("sp", (lambda i=i, tok0=tok0: nc.sync.dma_start(out=out_d[tok0:tok0 + 128, :], in_=x3t[i])), [b_x3t[i]], b_outs[Tt % 4])

        S.final_waits.extend(b_outs)
        S.emit()
    return nc


_NC_CACHE = {}


def kernel(x, c, ctx, c_ctx, w_mod, b_mod, norm1_g, norm2_g, w_in, s5_lambda_re, s5_lambda_im,
           s5_log_dt, s5_b_re, s5_b_im, s5_c_re, s5_c_im, s5_d, w_glu, b_glu, conv_w, w_out,
           router_group_w, router_group_b, router_expert_w, router_expert_b,
           expert_w1, expert_w3, expert_w2, final_g):
    f = lambda a: np.ascontiguousarray(np.asarray(a, dtype=np.float32))
    if "nc" not in _NC_CACHE:
        _NC_CACHE["nc"] = build()
    nc = _NC_CACHE["nc"]
    x = f(x); ctx = f(ctx); c = f(c); c_ctx = f(c_ctx)
    shared = {
        "w_mod": f(w_mod[0]), "b_mod": f(b_mod[0]), "norm1_g": f(norm1_g[0]), "norm2_g": f(norm2_g[0]),
        "w_in": f(w_in[0]), "lam_re": f(s5_lambda_re[0]), "lam_im": f(s5_lambda_im[0]),
        "log_dt": f(s5_log_dt[0]).reshape(32), "b_re": f(s5_b_re[0]), "b_im": f(s5_b_im[0]),
        "c_re": f(s5_c_re[0]).reshape(512, 64), "c_im": f(s5_c_im[0]).reshape(512, 64),
        "s5_d": f(s5_d[0]), "w_glu": f(w_glu[0]), "b_glu": f(b_glu[0]), "conv_w": f(conv_w[0]),
        "w_out": f(w_out[0]), "rgw": f(router_group_w[0]), "rgb": f(router_group_b[0]),
        "rew": f(router_expert_w[0]), "reb": f(router_expert_b[0]),
        "w1": f(expert_w1[0]), "w3": f(expert_w3[0]), "w2": f(expert_w2[0]),
        "final_g": f(final_g), "consts": CONSTS, "gsel": GSEL,
    }
    in_maps = []
    for i in range(8):
        m = dict(shared)
        m["x"] = x[i * NB:(i + 1) * NB].reshape(NB * SEQ, D)
        m["ctx"] = ctx[i * NB:(i + 1) * NB].reshape(NB * NCTX, D)
        m["cond"] = np.concatenate([c[i * NB:(i + 1) * NB], c_ctx[None, :]], axis=0)
        in_maps.append(m)
    res = run_bass_kernel_spmd(nc, in_maps, core_ids=list(range(8)))
    outs = [np.asarray(r["out"]).reshape(NB, SEQ, D) for r in res.results]
    return np.concatenate(outs, axis=0).astype(np.float32)
```

```python
import contextlib
import math
import numpy as np
import concourse.bass as bass
import concourse.mybir as mybir
from concourse.bass_utils import run_bass_kernel_spmd

F32 = mybir.dt.float32
BF16 = mybir.dt.bfloat16
AF = mybir.ActivationFunctionType
ALU = mybir.AluOpType
AX = mybir.AxisListType
ENGS = ("pe", "act", "dve", "pool", "sp")
NB = 4
SEQ = 2048
NCTX = 256
D = 1024
LTOT = SEQ + NCTX
NLEV = 9
NBLK = 288
NX = 320
TAIL = 47104
TSZ = 512
NST = 19
NSLOT = NST * TSZ
ROWW = 1040
I32 = mybir.dt.int32
ARB_N = 65536
ARF_N = 9472


class Buf:
    __slots__ = ("name", "writers", "readers", "sem", "dma_total", "_par", "_epoch")

    def __init__(self, name):
        self.name = name
        self.writers = []
        self.readers = []
        self.sem = None
        self.dma_total = 0
        self._par = False
        self._epoch = []


class Op:
    __slots__ = ("eng", "fn", "deps", "needs_inc", "count", "dma_buf", "dma_count")

    def __init__(self, eng, fn):
        self.eng = eng
        self.fn = fn
        self.deps = []
        self.needs_inc = False
        self.count = None
        self.dma_buf = None
        self.dma_count = None


class Sched:
    def __init__(self, nc, stack):
        self.nc = nc
        self.stack = stack
        self.ops = {e: [] for e in ENGS}
        self.bufs = []
        self.final_waits = []

    def buf(self, name):
        b = Buf(name)
        self.bufs.append(b)
        return b

    def _add(self, eng, fn, reads, writes, dma_buf=None, par=False):
        op = Op(eng, fn)
        seen = set()
        for b in reads:
            for d in b.writers:
                if id(d) not in seen:
                    seen.add(id(d)); op.deps.append(d)
        for b in writes:
            if par:
                if b.readers or not getattr(b, "_par", False):
                    b._epoch = b.writers + b.readers
                    b.writers = []
                    b.readers = []
                    b._par = True
                dl = b._epoch
            else:
                dl = b.writers + b.readers
                b._par = False
            for d in dl:
                if id(d) not in seen:
                    seen.add(id(d)); op.deps.append(d)
        for b in reads:
            b.readers.append(op)
        for b in writes:
            if par:
                b.writers.append(op)
            else:
                b.writers = [op]
                b.readers = []
        if dma_buf is not None:
            op.dma_buf = dma_buf
            if dma_buf.sem is None:
                dma_buf.sem = self.stack.enter_context(self.nc.semaphore("s_" + dma_buf.name))
            dma_buf.dma_total += 16
            op.dma_count = dma_buf.dma_total
        self.ops[eng].append(op)
        return op

    def op(self, eng, fn, reads=(), writes=()):
        return self._add(eng, fn, list(reads), list(writes))

    def dma(self, eng, fn, reads, write, par=False):
        return self._add(eng, fn, list(reads), [write], dma_buf=write, par=par)

    def barrier(self):
        nc = self.nc
        pend = []
        seen = set()
        for b in self.bufs:
            if b.name.endswith("_d") or "_d" in b.name and b.name.split("_d")[-1].isdigit() or b.name in ("wb_scr", "h2s_zero"):
                continue
            for d in b.writers + b.readers:
                if id(d) not in seen:
                    seen.add(id(d)); pend.append(d)
        engobj = {"pe": nc.tensor, "act": nc.scalar, "dve": nc.vector, "pool": nc.gpsimd, "sp": nc.sync}
        for e in ENGS:
            op = Op(e, (lambda e=e: engobj[e].nop()))
            op.deps = list(pend)
            self.ops[e].append(op)

    def emit(self):
        nc = self.nc
        for e in ENGS:
            for op in self.ops[e]:
                for d in op.deps:
                    if d.dma_buf is None and not (d.eng == "pe" and op.eng == "pe" and op.dma_buf is None):
                        d.needs_inc = True
        sems = {e: self.stack.enter_context(nc.semaphore("eng_" + e)) for e in ENGS}
        for e in ENGS:
            c = 0
            for op in self.ops[e]:
                if op.dma_buf is None and op.needs_inc:
                    c += 1
                    op.count = c
        final_waits = self.final_waits

        def run(e, eng):
            waited = {}
            for op in self.ops[e]:
                need = {}
                for d in op.deps:
                    if d.dma_buf is not None:
                        key = ("d", id(d.dma_buf)); v = d.dma_count; sem = d.dma_buf.sem
                    else:
                        if d.eng == "pe" and e == "pe" and op.dma_buf is None:
                            continue
                        key = ("e", d.eng); v = d.count; sem = sems[d.eng]
                    if key not in need or need[key][1] < v:
                        need[key] = (sem, v)
                for key, (sem, v) in need.items():
                    if waited.get(key, 0) >= v:
                        continue
                    waited[key] = v
                    eng.wait_ge(sem, v)
                ins = op.fn()
                if op.dma_buf is not None:
                    ins.then_inc(op.dma_buf.sem, 16)
                elif op.needs_inc:
                    ins.then_inc(sems[e], 1)
            if e == "sp":
                for b in final_waits:
                    eng.wait_ge(b.sem, b.dma_total)

        with nc.Block() as block:
            @block.tensor
            def _(eng):
                run("pe", eng)

            @block.scalar
            def _(eng):
                run("act", eng)

            @block.vector
            def _(eng):
                run("dve", eng)

            @block.gpsimd
            def _(eng):
                run("pool", eng)

            @block.sync
            def _(eng):
                run("sp", eng)


def make_consts():
    c = {}
    c["ident"] = np.eye(128, dtype=np.float32)
    isw = np.zeros((128, 128), np.float32)
    for p in range(128):
        isw[p, (p + 64) % 128] = 1.0
    c["iswap"] = isw
    c["ones"] = np.ones((128, 128), np.float32)
    sg = np.ones((128, 1), np.float32); sg[64:] = -1.0
    c["sgn"] = sg
    rm = np.zeros((128, 8), np.float32)
    for p in range(128):
        rm[p, p // 16] = 1.0
    c["rowmask"] = rm
    s8i = np.arange(128)[:, None] // 16
    t8i = np.arange(128)[None, :] // 16
    c["maskf"] = (s8i <= t8i).astype(np.float32)
    c["maskb"] = (s8i >= t8i).astype(np.float32)
    c["eps"] = np.full((128, 1), 1e-6, np.float32)
    c["lst"] = (np.arange(128)[:, None] < np.arange(128)[None, :]).astype(np.float32)
    c["thr"] = np.tile((np.arange(NST, dtype=np.float32) * float(TSZ))[None, :], (128, 1))
    c["iotap"] = np.arange(128, dtype=np.float32)[:, None]
    order = ["ident", "iswap", "ones", "sgn", "rowmask", "maskf", "maskb", "eps", "lst", "thr", "iotap"]
    offs = {}
    o = 0
    for k in order:
        offs[k] = (o, c[k].shape[1]); o += c[k].shape[1]
    return np.concatenate([c[k] for k in order], axis=1), offs


CONSTS, COFF = make_consts()


def make_gsel():
    g = np.zeros((128, 8, 240), np.float32)
    for g8 in range(8):
        for h in range(16):
            g[16 * g8 + h, g8, 112 + h] = 1.0
    return g.reshape(128, 8 * 240)


GSEL = make_gsel()


def build():
    nc = bass.Bass("TRN2", target_bir_lowering=False, dynamic_dma_scratch_size=8192)

    def din(name, shape):
        return nc.dram_tensor(name, list(shape), F32, kind="ExternalInput").ap()

    x_d = din("x", [NB * SEQ, D]); ctx_d = din("ctx", [NB * NCTX, D]); cond_d = din("cond", [NB + 1, D])
    wmod_d = din("w_mod", [D, 6 * D]); bmod_d = din("b_mod", [6 * D])
    n1g_d = din("norm1_g", [D]); n2g_d = din("norm2_g", [D]); win_d = din("w_in", [D, 2560])
    lre_d = din("lam_re", [2, 16, 64]); lim_d = din("lam_im", [2, 16, 64]); ldt_d = din("log_dt", [32])
    bre_d = din("b_re", [2, 16, 64, 16]); bim_d = din("b_im", [2, 16, 64, 16])
    cre_d = din("c_re", [512, 64]); cim_d = din("c_im", [512, 64])
    s5d_d = din("s5_d", [256]); wglu_d = din("w_glu", [256, 256]); bglu_d = din("b_glu", [256])
    cw_d = din("conv_w", [3, 768]); wout_d = din("w_out", [D, D])
    rgw_d = din("rgw", [D, 4]); rgb_d = din("rgb", [4]); rew_d = din("rew", [D, 16]); reb_d = din("reb", [16])
    w1_d = din("w1", [16, D, 512]); w3_d = din("w3", [16, D, 512]); w2_d = din("w2", [16, 512, D])
    fg_d = din("final_g", [D]); consts_d = din("consts", list(CONSTS.shape)); gsel_d = din("gsel", [128, 1920])
    out_d = nc.dram_tensor("out", [NB * SEQ, D], F32, kind="ExternalOutput").ap()
    ycat_d = nc.dram_tensor("ycat_scr", [NB, 128, 8, SEQ], BF16, kind="Internal").ap()
    x1_d = nc.dram_tensor("x1_scr", [NB * SEQ, D], F32, kind="Internal").ap()
    modrow_d = nc.dram_tensor("modrow_scr", [NB + 1, 6 * D], F32, kind="Internal").ap()
    a2row_d = nc.dram_tensor("a2row_scr", [NB + 1, D], F32, kind="Internal").ap()
    rot_d = nc.dram_tensor("rot_scr", [16, 128, 2 * NLEV, 128], BF16, kind="Internal").ap()
    w1b_d = nc.dram_tensor("w1b_scr", [16, 128, 8, 512], BF16, kind="Internal").ap()
    w3b_d = nc.dram_tensor("w3b_scr", [16, 128, 8, 512], BF16, kind="Internal").ap()
    w2b_d = nc.dram_tensor("w2b_scr", [16, 128, 4, D], BF16, kind="Internal").ap()
    h2_d = nc.dram_tensor("h2_scr", [NB * SEQ, D], BF16, kind="Internal").ap()
    h2s_d = nc.dram_tensor("h2s_scr", [NSLOT, ROWW], BF16, kind="Internal").ap()
    moe_d = nc.dram_tensor("moe_scr", [NSLOT, D], BF16, kind="Internal").ap()

    with contextlib.ExitStack() as st:
        S = Sched(nc, st)

        def sb(name, shape, dt=F32):
            return st.enter_context(nc.sbuf_tensor(name, list(shape), dt))

        def ps(name, shape, dt=F32):
            return st.enter_context(nc.psum_tensor(name, list(shape), dt))

        V, A, P, T = nc.vector, nc.scalar, nc.gpsimd, nc.tensor
        NCQ = dict(allow_slow_non_contiguous=True)

        cst = sb("cst", list(CONSTS.shape)); b_cst = S.buf("cst")
        identf = cst[:, COFF["ident"][0]:COFF["ident"][0] + 128]
        iswapf = cst[:, COFF["iswap"][0]:COFF["iswap"][0] + 128]
        onesf = cst[:, COFF["ones"][0]:COFF["ones"][0] + 128]
        sgn = cst[:, COFF["sgn"][0]:COFF["sgn"][0] + 1]
        rowmask = cst[:, COFF["rowmask"][0]:COFF["rowmask"][0] + 8]
        maskf = cst[:, COFF["maskf"][0]:COFF["maskf"][0] + 128]
        maskb = cst[:, COFF["maskb"][0]:COFF["maskb"][0] + 128]
        epsc = cst[:, COFF["eps"][0]:COFF["eps"][0] + 1]
        lstf = cst[:, COFF["lst"][0]:COFF["lst"][0] + 128]
        thrf = cst[:, COFF["thr"][0]:COFF["thr"][0] + NST]
        iotap = cst[:, COFF["iotap"][0]:COFF["iotap"][0] + 1]
        posi = sb("posi", [128, 64], I32); b_posi = S.buf("posi")
        widx = sb("widx", [128, 2], I32); b_widx = [S.buf("widx0"), S.buf("widx1")]
        Gsel = sb("Gsel", [128, 8, 240], BF16); b_Gsel = S.buf("Gsel")
        identb = sb("identb", [128, 128], BF16); b_identb = S.buf("identb")
        modT = sb("modT", [128, 48, 8]); b_modT = S.buf("modT")
        A1T = sb("A1T", [128, 8, 8]); b_A1T = S.buf("A1T")
        A2T = sb("A2T", [128, 8, 8]); b_A2T = S.buf("A2T")
        gT12 = sb("gT12", [128, 2, 8]); b_gT12 = S.buf("gT12")
        rbias = sb("rbias", [128, 20]); b_rbias = S.buf("rbias")
        wr = sb("wr", [128, 8, 20], BF16); b_wr = S.buf("wr")
        cw = sb("cw", [128, 6, 3]); b_cw = S.buf("cw")
        dcol = sb("dcol", [128, 2]); b_dcol = S.buf("dcol")
        bglu = sb("bglu", [128, 2]); b_bglu = S.buf("bglu")
        wglu = sb("wglu", [128, 2, 256], BF16); b_wglu = S.buf("wglu")
        c1t = sb("c1t", [128, 32, NLEV]); b_c1t = S.buf("c1t")
        c2t = sb("c2t", [128, 32, NLEV]); b_c2t = S.buf("c2t")
        ss = sb("ss", [128, 4]); rstd = sb("rstd", [128, 4])
        b_ss = [S.buf("ss%d" % i) for i in range(4)]; b_rstd = [S.buf("rstd%d" % i) for i in range(4)]

        ARB = sb("arb", [128, ARB_N], BF16)
        ARF = sb("arf", [128, ARF_N], F32)

        class Carver:
            def __init__(self, t, n):
                self.t = t; self.n = n; self.o = 0

            def take(self, *shape):
                n = int(np.prod(shape))
                assert self.o + n <= self.n, ("arena overflow", self.o + n, self.n)
                ap = self.t[:, self.o:self.o + n]
                self.o += n
                if len(shape) == 2:
                    ap = ap.rearrange("p (a b) -> p a b", a=shape[0])
                elif len(shape) == 3:
                    ap = ap.rearrange("p (a b c) -> p a b c", a=shape[0], b=shape[1])
                return ap

        pbank = [ps("pb%d" % i, [128, 512]) for i in range(7)]
        b_pb = [S.buf("pb%d" % i) for i in range(7)]
        pT = ps("pT", [128, 8, 128], BF16); b_pT = S.buf("pT")

        S.dma("sp", lambda: nc.sync.dma_start(out=cst[:], in_=consts_d), [], b_cst)
        S.dma("pool", lambda: P.dma_start(out=identb[:], in_=consts_d[:, 0:128]), [], b_identb)
        S.dma("pool", lambda: P.dma_start(out=Gsel[:], in_=gsel_d.rearrange("p (a b) -> p a b", a=8)), [], b_Gsel)
        S.dma("sp", lambda: nc.sync.dma_start(out=rbias[:, 0:4], in_=rgb_d.partition_broadcast(128)), [], b_rbias)
        S.dma("sp", lambda: nc.sync.dma_start(out=rbias[:, 4:20], in_=reb_d.partition_broadcast(128)), [], b_rbias)
        S.dma("pool", lambda: P.dma_start(out=wr[:, :, 0:4], in_=rgw_d.rearrange("(k p) n -> p k n", p=128)), [], b_wr)
        S.dma("pool", lambda: P.dma_start(out=wr[:, :, 4:20], in_=rew_d.rearrange("(k p) n -> p k n", p=128)), [], b_wr)
        for tp in range(3):
            S.dma("sp", (lambda tp=tp: nc.sync.dma_start(out=cw[:, :, tp], in_=cw_d[tp].rearrange("(i p) -> p i", p=128), **NCQ)), [], b_cw)
        S.dma("sp", lambda: nc.sync.dma_start(out=dcol[:], in_=s5d_d.rearrange("(f p) -> p f", p=128), **NCQ), [], b_dcol)
        S.dma("sp", lambda: nc.sync.dma_start(out=bglu[:], in_=bglu_d.rearrange("(f p) -> p f", p=128), **NCQ), [], b_bglu)
        S.dma("pool", lambda: P.dma_start(out=wglu[:], in_=wglu_d.rearrange("(k p) n -> p k n", p=128)), [], b_wglu)
        S.dma("sp", lambda: nc.sync.dma_start(out=gT12[:, 0, :], in_=n1g_d.rearrange("(k p) -> p k", p=128), **NCQ), [], b_gT12)
        S.dma("sp", lambda: nc.sync.dma_start(out=gT12[:, 1, :], in_=n2g_d.rearrange("(k p) -> p k", p=128), **NCQ), [], b_gT12)

        cb = Carver(ARB, ARB_N); cf = Carver(ARF, ARF_N)
        win = cb.take(8, 2560); b_win = S.buf("win")
        S.dma("pool", lambda: P.dma_start(out=win[:, :, 0:1280], in_=win_d[:, 0:1280].rearrange("(k p) n -> p k n", p=128)), [], b_win, par=True)
        S.dma("pool", lambda: P.dma_start(out=win[:, :, 1280:2560], in_=win_d[:, 1280:2560].rearrange("(k p) n -> p k n", p=128)), [], b_win, par=True)
        wm = [cb.take(8, 512).rearrange("p k n -> p (k n)").bitcast(F32).rearrange("p (k n) -> p k n", k=8), cb.take(8, 512).rearrange("p k n -> p (k n)").bitcast(F32).rearrange("p (k n) -> p k n", k=8)]
        b_wm = [S.buf("wm0"), S.buf("wm1")]
        LFa = cb.take(16, 8, 16); LFb = cb.take(16, 8, 16); b_LFa = S.buf("LFa"); b_LFb = S.buf("LFb")
        LF = cb.take(32, 128); b_LF = S.buf("LF")
        LF4 = LF.rearrange("p a (b c) -> p a b c", b=8)
        RCa = cb.take(16, 16, 16); RCb = cb.take(16, 16, 16); b_RCa = S.buf("RCa"); b_RCb = S.buf("RCb")
        assert cb.o <= TAIL
        cb.o = TAIL
        Wt = cb.take(32, 128); b_Wt = S.buf("Wt")
        RC = cb.take(32, 256); b_RC = S.buf("RC")
        RC4 = RC.rearrange("p a (b c) -> p a b c", b=16)
        M0 = cb.take(16, 128); b_M0 = S.buf("M0")
        condT = cf.take(8, 8); b_condT = S.buf("condT")
        scT = cf.take(8, 8); b_scT = S.buf("scT")
        bmodT = cf.take(48); b_bmodT = S.buf("bmodT")
        NSM = 30
        sm = cf.take(NSM, 32); b_sm = [S.buf("sm%d" % i) for i in range(NSM)]
        Bm1 = cf.take(32, 16); Bm2 = cf.take(32, 16); b_Bm1 = S.buf("Bm1"); b_Bm2 = S.buf("Bm2")
        CIN = cf.take(4, 128); b_CIN = S.buf("CIN")
        CIN2 = cf.take(4, 128); b_CIN2 = S.buf("CIN2")
        Cm1 = cf.take(512); b_Cm1 = S.buf("Cm1")
        Cm2 = cf.take(512); b_Cm2 = S.buf("Cm2")
        Pre = cf.take(32, 16); Pim = cf.take(32, 16); b_P = [S.buf("P%d" % i) for i in range(16)]
        PEre = cf.take(32, 8); PEim = cf.take(32, 8); b_PE = S.buf("PE")
        PQre = cf.take(32, 8); PQim = cf.take(32, 8); PQt = cf.take(32, 8)
        b_PQ = S.buf("PQ"); b_PQi = S.buf("PQi"); b_PQt = S.buf("PQt")
        PRre = cf.take(32, 16); PRim = cf.take(32, 16); b_PR = S.buf("PR")
        Mt1 = cf.take(4, 128); Mt2 = cf.take(4, 128); b_Mt1 = S.buf("Mt1"); b_Mt2 = S.buf("Mt2")

        (LRE, LIM, DT, XR, XI, MAG, SN, CS, ARE, AIM, T1, T2, T3, NR, D2, RD, QRE, QIM, QB, T4) = range(20)

        def smv(i):
            return sm[:, i, :]

        for half in range(2):
            S.dma("sp", (lambda half=half: nc.sync.dma_start(out=sm[half * 64:(half + 1) * 64, LRE, :], in_=lre_d.rearrange("d g p -> p (d g)"), **NCQ)), [], b_sm[LRE])
            S.dma("sp", (lambda half=half: nc.sync.dma_start(out=sm[half * 64:(half + 1) * 64, LIM, :], in_=lim_d.rearrange("d g p -> p (d g)"), **NCQ)), [], b_sm[LIM])
        S.dma("sp", lambda: nc.sync.dma_start(out=smv(DT), in_=ldt_d.partition_broadcast(128)), [], b_sm[DT])
        S.dma("sp", lambda: nc.sync.dma_start(out=Bm1[0:64], in_=bre_d.rearrange("d g p h -> p (d g) h")), [], b_Bm1)
        S.dma("sp", lambda: nc.sync.dma_start(out=Bm1[64:128], in_=bim_d.rearrange("d g p h -> p (d g) h")), [], b_Bm1)
        S.dma("sp", lambda: nc.sync.dma_start(out=Bm2[0:64], in_=bim_d.rearrange("d g p h -> p (d g) h")), [], b_Bm2)
        S.dma("sp", lambda: nc.sync.dma_start(out=Bm2[64:128], in_=bre_d.rearrange("d g p h -> p (d g) h")), [], b_Bm2)
        S.dma("sp", lambda: nc.sync.dma_start(out=CIN[:, :, 0:64], in_=cre_d.rearrange("(r q) p -> q r p", q=128)), [], b_CIN)
        S.dma("sp", lambda: nc.sync.dma_start(out=CIN[:, :, 64:128], in_=cim_d.rearrange("(r q) p -> q r p", q=128)), [], b_CIN)
        S.dma("sp", lambda: nc.sync.dma_start(out=CIN2[:, :, 0:64], in_=cim_d.rearrange("(r q) p -> q r p", q=128)), [], b_CIN2)
        S.dma("sp", lambda: nc.sync.dma_start(out=CIN2[:, :, 64:128], in_=cre_d.rearrange("(r q) p -> q r p", q=128)), [], b_CIN2)
        for bb in range(5):
            S.dma("sp", (lambda bb=bb: nc.sync.dma_start(out=condT[:, :, bb], in_=cond_d[bb].rearrange("(k p) -> p k", p=128), **NCQ)), [], b_condT)
        S.op("act", lambda: A.activation(out=scT[:, :, 0:5], in_=condT[:, :, 0:5], func=AF.Silu), [b_condT], [b_scT])
        S.dma("sp", lambda: nc.sync.dma_start(out=bmodT, in_=bmod_d.rearrange("(t p) -> p t", p=128), **NCQ), [], b_bmodT)
        pmod = pbank[0][:, 0:384].rearrange("p (t e) -> p t e", e=8)
        for j in range(24):
            s_ = j % 2
            S.dma("sp", (lambda j=j, s_=s_: nc.sync.dma_start(out=wm[s_], in_=wmod_d[:, j * 256:(j + 1) * 256].rearrange("(k p) n -> p k n", p=128))), [], b_wm[s_])
            for f in range(2):
                t = j * 2 + f
                for k in range(8):
                    S.op("pe", (lambda t=t, f=f, k=k, s_=s_: T.matmul(pmod[:, t, 0:5], lhsT=wm[s_][:, k, f * 128:(f + 1) * 128], rhs=scT[:, k, 0:5], start=(k == 0), stop=(k == 7))), [b_wm[s_], b_scT], [b_pb[0]])
        for b in range(5):
            S.op("dve", (lambda b=b: V.tensor_tensor(out=modT[:, :, b], in0=pmod[:, :, b], in1=bmodT, op=ALU.add)), [b_pb[0], b_bmodT], [b_modT])
        S.op("dve", lambda: V.tensor_scalar(out=A1T[:, :, 0:5], in0=modT[:, 8:16, 0:5], scalar1=1.0, scalar2=None, op0=ALU.add), [b_modT], [b_A1T])
        S.op("dve", lambda: V.tensor_scalar(out=A2T[:, :, 0:5], in0=modT[:, 32:40, 0:5], scalar1=1.0, scalar2=None, op0=ALU.add), [b_modT], [b_A2T])
        for b in range(5):
            S.op("dve", (lambda b=b: V.tensor_tensor(out=A1T[:, :, b], in0=A1T[:, :, b], in1=gT12[:, 0, :], op=ALU.mult)), [b_A1T, b_gT12], [b_A1T])
            S.op("dve", (lambda b=b: V.tensor_tensor(out=A2T[:, :, b], in0=A2T[:, :, b], in1=gT12[:, 1, :], op=ALU.mult)), [b_A2T, b_gT12], [b_A2T])

        b_modrow = S.buf("modrow_d")
        for bb in range(NB):
            S.dma("sp", (lambda bb=bb: nc.sync.dma_start(out=modrow_d[bb].rearrange("(t p) -> p t", p=128), in_=modT[:, :, bb], **NCQ)), [b_modT], b_modrow, par=True)
            S.dma("sp", (lambda bb=bb: nc.sync.dma_start(out=a2row_d[bb].rearrange("(k p) -> p k", p=128), in_=A2T[:, :, bb], **NCQ)), [b_A2T], b_modrow, par=True)
        (LRE, LIM, DT, XR, XI, MAG, SN, CS, ARE, AIM, T1, T2, T3, NR, D2, RD, QRE, QIM, QB, T4) = range(20)

        def smv(i):
            return sm[:, i, :]

        def dv(fn, reads, writes):
            S.op("dve", fn, [b_sm[i] for i in reads], [b_sm[i] for i in writes])

        def tt(o, a, b, op):
            dv((lambda: V.tensor_tensor(out=smv(o), in0=smv(a), in1=smv(b), op=op)), [a, b], [o])

        S.op("act", lambda: A.activation(out=smv(DT), in_=smv(DT), func=AF.Exp), [b_sm[DT]], [b_sm[DT]])
        tt(XR, LRE, DT, ALU.mult); tt(XI, LIM, DT, ALU.mult)
        S.op("act", lambda: A.activation(out=smv(MAG), in_=smv(XR), func=AF.Exp, scale=1.0 / 16), [b_sm[XR]], [b_sm[MAG]])
        S.op("act", lambda: A.activation(out=smv(SN), in_=smv(XI), func=AF.Sin, scale=1.0 / 16), [b_sm[XI]], [b_sm[SN]])
        dv((lambda: V.tensor_scalar(out=smv(T4), in0=smv(XI), scalar1=1.0 / 16, scalar2=math.pi / 2, op0=ALU.mult, op1=ALU.add)), [XI], [T4])
        S.op("act", lambda: A.activation(out=smv(CS), in_=smv(T4), func=AF.Sin), [b_sm[T4]], [b_sm[CS]])
        tt(ARE, MAG, CS, ALU.mult); tt(AIM, MAG, SN, ALU.mult)

        def csq(re, im):
            tt(T1, re, re, ALU.mult); tt(T2, im, im, ALU.mult); tt(T3, re, im, ALU.mult)
            tt(re, T1, T2, ALU.subtract)
            dv((lambda: V.tensor_scalar(out=smv(im), in0=smv(T3), scalar1=2.0, scalar2=None, op0=ALU.mult)), [T3], [im])

        for _ in range(4):
            csq(ARE, AIM)
        dv((lambda: V.tensor_scalar(out=smv(NR), in0=smv(ARE), scalar1=-1.0, scalar2=None, op0=ALU.add)), [ARE], [NR])
        tt(T1, LRE, LRE, ALU.mult); tt(T2, LIM, LIM, ALU.mult); tt(D2, T1, T2, ALU.add)
        dv((lambda: V.reciprocal(out=smv(RD), in_=smv(D2))), [D2], [RD])
        tt(T1, NR, LRE, ALU.mult); tt(T2, AIM, LIM, ALU.mult); tt(T3, T1, T2, ALU.add); tt(QRE, T3, RD, ALU.mult)
        tt(T1, AIM, LRE, ALU.mult); tt(T2, NR, LIM, ALU.mult); tt(T3, T1, T2, ALU.subtract); tt(QIM, T3, RD, ALU.mult)
        S.op("dve", lambda: V.tensor_scalar(out=smv(QB), in0=smv(QIM), scalar1=sgn, scalar2=-1.0, op0=ALU.mult, op1=ALU.mult), [b_sm[QIM], b_cst], [b_sm[QB]])
        def cmulP(o_re, o_im, bo, x_re, x_im, bx, y_re, y_im, by):
            bxy = list(bx) + list(by)
            S.op("dve", (lambda: V.tensor_tensor(out=smv(T1), in0=x_re, in1=y_re, op=ALU.mult)), bxy, [b_sm[T1]])
            S.op("dve", (lambda: V.tensor_tensor(out=smv(T2), in0=x_im, in1=y_im, op=ALU.mult)), bxy, [b_sm[T2]])
            S.op("dve", (lambda: V.tensor_tensor(out=o_re, in0=smv(T1), in1=smv(T2), op=ALU.subtract)), [b_sm[T1], b_sm[T2]], [bo])
            S.op("dve", (lambda: V.tensor_tensor(out=smv(T1), in0=x_re, in1=y_im, op=ALU.mult)), bxy, [b_sm[T1]])
            S.op("dve", (lambda: V.tensor_tensor(out=smv(T2), in0=x_im, in1=y_re, op=ALU.mult)), bxy, [b_sm[T2]])
            S.op("dve", (lambda: V.tensor_tensor(out=o_im, in0=smv(T1), in1=smv(T2), op=ALU.add)), [b_sm[T1], b_sm[T2]], [bo])

        AIRE, AIIM, A8RE, A8IM = 20, 21, 22, 23
        b_A = S.buf("a_pair")
        S.op("dve", lambda: V.memset(Pre[:, :, 7], 1.0), [], [b_P[7]])
        S.op("dve", lambda: V.memset(Pim[:, :, 7], 0.0), [], [b_P[7]])
        S.op("dve", lambda: V.tensor_copy(out=Pre[:, :, 8], in_=smv(ARE)), [b_sm[ARE], b_sm[AIM]], [b_P[8]])
        S.op("dve", lambda: V.tensor_copy(out=Pim[:, :, 8], in_=smv(AIM)), [b_sm[AIM]], [b_P[8]])
        for m in range(8, 15):
            cmulP(Pre[:, :, m + 1], Pim[:, :, m + 1], b_P[m + 1], Pre[:, :, m], Pim[:, :, m], [b_P[m]], smv(ARE), smv(AIM), [b_sm[ARE], b_sm[AIM]])
        tt(T1, ARE, ARE, ALU.mult); tt(T2, AIM, AIM, ALU.mult); tt(T3, T1, T2, ALU.add)
        dv((lambda: V.reciprocal(out=smv(T4), in_=smv(T3))), [T3], [T4])
        tt(AIRE, ARE, T4, ALU.mult)
        dv((lambda: V.scalar_tensor_tensor(out=smv(AIIM), in0=smv(AIM), scalar=-1.0, in1=smv(T4), op0=ALU.mult, op1=ALU.mult)), [AIM, T4], [AIIM])
        b_AI = S.buf("ainv_pair")
        for m in range(7, 0, -1):
            cmulP(Pre[:, :, m - 1], Pim[:, :, m - 1], b_P[m - 1], Pre[:, :, m], Pim[:, :, m], [b_P[m]], smv(AIRE), smv(AIIM), [b_sm[AIRE], b_sm[AIIM]])
        S.op("dve", lambda: V.tensor_copy(out=smv(A8RE), in_=Pre[:, :, 15]), [b_P[15]], [b_sm[A8RE]])
        S.op("dve", lambda: V.tensor_copy(out=smv(A8IM), in_=Pim[:, :, 15]), [b_P[15]], [b_sm[A8IM]])
        for m in range(NLEV):
            S.op("dve", (lambda m=m: V.tensor_copy(out=c1t[:, :, m], in_=smv(A8RE))), [b_sm[A8RE]], [b_c1t])
            S.op("dve", (lambda m=m: V.tensor_scalar(out=c2t[:, :, m], in0=smv(A8IM), scalar1=sgn, scalar2=None, op0=ALU.mult)), [b_sm[A8IM], b_cst], [b_c2t])
            if m < NLEV - 1:
                csq(A8RE, A8IM)
        b_rotds = [S.buf("rot_d0"), S.buf("rot_d1")]
        b_Pall = b_P
        for s8 in range(8):
            S.op("dve", (lambda s8=s8: V.tensor_copy(out=PEre[:, 0:16, s8], in_=Pre[:, 0:16, 14 - s8])), [b_P[14 - s8]], [b_PE])
            S.op("dve", (lambda s8=s8: V.tensor_copy(out=PEim[:, 0:16, s8], in_=Pim[:, 0:16, 14 - s8])), [b_P[14 - s8]], [b_PE])
            S.op("dve", (lambda s8=s8: V.tensor_copy(out=PEre[:, 16:32, s8], in_=Pre[:, 16:32, 7 + s8])), [b_P[7 + s8]], [b_PE])
            S.op("dve", (lambda s8=s8: V.tensor_copy(out=PEim[:, 16:32, s8], in_=Pim[:, 16:32, 7 + s8])), [b_P[7 + s8]], [b_PE])
        qre_b = smv(QRE).unsqueeze(2).broadcast_to([128, 32, 8])
        qim_b = smv(QIM).unsqueeze(2).broadcast_to([128, 32, 8])
        S.op("dve", lambda: V.tensor_tensor(out=PQre, in0=PEre, in1=qre_b, op=ALU.mult), [b_PE, b_sm[QRE]], [b_PQ])
        S.op("dve", lambda: V.tensor_tensor(out=PQt, in0=PEim, in1=qim_b, op=ALU.mult), [b_PE, b_sm[QIM]], [b_PQt])
        S.op("dve", lambda: V.tensor_tensor(out=PQre, in0=PQre, in1=PQt, op=ALU.subtract), [b_PQ, b_PQt], [b_PQ])
        S.op("dve", lambda: V.tensor_tensor(out=PQim, in0=PEre, in1=qim_b, op=ALU.mult), [b_PE, b_sm[QIM]], [b_PQi])
        S.op("dve", lambda: V.tensor_tensor(out=PQt, in0=PEim, in1=qre_b, op=ALU.mult), [b_PE, b_sm[QRE], b_PQ], [b_PQt])
        S.op("dve", lambda: V.tensor_tensor(out=PQim, in0=PQim, in1=PQt, op=ALU.add), [b_PQi, b_PQt], [b_PQi])
        S.op("dve", lambda: V.tensor_scalar(out=PQim, in0=PQim, scalar1=sgn, scalar2=-1.0, op0=ALU.mult, op1=ALU.mult), [b_PQi, b_cst], [b_PQi])
        for d in range(2):
            dsl = slice(d * 16, (d + 1) * 16)
            S.op("dve", (lambda dsl=dsl: V.tensor_tensor(out=LFa, in0=PQre[:, dsl, :].unsqueeze(3).broadcast_to([128, 16, 8, 16]), in1=Bm1[:, dsl, :].unsqueeze(2).broadcast_to([128, 16, 8, 16]), op=ALU.mult)), [b_PQ, b_Bm1], [b_LFa])
            S.op("dve", (lambda dsl=dsl: V.tensor_tensor(out=LFb, in0=PQim[:, dsl, :].unsqueeze(3).broadcast_to([128, 16, 8, 16]), in1=Bm2[:, dsl, :].unsqueeze(2).broadcast_to([128, 16, 8, 16]), op=ALU.mult)), [b_PQi, b_Bm2], [b_LFb])
            S.op("dve", (lambda dsl=dsl: V.tensor_tensor(out=LF4[:, dsl], in0=LFa, in1=LFb, op=ALU.add)), [b_LFa, b_LFb], [b_LF])
        for r in range(4):
            for q8 in range(8):
                dg = r * 8 + q8
                S.op("pe", (lambda dg=dg, q8=q8: T.transpose(out=pT[:, q8, :], in_=LF[:, dg, :], identity=identb[:])), [b_LF, b_identb], [b_pT])
            S.op("act", (lambda r=r: A.copy(out=Wt[:, r * 8:(r + 1) * 8, :], in_=pT[:, :, :])), [b_pT], [b_Wt])
        for r in range(4):
            S.op("pe", (lambda r=r: T.transpose(out=pbank[1][:, r * 128:(r + 1) * 128], in_=CIN[:, r, :], identity=identf)), [b_CIN, b_cst], [b_pb[1]])
        S.op("dve", lambda: V.tensor_scalar(out=Cm1, in0=pbank[1][:, :], scalar1=sgn, scalar2=None, op0=ALU.mult), [b_pb[1], b_cst], [b_Cm1])
        for r in range(4):
            S.op("pe", (lambda r=r: T.transpose(out=pbank[2][:, r * 128:(r + 1) * 128], in_=CIN2[:, r, :], identity=identf)), [b_CIN2, b_cst], [b_pb[2]])
        S.op("dve", lambda: V.tensor_scalar(out=Cm2, in0=pbank[2][:, :], scalar1=sgn, scalar2=None, op0=ALU.mult), [b_pb[2], b_cst], [b_Cm2])
        S.op("dve", lambda: V.tensor_copy(out=PRre[:, 0:16, :], in_=Pre[:, 0:16, :]), b_P, [b_PR])
        S.op("dve", lambda: V.tensor_copy(out=PRim[:, 0:16, :], in_=Pim[:, 0:16, :]), b_P, [b_PR])
        for m in range(16):
            S.op("dve", (lambda m=m: V.tensor_copy(out=PRre[:, 16:32, m], in_=Pre[:, 16:32, 15 - m])), [b_P[15 - m]], [b_PR])
            S.op("dve", (lambda m=m: V.tensor_copy(out=PRim[:, 16:32, m], in_=Pim[:, 16:32, 15 - m])), [b_P[15 - m]], [b_PR])
        S.op("dve", lambda: V.tensor_scalar(out=PRim, in0=PRim, scalar1=sgn, scalar2=-1.0, op0=ALU.mult, op1=ALU.mult), [b_PR, b_cst], [b_PR])
        Cm1v = Cm1.rearrange("p (a b) -> p a b", a=32)
        Cm2v = Cm2.rearrange("p (a b) -> p a b", a=32)
        for d in range(2):
            dsl = slice(d * 16, (d + 1) * 16)
            S.op("dve", (lambda dsl=dsl: V.tensor_tensor(out=RCa, in0=PRre[:, dsl, :].unsqueeze(3).broadcast_to([128, 16, 16, 16]), in1=Cm1v[:, dsl, :].unsqueeze(2).broadcast_to([128, 16, 16, 16]), op=ALU.mult)), [b_PR, b_Cm1], [b_RCa])
            S.op("dve", (lambda dsl=dsl: V.tensor_tensor(out=RCb, in0=PRim[:, dsl, :].unsqueeze(3).broadcast_to([128, 16, 16, 16]), in1=Cm2v[:, dsl, :].unsqueeze(2).broadcast_to([128, 16, 16, 16]), op=ALU.mult)), [b_PR, b_Cm2], [b_RCb])
            S.op("dve", (lambda dsl=dsl: V.tensor_tensor(out=RC4[:, dsl], in0=RCa, in1=RCb, op=ALU.add)), [b_RCa, b_RCb], [b_RC])
        for g4 in range(4):
            for gi in range(4):
                g = g4 * 4 + gi
                S.op("pe", (lambda g=g, gi=gi: T.matmul(pbank[3][:, gi * 128:(gi + 1) * 128], lhsT=LF[:, g, :], rhs=RC[:, g, 0:128], start=True, stop=True)), [b_LF, b_RC], [b_pb[3]])
                S.op("pe", (lambda g=g, gi=gi: T.matmul(pbank[4][:, gi * 128:(gi + 1) * 128], lhsT=LF[:, 16 + g, :], rhs=RC[:, 16 + g, 128:256], start=True, stop=True)), [b_LF, b_RC], [b_pb[4]])
            S.op("dve", lambda: V.tensor_tensor(out=Mt1, in0=pbank[3][:, :].rearrange("p (a b) -> p a b", a=4), in1=maskf.unsqueeze(1).broadcast_to([128, 4, 128]), op=ALU.mult), [b_pb[3], b_cst], [b_Mt1])
            S.op("dve", lambda: V.tensor_tensor(out=Mt2, in0=pbank[4][:, :].rearrange("p (a b) -> p a b", a=4), in1=maskb.unsqueeze(1).broadcast_to([128, 4, 128]), op=ALU.mult), [b_pb[4], b_cst], [b_Mt2])
            S.op("dve", (lambda g4=g4: V.tensor_tensor(out=M0[:, g4 * 4:(g4 + 1) * 4, :], in0=Mt1, in1=Mt2, op=ALU.add)), [b_Mt1, b_Mt2], [b_M0])

        S.barrier()

        cb = Carver(ARB, ARB_N); cf = Carver(ARF, ARF_N)
        cb.take(8, 2560)
        UT = cb.take(2, 2560); b_UT = [S.buf("UT0"), S.buf("UT1")]
        o_alias = cb.o
        cvcol = cb.take(3, SEQ); b_cvcol = [S.buf("cvcol%d" % i) for i in range(3)]
        bcol = cb.take(3, SEQ); b_bcol = [S.buf("bcol%d" % i) for i in range(3)]
        o_end = cb.o
        cb.o = o_alias
        Hs2 = [[cb.take(NBLK), cb.take(NBLK)], [cb.take(NBLK), cb.take(NBLK)]]
        b_Hs2 = [[S.buf("Hs00"), S.buf("Hs01")], [S.buf("Hs10"), S.buf("Hs11")]]
        rot = [cb.take(2 * NLEV, 128), cb.take(2 * NLEV, 128)]; b_rot = [S.buf("rot0"), S.buf("rot1")]
        gTt = cb.take(2, SEQ); b_gTt = [S.buf("gT0"), S.buf("gT1")]
        Yall = cb.take(8, 256); b_Yall = S.buf("Yall")
        assert cb.o <= o_end
        cb.o = o_end
        hT0 = cb.take(8, 512)
        junk = cb.take(D); b_junk = S.buf("junk")
        xn = cb.take(D); b_xn = S.buf("xn")
        yst = [cb.take(512), cb.take(512)]; b_yst = [S.buf("yst0"), S.buf("yst1")]
        ycolst = cb.take(SEQ); b_ycolst = S.buf("ycolst")
        Xg = [ycolst[:, 0:NX], ycolst[:, 1024:1024 + NX]]; b_Xg = [S.buf("Xg0"), S.buf("Xg1")]
        assert cb.o <= TAIL, cb.o
        cb.o = TAIL + 14336
        hT1 = cb.take(8, 512)
        hTs = [hT0, hT1]; b_hTs = [S.buf("hT0"), S.buf("hT1")]
        hcnt = {"i": 0}
        xt = [cf.take(D), cf.take(D)]; b_xt = [S.buf("xt0"), S.buf("xt1")]
        ysacc = cf.take(SEQ); b_ysacc = S.buf("ysacc")
        Csb2 = [cf.take(512), cf.take(512)]; b_Csb2 = [S.buf("Csb0"), S.buf("Csb1")]
        cvt2 = [cf.take(512), cf.take(512)]; b_cvt2 = [S.buf("cvt0"), S.buf("cvt1")]
        cacc2 = [cf.take(512), cf.take(512)]; b_cacc2 = [S.buf("cacc0"), S.buf("cacc1")]
        ge1 = cf.take(512); ge2 = cf.take(512); b_ge1 = S.buf("ge1"); b_ge2 = S.buf("ge2")
        rt1 = cf.take(128); rt2 = cf.take(128); b_rt1 = S.buf("rt1"); b_rt2 = S.buf("rt2")
        colacc = cf.take(SEQ) if False else None


        b_wb = S.buf("wb_scr")
        precast = []
        for e in range(16):
            precast.append(lambda e=e: S.dma("pool", (lambda: P.dma_start(out=w1b_d[e], in_=w1_d[e].rearrange("(k p) n -> p k n", p=128))), [], b_wb))
            precast.append(lambda e=e: S.dma("pool", (lambda: P.dma_start(out=w3b_d[e], in_=w3_d[e].rearrange("(k p) n -> p k n", p=128))), [], b_wb))
            precast.append(lambda e=e: S.dma("pool", (lambda: P.dma_start(out=w2b_d[e], in_=w2_d[e].rearrange("(k p) n -> p k n", p=128))), [], b_wb))
        b_ycats = [S.buf("ycat_d%d" % i) for i in range(4)]
        b_x1ds = [S.buf("x1_d%d" % i) for i in range(4)]
        b_outs = [S.buf("out_d%d" % i) for i in range(4)]
        rr = {"yc": 0, "out": 0}

        def nyc():
            rr["yc"] += 1
            return b_ycats[rr["yc"] % 4]
        cnt = {"xt": 0, "ev": 0, "ss": 0}

        def norm_transpose(nb, src_rows, AT, ST, bA, bS, idx, dst, b_dst, eps=1e-6, src_sb=None, b_src=None):
            junk, xn, b_junk, b_xn, xt, b_xt = nb
            if src_sb is None:
                i = cnt["xt"] % 2; cnt["xt"] += 1
                xs = xt[i]; bx = b_xt[i]
                S.dma("sp", (lambda: nc.sync.dma_start(out=xs, in_=src_rows)), [], bx)
            else:
                xs = src_sb; bx = b_src
            j = cnt["ss"] % 4; cnt["ss"] += 1
            S.op("act", (lambda: A.activation(out=junk, in_=xs, func=AF.Square, accum_out=ss[:, j:j + 1])), [bx], [b_junk, b_ss[j]])
            S.op("act", (lambda: A.activation(out=rstd[:, j:j + 1], in_=ss[:, j:j + 1], func=AF.Sqrt, scale=1.0 / D, bias=epsc)), [b_ss[j], b_cst], [b_rstd[j]])
            S.op("dve", (lambda: V.reciprocal(out=rstd[:, j:j + 1], in_=rstd[:, j:j + 1])), [b_rstd[j]], [b_rstd[j]])
            S.op("act", (lambda: A.activation(out=xn, in_=xs, func=AF.Copy, scale=rstd[:, j:j + 1])), [bx, b_rstd[j]], [b_xn])
            for k in range(8):
                S.op("pe", (lambda k=k: T.transpose(out=pT[:, k, :], in_=xn[:, k * 128:(k + 1) * 128], identity=identb[:])), [b_xn, b_identb], [b_pT])
            for k in range(8):
                if k % 2 == 0:
                    S.op("dve", (lambda k=k: V.tensor_scalar(out=dst[:, k, :], in0=pT[:, k, :], scalar1=AT[:, k, idx:idx + 1], scalar2=ST[:, k, idx:idx + 1], op0=ALU.mult, op1=ALU.add)), [b_pT, bA, bS], [b_dst])
                else:
                    S.op("act", (lambda k=k: A.activation(out=dst[:, k, :], in_=pT[:, k, :], func=AF.Identity, scale=AT[:, k, idx:idx + 1], bias=ST[:, k, idx:idx + 1])), [b_pT, bA, bS], [b_dst])
            return j

        S1T = modT[:, 0:8, :]
        nbB = (junk, xn, b_junk, b_xn, xt, b_xt)
        S2T = modT[:, 24:32, :]
        pz = {"i": 0}

        def zbank():
            i = 1 + (pz["i"] % 6); pz["i"] += 1
            return pbank[i], b_pb[i]

        def ctx_norms(bb):
            hT_ = hTs[hcnt["i"] % 2]; b_hT_ = b_hTs[hcnt["i"] % 2]; hcnt["i"] += 1
            for ti in range(2):
                norm_transpose(nbB, ctx_d[bb * NCTX + ti * 128: bb * NCTX + (ti + 1) * 128, :], A1T, S1T, b_A1T, b_modT, 4, hT_[:, :, ti * 128:(ti + 1) * 128], b_hT_)
            return hT_, b_hT_

        def chunk_norms(bb, c_):
            t0_ = bb * SEQ + c_ * 512
            hT_ = hTs[hcnt["i"] % 2]; b_hT_ = b_hTs[hcnt["i"] % 2]; hcnt["i"] += 1
            for ti in range(4):
                norm_transpose(nbB, x_d[t0_ + ti * 128: t0_ + (ti + 1) * 128, :], A1T, S1T, b_A1T, b_modT, bb, hT_[:, :, ti * 128:(ti + 1) * 128], b_hT_)
            return hT_, b_hT_

        def chunk_norm_alloc():
            hT_ = hTs[hcnt["i"] % 2]; b_hT_ = b_hTs[hcnt["i"] % 2]; hcnt["i"] += 1
            return hT_, b_hT_

        def chunk_norm_tile(bb, c_, ti, hT_, b_hT_):
            t0_ = bb * SEQ + c_ * 512
            norm_transpose(nbB, x_d[t0_ + ti * 128: t0_ + (ti + 1) * 128, :], A1T, S1T, b_A1T, b_modT, bb, hT_[:, :, ti * 128:(ti + 1) * 128], b_hT_)

        pref = {}
        need_bar = {"v": False}
        for b in range(NB):
            if b in pref:
                (hT, b_hT), pre_c0 = pref[b]
            else:
                hT, b_hT = ctx_norms(b)
                pre_c0 = None
            for ft in range(2):
                pb_, bpb_ = zbank()
                for k in range(8):
                    S.op("pe", (lambda k=k, ft=ft, pb_=pb_, hT=hT: T.matmul(pb_[:, 0:256], lhsT=win[:, k, ft * 128:(ft + 1) * 128], rhs=hT[:, k, 0:256], start=(k == 0), stop=(k == 7))), [b_win, b_hT], [bpb_])
                S.op("act", (lambda ft=ft, pb_=pb_: A.copy(out=UT[:, ft, 0:256], in_=pb_[:, 0:256])), [bpb_], [b_UT[ft]])
                S.op("dve", (lambda ft=ft, pb_=pb_: V.tensor_copy(out=UT[:, ft, 2304:2560], in_=pb_[:, 0:256])), [bpb_], [b_UT[ft]])
            def do_norms(c_):
                return chunk_norms(b, c_)

            nxt_h = pre_c0 if pre_c0 is not None else do_norms(0)
            for c in range(4):
                t0 = b * SEQ + c * 512
                hT, b_hT = nxt_h

                def zx(col0, hT=hT, b_hT=b_hT):
                    pb_, bpb_ = zbank()
                    for k in range(8):
                        S.op("pe", (lambda k=k, pb_=pb_: T.matmul(pb_[:, :], lhsT=win[:, k, col0:col0 + 128], rhs=hT[:, k, :], start=(k == 0), stop=(k == 7))), [b_win, b_hT], [bpb_])
                    return pb_, bpb_

                for ft in range(2):
                    pb_, bpb_ = zx(ft * 128)
                    S.op("act", (lambda ft=ft, pb_=pb_, c=c: A.copy(out=UT[:, ft, 256 + c * 512: 256 + (c + 1) * 512], in_=pb_[:, :])), [bpb_], [b_UT[ft]])
                if c + 1 < 4:
                    nxt_h = chunk_norm_alloc()
                for i in range(6):
                    if i == 3 and need_bar["v"]:
                        S.barrier()
                        need_bar["v"] = False
                    if c + 1 < 4 and 1 <= i <= 4:
                        chunk_norm_tile(b, c + 1, i - 1, nxt_h[0], nxt_h[1])
                    q = i % 2
                    Csb_, cvt_, cacc_ = Csb2[q], cvt2[q], cacc2[q]
                    bCsb_, bcvt_, bcacc_ = b_Csb2[q], b_cvt2[q], b_cacc2[q]
                    pC, bC = zx(1024 + i * 128)
                    S.op("act", (lambda pC=pC, Csb_=Csb_: A.copy(out=Csb_, in_=pC[:, :])), [bC], [bCsb_])
                    pV, bV = zx(1792 + i * 128)
                    if i < 3:
                        S.op("dve", (lambda pV=pV, Csb_=Csb_, cvt_=cvt_: V.tensor_tensor(out=cvt_, in0=Csb_, in1=pV[:, :], op=ALU.mult)), [bCsb_, bV], [bcvt_])
                        pB, bB = zx(256 + i * 128)
                        S.op("act", (lambda i=i, cvt_=cvt_, cacc_=cacc_: A.activation(out=cacc_, in_=cvt_, func=AF.Copy, scale=cw[:, i, 1:2])), [bcvt_, b_cw], [bcacc_])
                        c3 = cvt_.rearrange("p (r w) -> p r w", w=64)
                        a3 = cacc_.rearrange("p (r w) -> p r w", w=64)
                        S.op("dve", (lambda i=i, c3=c3, a3=a3: V.scalar_tensor_tensor(out=a3[:, :, 1:64], in0=c3[:, :, 0:63], scalar=cw[:, i, 0:1], in1=a3[:, :, 1:64], op0=ALU.mult, op1=ALU.add)), [bcvt_, bcacc_, b_cw], [bcacc_])
                        S.op("dve", (lambda i=i, c3=c3, a3=a3: V.scalar_tensor_tensor(out=a3[:, :, 0:63], in0=c3[:, :, 1:64], scalar=cw[:, i, 2:3], in1=a3[:, :, 0:63], op0=ALU.mult, op1=ALU.add)), [bcvt_, bcacc_, b_cw], [bcacc_])
                        yi = (c * 6 + i) % 2
                        S.op("dve", (lambda pB=pB, yi=yi, cacc_=cacc_: V.tensor_tensor(out=yst[yi], in0=cacc_, in1=pB[:, :], op=ALU.mult)), [bcacc_, bB], [b_yst[yi]])
                        S.dma("pool", (lambda b=b, i=i, c=c, yi=yi: P.dma_start(out=ycat_d[b, :, 2 + i, c * 512:(c + 1) * 512], in_=yst[yi])), [b_yst[yi]], nyc())
                    else:
                        j = i - 3
                        S.op("dve", (lambda pV=pV, j=j, c=c, Csb_=Csb_: V.tensor_tensor(out=cvcol[:, j, c * 512:(c + 1) * 512], in0=Csb_, in1=pV[:, :], op=ALU.mult)), [bCsb_, bV], [b_cvcol[j]])
                        pB, bB = zx(256 + i * 128)
                        S.op("act", (lambda pB=pB, j=j, c=c: A.copy(out=bcol[:, j, c * 512:(c + 1) * 512], in_=pB[:, :])), [bB], [b_bcol[j]])
            for j in range(3):
                i = 3 + j
                S.op("act", (lambda j=j, i=i: A.activation(out=ysacc, in_=cvcol[:, j, :], func=AF.Copy, scale=cw[:, i, 1:2])), [b_cvcol[j], b_cw], [b_ysacc])
                S.op("dve", (lambda j=j, i=i: V.scalar_tensor_tensor(out=ysacc[:, 64:SEQ], in0=cvcol[:, j, 0:SEQ - 64], scalar=cw[:, i, 0:1], in1=ysacc[:, 64:SEQ], op0=ALU.mult, op1=ALU.add)), [b_cvcol[j], b_ysacc, b_cw], [b_ysacc])
                S.op("dve", (lambda j=j, i=i: V.scalar_tensor_tensor(out=ysacc[:, 0:SEQ - 64], in0=cvcol[:, j, 64:SEQ], scalar=cw[:, i, 2:3], in1=ysacc[:, 0:SEQ - 64], op0=ALU.mult, op1=ALU.add)), [b_cvcol[j], b_ysacc, b_cw], [b_ysacc])
                S.op("pool", (lambda j=j: P.tensor_tensor(out=ycolst, in0=ysacc, in1=bcol[:, j, :], op=ALU.mult)), [b_ysacc, b_bcol[j]], [b_ycolst])
                S.dma("pool", (lambda b=b, j=j: P.dma_start(out=ycat_d[b, :, 5 + j, :], in_=ycolst)), [b_ycolst], nyc())
            S.barrier()
            for _ in range(16):
                if precast:
                    precast.pop(0)()
            if b + 1 < NB:
                pref[b + 1] = (ctx_norms(b + 1), chunk_norms(b + 1, 0))
            for ft in range(2):
                for gp in range(4):
                    gl = [(ft * 8 + gp * 2 + u, gp * 2 + u, u) for u in range(2)]
                    for (g, g8, ri) in gl:
                        if b == 0:
                            for d in range(2):
                                dg = d * 16 + g
                                for m in range(NLEV):
                                    S.op("dve", (lambda dg=dg, m=m: V.tensor_scalar(out=rt1, in0=identf, scalar1=c1t[:, dg, m:m + 1], scalar2=None, op0=ALU.mult)), [b_cst, b_c1t], [b_rt1])
                                    S.op("dve", (lambda d=d, dg=dg, m=m, ri=ri: V.scalar_tensor_tensor(out=rot[ri][:, d * NLEV + m, :], in0=iswapf, scalar=c2t[:, dg, m:m + 1], in1=rt1, op0=ALU.mult, op1=ALU.add)), [b_cst, b_c2t, b_rt1], [b_rot[ri]])
                            S.dma("pool", (lambda g=g, ri=ri: P.dma_start(out=rot_d[g], in_=rot[ri])), [b_rot[ri]], b_rotds[ri])
                        else:
                            S.dma("sp", (lambda g=g, ri=ri: nc.sync.dma_start(out=rot[ri], in_=rot_d[g])), b_rotds, b_rot[ri])
                        pb_, bpb_ = zbank()
                        for s8 in range(8):
                            S.op("pe", (lambda pb_=pb_, s8=s8, g8=g8, ft=ft: T.matmul(pb_[:, 0:NX], lhsT=Gsel[:, g8, 112 - 16 * s8: 240 - 16 * s8], rhs=UT[:, ft, s8:2560:8], start=(s8 == 0), stop=(s8 == 7))), [b_Gsel, b_UT[ft]], [bpb_])
                        S.op("act", (lambda pb_=pb_, ri=ri: A.copy(out=Xg[ri], in_=pb_[:, 0:NX])), [bpb_], [b_Xg[ri]])
                    for (g, g8, ri) in gl:
                        for d in range(2):
                            dg = d * 16 + g
                            off = 0 if d == 0 else 32
                            pb_, bpb_ = zbank()
                            S.op("pe", (lambda pb_=pb_, dg=dg, off=off, ri=ri: T.matmul(pb_[:, 0:NBLK], lhsT=Wt[:, dg, :], rhs=Xg[ri][:, off:off + NBLK], start=True, stop=True)), [b_Wt, b_Xg[ri]], [bpb_])
                            if d == 0:
                                S.op("act", (lambda pb_=pb_, d=d, ri=ri: A.copy(out=Hs2[ri][d], in_=pb_[:, 0:NBLK])), [bpb_], [b_Hs2[ri][d]])
                            else:
                                S.op("dve", (lambda pb_=pb_, d=d, ri=ri: V.tensor_copy(out=Hs2[ri][d], in_=pb_[:, 0:NBLK])), [bpb_], [b_Hs2[ri][d]])
                    for m in range(NLEV):
                        s_ = 1 << m
                        n = NBLK - s_
                        for (g, g8, ri) in gl:
                            for d in range(2):
                                lo, rlo = (s_, 0) if d == 0 else (0, s_)
                                pb_, bpb_ = zbank()
                                S.op("pe", (lambda pb_=pb_, d=d, m=m, rlo=rlo, n=n, ri=ri: T.matmul(pb_[:, 0:n], lhsT=rot[ri][:, d * NLEV + m, :], rhs=Hs2[ri][d][:, rlo:rlo + n], start=True, stop=True)), [b_rot[ri], b_Hs2[ri][d]], [bpb_])
                                S.op("dve", (lambda pb_=pb_, d=d, lo=lo, n=n, ri=ri: V.tensor_tensor(out=Hs2[ri][d][:, lo:lo + n], in0=pb_[:, 0:n], in1=Hs2[ri][d][:, lo:lo + n], op=ALU.add)), [bpb_, b_Hs2[ri][d]], [b_Hs2[ri][d]])
                    for (g, g8, ri) in gl:
                        pb_, bpb_ = zbank()
                        S.op("pe", (lambda pb_=pb_, g=g, ri=ri: T.matmul(pb_[:, 0:256], lhsT=M0[:, g, :], rhs=Xg[ri][:, 32:288], start=True, stop=False)), [b_M0, b_Xg[ri]], [bpb_])
                        S.op("pe", (lambda pb_=pb_, g=g, ri=ri: T.matmul(pb_[:, 0:256], lhsT=RC[:, g, 128:256], rhs=Hs2[ri][0][:, 31:287], start=False, stop=False)), [b_RC, b_Hs2[ri][0]], [bpb_])
                        S.op("pe", (lambda pb_=pb_, g=g, ri=ri: T.matmul(pb_[:, 0:256], lhsT=RC[:, 16 + g, 0:128], rhs=Hs2[ri][1][:, 1:257], start=False, stop=True)), [b_RC, b_Hs2[ri][1]], [bpb_])
                        S.op("act", (lambda pb_=pb_, g8=g8: A.copy(out=Yall[:, g8, :], in_=pb_[:, 0:256])), [bpb_], [b_Yall])
                for q in range(4):
                    pb_, bpb_ = zbank()
                    for t8 in range(8):
                        for g8 in range(8):
                            S.op("pe", (lambda pb_=pb_, t8=t8, g8=g8, q=q: T.matmul(pb_[:, t8:512:8], lhsT=Gsel[:, t8, 112 - 16 * g8: 240 - 16 * g8], rhs=Yall[:, g8, q * 64:(q + 1) * 64], start=(g8 == 0), stop=(g8 == 7))), [b_Gsel, b_Yall], [bpb_])
                    S.op("dve", (lambda pb_=pb_, q=q, ft=ft: V.scalar_tensor_tensor(out=ysacc[:, q * 512:(q + 1) * 512], in0=UT[:, ft, 256 + q * 512: 256 + (q + 1) * 512], scalar=dcol[:, ft:ft + 1], in1=pb_[:, :], op0=ALU.mult, op1=ALU.add)), [b_UT[ft], b_dcol, bpb_], [b_ysacc])
                for c in range(4):
                    xs_ = ysacc[:, c * 512:(c + 1) * 512]
                    S.op("act", (lambda xs_=xs_: A.activation(out=ge1, in_=xs_, func=AF.Square)), [b_ysacc], [b_ge1])
                    S.op("dve", (lambda: V.tensor_scalar(out=ge1, in0=ge1, scalar1=0.044715, scalar2=1.0, op0=ALU.mult, op1=ALU.add)), [b_ge1], [b_ge1])
                    S.op("dve", (lambda xs_=xs_: V.tensor_tensor(out=ge2, in0=ge1, in1=xs_, op=ALU.mult)), [b_ge1, b_ysacc], [b_ge2])
                    S.op("act", (lambda: A.activation(out=ge1, in_=ge2, func=AF.Sigmoid, scale=1.5957691216057308)), [b_ge2, b_ge1], [b_ge1])
                    S.op("dve", (lambda xs_=xs_, ft=ft, c=c: V.tensor_tensor(out=gTt[:, ft, c * 512:(c + 1) * 512], in0=ge1, in1=xs_, op=ALU.mult)), [b_ge1, b_ysacc], [b_gTt[ft]])
            for c in range(4):
                for f2 in range(2):
                    pb_, bpb_ = zbank()
                    for k in range(2):
                        S.op("pe", (lambda pb_=pb_, k=k, f2=f2, c=c: T.matmul(pb_[:, :], lhsT=wglu[:, k, f2 * 128:(f2 + 1) * 128], rhs=gTt[:, k, c * 512:(c + 1) * 512], start=(k == 0), stop=(k == 1))), [b_wglu, b_gTt[k]], [bpb_])
                    S.op("act", (lambda pb_=pb_, f2=f2: A.activation(out=ge1, in_=pb_[:, :], func=AF.Sigmoid, bias=bglu[:, f2:f2 + 1])), [bpb_, b_bglu], [b_ge1])
                    yi = (c * 2 + f2) % 2
                    S.op("dve", (lambda f2=f2, c=c, yi=yi: V.tensor_tensor(out=yst[yi], in0=ge1, in1=gTt[:, f2, c * 512:(c + 1) * 512], op=ALU.mult)), [b_ge1, b_gTt[f2]], [b_yst[yi]])
                    S.dma("pool", (lambda b=b, f2=f2, c=c, yi=yi: P.dma_start(out=ycat_d[b, :, f2, c * 512:(c + 1) * 512], in_=yst[yi])), [b_yst[yi]], nyc())
            need_bar["v"] = True

        S.barrier()

        while precast:
            precast.pop(0)()
        cf = Carver(ARF, ARF_N)
        OH_all = cf.take(64, 4); b_OH = S.buf("OH_all")
        GI_all = cf.take(64, 4); b_GI = S.buf("GI_all")
        POSf = cf.take(64); b_POSf = S.buf("POSf")
        GIDf = cf.take(NST); b_GIDf = S.buf("GIDf")
        ssC = cf.take(4); sdC = cf.take(4); b_ssC = [S.buf("ssC%d" % i) for i in range(4)]; b_sdC = [S.buf("sdC%d" % i) for i in range(4)]
        ef = cf.take(2); b_ef = [S.buf("ef0"), S.buf("ef1")]
        rdiag = cf.take(128); b_rdiag = S.buf("rdiag")
        gsl2 = [cf.take(8, 4), cf.take(8, 4)]; b_gsl2 = [S.buf("gsl0"), S.buf("gsl1")]
        f_persist = cf.o
        cntC = {"x": 0, "s": 0, "y": 0, "x3": 0, "r": 0}
        b_h2d = [S.buf("h2_d%d" % i) for i in range(4)]
        b_h2ss = [S.buf("h2s_d0"), S.buf("h2s_d1")]
        b_h2z = S.buf("h2s_zero")
        b_moe = S.buf("moe_d")

        cb = Carver(ARB, ARB_N)
        wout = cb.take(8, D); b_wout = S.buf("wout")
        yc = [cb.take(8, 512), cb.take(8, 512)]; b_yc = [S.buf("yc0"), S.buf("yc1")]
        xnC = [cb.take(D), cb.take(D)]; b_xnC = [S.buf("xnC0"), S.buf("xnC1")]
        xnT = [cb.take(8, 128), cb.take(8, 128)]; b_xnT = [S.buf("xnT0"), S.buf("xnT1")]
        rowbuf = [cb.take(ROWW), cb.take(ROWW)]; b_rowbuf = [S.buf("rowbuf0"), S.buf("rowbuf1")]
        wrb = cb.take(8, 20); b_wrb = S.buf("wrb")
        swr = cb.take(8, 20); b_swr = S.buf("swr")
        onesb = cb.take(128); b_onesb = S.buf("onesb")
        zrow = cb.take(8, ROWW); b_zrow = S.buf("zrow")
        xtC = [cf.take(D), cf.take(D)]; b_xtC = [S.buf("xtC0"), S.buf("xtC1")]
        A2b = cf.take(D); b_A2b = S.buf("A2b")
        S2b = cf.take(D); b_S2b = S.buf("S2b")
        G1b = cf.take(D); b_G1b = S.buf("G1b")
        rbb = cf.take(20); b_rbb = S.buf("rbb")
        lg = cf.take(8, 20); b_lg = S.buf("lg")
        NR_ = 18
        rsm = cf.take(NR_, 8, 4); b_rsm = [S.buf("rsm%d" % i) for i in range(NR_)]
        lgp = pbank[0][:, 0:160].rearrange("p (t e) -> p t e", e=20)
        b_lgp = b_pb[0]

        S.op("dve", lambda: V.memset(zrow, 0.0), [], [b_zrow])
        for s_ in range(NST):
            S.dma("sp", (lambda s_=s_: nc.sync.dma_start(out=h2s_d[s_ * TSZ:(s_ + 1) * TSZ, :].rearrange("(a p) c -> p a c", p=128), in_=zrow[:, 0:4, :])), [b_zrow], b_h2z, par=True)
        S.op("dve", lambda: V.tensor_copy(out=onesb, in_=onesf), [b_cst], [b_onesb])

        (GM, GE, GS, GP, OHG, EIN, ET, M1, MK1, E2, M2, MK2, DL, W1, W2, GI, GT) = range(17)

        def rs(i, n=4):
            return rsm[:, i, :, 0:n]

        def rop(fn, reads, writes, extra_r=(), extra_w=()):
            S.op("dve", fn, [b_rsm[i] for i in reads] + list(extra_r), [b_rsm[i] for i in writes] + list(extra_w))

        def bc4(ap1):
            return ap1.broadcast_to([128, 8, 4])

        def router_batch(sc):
            T0 = sc * 8
            S.op("dve", (lambda: V.tensor_tensor(out=lg, in0=lgp, in1=rbb.unsqueeze(1).broadcast_to([128, 8, 20]), op=ALU.add)), [b_lgp, b_rbb], [b_lg])
            rop((lambda: V.tensor_reduce(out=rs(GM, 1), in_=lg[:, :, 0:4], axis=AX.X, op=ALU.max)), [], [GM], [b_lg])
            rop((lambda: V.tensor_tensor(out=rs(GE), in0=lg[:, :, 0:4], in1=bc4(rs(GM, 1)), op=ALU.subtract)), [GM], [GE], [b_lg])
            S.op("act", (lambda: A.activation(out=rs(GE), in_=rs(GE), func=AF.Exp)), [b_rsm[GE]], [b_rsm[GE]])
            rop((lambda: V.tensor_reduce(out=rs(GS, 1), in_=rs(GE), axis=AX.X, op=ALU.add)), [GE], [GS])
            rop((lambda: V.reciprocal(out=rs(GP, 1), in_=rs(GS, 1))), [GS], [GP])
            rop((lambda: V.tensor_tensor(out=OH_all[:, T0:T0 + 8, :], in0=lg[:, :, 0:4], in1=bc4(rs(GM, 1)), op=ALU.is_equal)), [GM], [], [b_lg], [b_OH])
            ohv = OH_all[:, T0:T0 + 8, :]
            rop((lambda: V.tensor_tensor(out=rs(EIN), in0=lg[:, :, 4:8], in1=bc4(ohv[:, :, 0:1]), op=ALU.mult)), [], [EIN], [b_lg, b_OH])
            for g in range(1, 4):
                rop((lambda g=g: V.tensor_tensor(out=rs(ET), in0=lg[:, :, 4 + 4 * g: 8 + 4 * g], in1=bc4(ohv[:, :, g:g + 1]), op=ALU.mult)), [], [ET], [b_lg, b_OH])
                rop((lambda: V.tensor_tensor(out=rs(EIN), in0=rs(EIN), in1=rs(ET), op=ALU.add)), [EIN, ET], [EIN])
            rop((lambda: V.tensor_reduce(out=rs(M1, 1), in_=rs(EIN), axis=AX.X, op=ALU.max)), [EIN], [M1])
            rop((lambda: V.tensor_tensor(out=rs(MK1), in0=rs(EIN), in1=bc4(rs(M1, 1)), op=ALU.is_equal)), [EIN, M1], [MK1])
            rop((lambda: V.scalar_tensor_tensor(out=rs(E2), in0=rs(MK1), scalar=-1e30, in1=rs(EIN), op0=ALU.mult, op1=ALU.add)), [MK1, EIN], [E2])
            rop((lambda: V.tensor_reduce(out=rs(M2, 1), in_=rs(E2), axis=AX.X, op=ALU.max)), [E2], [M2])
            rop((lambda: V.tensor_tensor(out=rs(MK2), in0=rs(E2), in1=bc4(rs(M2, 1)), op=ALU.is_equal)), [E2, M2], [MK2])
            rop((lambda: V.tensor_tensor(out=rs(DL, 1), in0=rs(M2, 1), in1=rs(M1, 1), op=ALU.subtract)), [M1, M2], [DL])
            S.op("act", (lambda: A.activation(out=rs(DL, 1), in_=rs(DL, 1), func=AF.Exp)), [b_rsm[DL]], [b_rsm[DL]])
            rop((lambda: V.tensor_scalar(out=rs(W1, 1), in0=rs(DL, 1), scalar1=1.0, scalar2=None, op0=ALU.add)), [DL], [W1])
            rop((lambda: V.reciprocal(out=rs(W1, 1), in_=rs(W1, 1))), [W1], [W1])
            rop((lambda: V.tensor_tensor(out=rs(W2, 1), in0=rs(DL, 1), in1=rs(W1, 1), op=ALU.mult)), [DL, W1], [W2])
            rop((lambda: V.tensor_tensor(out=rs(W1, 1), in0=rs(W1, 1), in1=rs(GP, 1), op=ALU.mult)), [W1, GP], [W1])
            rop((lambda: V.tensor_tensor(out=rs(W2, 1), in0=rs(W2, 1), in1=rs(GP, 1), op=ALU.mult)), [W2, GP], [W2])
            rop((lambda: V.tensor_tensor(out=rs(GI), in0=rs(MK1), in1=bc4(rs(W1, 1)), op=ALU.mult)), [MK1, W1], [GI])
            rop((lambda: V.tensor_tensor(out=rs(GT), in0=rs(MK2), in1=bc4(rs(W2, 1)), op=ALU.mult)), [MK2, W2], [GT])
            rop((lambda: V.tensor_tensor(out=GI_all[:, T0:T0 + 8, :], in0=rs(GI), in1=rs(GT), op=ALU.add)), [GI, GT], [], (), [b_GI])

        def bcast_rows(gt0, b, Gd, bG):
            for k in range(8):
                S.op("dve", (lambda gt0=gt0, k=k, b=b: V.tensor_scalar(out=rdiag, in0=identf, scalar1=modT[:, gt0 + k, b:b + 1], scalar2=None, op0=ALU.mult)), [b_cst, b_modT], [b_rdiag])
                pb_, bpb_ = zbank()
                S.op("pe", (lambda pb_=pb_: T.matmul(pb_[:, 0:128], lhsT=onesf, rhs=rdiag, start=True, stop=True)), [b_cst, b_rdiag], [bpb_])
                S.op("act", (lambda pb_=pb_, Gd=Gd, k=k: A.copy(out=Gd[:, k * 128:(k + 1) * 128], in_=pb_[:, 0:128])), [bpb_], [bG])

        def bcast_rows_tab(tab, b_tab, b, Gd, bG):
            for k in range(8):
                S.op("dve", (lambda k=k, b=b: V.tensor_scalar(out=rdiag, in0=identf, scalar1=tab[:, k, b:b + 1], scalar2=None, op0=ALU.mult)), [b_cst, b_tab], [b_rdiag])
                pb_, bpb_ = zbank()
                S.op("pe", (lambda pb_=pb_: T.matmul(pb_[:, 0:128], lhsT=onesf, rhs=rdiag, start=True, stop=True)), [b_cst, b_rdiag], [bpb_])
                S.op("act", (lambda pb_=pb_, Gd=Gd, k=k: A.copy(out=Gd[:, k * 128:(k + 1) * 128], in_=pb_[:, 0:128])), [bpb_], [bG])

        def batch_prep(b):
            S.dma("sp", (lambda b=b: nc.sync.dma_start(out=G1b, in_=modrow_d[b, 2 * D:3 * D].partition_broadcast(128))), [b_modrow], b_G1b)
            S.dma("sp", (lambda b=b: nc.sync.dma_start(out=A2b, in_=a2row_d[b].partition_broadcast(128))), [b_modrow], b_A2b)
            S.dma("sp", (lambda b=b: nc.sync.dma_start(out=S2b, in_=modrow_d[b, 3 * D:4 * D].partition_broadcast(128))), [b_modrow], b_S2b)
            S.dma("pool", lambda: P.dma_start(out=wout, in_=wout_d.rearrange("(k p) n -> p k n", p=128)), [], b_wout)
            for k in range(8):
                S.op("dve", (lambda k=k: V.tensor_tensor(out=wout[:, k, :], in0=wout[:, k, :], in1=G1b, op=ALU.mult)), [b_wout, b_G1b], [b_wout])
            for k in range(8):
                S.op("dve", (lambda k=k, b=b: V.tensor_scalar(out=wrb[:, k, :], in0=wr[:, k, :], scalar1=A2T[:, k, b:b + 1], scalar2=None, op0=ALU.mult)), [b_wr, b_A2T], [b_wrb])
                S.op("dve", (lambda k=k, b=b: V.tensor_scalar(out=swr[:, k, :], in0=wr[:, k, :], scalar1=S2T[:, k, b:b + 1], scalar2=None, op0=ALU.mult)), [b_wr, b_modT], [b_swr])
            pb_, bpb_ = zbank()
            for k in range(8):
                S.op("pe", (lambda pb_=pb_, k=k: T.matmul(pb_[:, 0:20], lhsT=onesb, rhs=swr[:, k, :], start=(k == 0), stop=(k == 7))), [b_onesb, b_swr], [bpb_])
            S.op("dve", (lambda pb_=pb_: V.tensor_tensor(out=rbb, in0=pb_[:, 0:20], in1=rbias[:, :], op=ALU.add)), [bpb_, b_rbias], [b_rbb])

        def part1A(sc, t):
            b = sc // 2; half = sc % 2
            tok0 = sc * 1024 + t * 128
            col = half * 1024 + t * 128
            if t % 4 == 0:
                cntC["y"] += 1
                yi = cntC["y"] % 2
                S.dma("sp", (lambda b=b, col=col, yi=yi: nc.sync.dma_start(out=yc[yi], in_=ycat_d[b, :, :, col:col + 512])), b_ycats, b_yc[yi])
            yi = cntC["y"] % 2
            tc_ = (t % 4) * 128
            i = cntC["x"] % 2; cntC["x"] += 1
            S.dma("sp", (lambda i=i, tok0=tok0: nc.sync.dma_start(out=xtC[i], in_=x_d[tok0:tok0 + 128, :])), [], b_xtC[i])
            for hf in range(2):
                pb_, bpb_ = zbank()
                for k in range(8):
                    S.op("pe", (lambda pb_=pb_, k=k, hf=hf, yi=yi, tc_=tc_: T.matmul(pb_[:, :], lhsT=yc[yi][:, k, tc_:tc_ + 128], rhs=wout[:, k, hf * 512:(hf + 1) * 512], start=(k == 0), stop=(k == 7))), [b_yc[yi], b_wout], [bpb_])
                S.op("dve", (lambda pb_=pb_, hf=hf, i=i: V.tensor_tensor(out=xtC[i][:, hf * 512:(hf + 1) * 512], in0=pb_[:, :], in1=xtC[i][:, hf * 512:(hf + 1) * 512], op=ALU.add)), [bpb_, b_xtC[i]], [b_xtC[i]])
            S.dma("pool", (lambda i=i, tok0=tok0: P.dma_start(out=x1_d[tok0:tok0 + 128, :], in_=xtC[i])), [b_xtC[i]], b_x1ds[t % 4])
            return (i, tok0)

        def part1A2(sc, t, st1):
            i, tok0 = st1
            j = cntC["s"] % 4; cntC["s"] += 1
            xq = xnC[i]; bxq = b_xnC[i]
            S.op("act", (lambda i=i, j=j, xq=xq: A.activation(out=xq, in_=xtC[i], func=AF.Square, accum_out=ssC[:, j:j + 1])), [b_xtC[i]], [bxq, b_ssC[j]])
            S.op("act", (lambda j=j: A.activation(out=sdC[:, j:j + 1], in_=ssC[:, j:j + 1], func=AF.Sqrt, scale=1.0 / D, bias=epsc)), [b_ssC[j], b_cst], [b_sdC[j]])
            S.op("dve", (lambda j=j: V.reciprocal(out=sdC[:, j:j + 1], in_=sdC[:, j:j + 1])), [b_sdC[j]], [b_sdC[j]])
            S.op("act", (lambda i=i, j=j, xq=xq: A.activation(out=xq, in_=xtC[i], func=AF.Copy, scale=sdC[:, j:j + 1])), [b_xtC[i], b_sdC[j]], [bxq])
            rb_ = rowbuf[i]; brb_ = b_rowbuf[i]
            S.op("dve", (lambda xq=xq, rb_=rb_: V.tensor_tensor(out=rb_[:, 0:D], in0=xq, in1=A2b, op=ALU.mult)), [bxq, b_A2b], [brb_])
            S.op("pool", (lambda rb_=rb_: P.tensor_tensor(out=rb_[:, 0:D], in0=rb_[:, 0:D], in1=S2b, op=ALU.add)), [brb_, b_S2b], [brb_])
            S.dma("pool", (lambda rb_=rb_, tok0=tok0: P.dma_start(out=h2_d[tok0:tok0 + 128, :], in_=rb_[:, 0:D])), [brb_], b_h2d[t % 4])
            return (i, xq, bxq)

        def part1B(sc, t, st_):
            i, xq, bxq = st_
            for k in range(8):
                S.op("pe", (lambda k=k, xq=xq: T.transpose(out=pT[:, k, :], in_=xq[:, k * 128:(k + 1) * 128], identity=identb[:])), [bxq, b_identb], [b_pT])
            S.op("act", (lambda i=i: A.copy(out=xnT[i], in_=pT[:, :, :])), [b_pT], [b_xnT[i]])
            for k in range(8):
                S.op("pe", (lambda k=k, t=t, i=i: T.matmul(lgp[:, t, :], lhsT=xnT[i][:, k, :], rhs=wrb[:, k, :], start=(k == 0), stop=(k == 7))), [b_xnT[i], b_wrb], [b_lgp])

        NSC = 2 * NB
        for sc in range(NSC):
            if sc % 2 == 0:
                batch_prep(sc // 2)
            prev = None
            for t in range(8):
                st1 = part1A(sc, t)
                if prev is not None:
                    part1B(sc, t - 1, prev)
                prev = part1A2(sc, t, st1)
            part1B(sc, 7, prev)
            router_batch(sc)

        WITH = cf.take(64, 4); b_WITH = S.buf("WITH")
        TOT = cf.take(64, 4); b_TOT = S.buf("TOT")
        CUM = cf.take(64, 4); b_CUM = S.buf("CUM")
        ones64 = cf.take(64); b_ones64 = S.buf("ones64")
        tmpN = cf.take(NST); b_tmpN = S.buf("tmpN")
        NT = cf.take(4); b_NT = S.buf("NT")
        EE = cf.take(4); b_EE = S.buf("EE")
        BASE = cf.take(4); b_BASE = S.buf("BASE")
        OHf = OH_all.rearrange("p a b -> p (a b)")
        S.op("pe", lambda: T.matmul(pbank[1][:, 0:256], lhsT=lstf, rhs=OHf, start=True, stop=True), [b_cst, b_OH], [b_pb[1]])
        S.op("pe", lambda: T.matmul(pbank[2][:, 0:256], lhsT=onesf, rhs=OHf, start=True, stop=True), [b_cst, b_OH], [b_pb[2]])
        S.op("dve", lambda: V.tensor_copy(out=WITH.rearrange("p a b -> p (a b)"), in_=pbank[1][:, 0:256]), [b_pb[1]], [b_WITH])
        S.op("dve", lambda: V.tensor_copy(out=TOT.rearrange("p a b -> p (a b)"), in_=pbank[2][:, 0:256]), [b_pb[2]], [b_TOT])
        S.op("dve", lambda: V.memset(ones64, 1.0), [], [b_ones64])
        for g in range(4):
            S.op("dve", (lambda g=g: V.tensor_tensor_scan(out=CUM[:, :, g], data0=ones64, data1=TOT[:, :, g], initial=0.0, op0=ALU.mult, op1=ALU.add)), [b_ones64, b_TOT], [b_CUM])
        for g in range(4):
            S.op("dve", (lambda g=g: V.tensor_scalar(out=tmpN, in0=thrf, scalar1=CUM[:, 63, g:g + 1], scalar2=None, op0=ALU.is_lt)), [b_cst, b_CUM], [b_tmpN])
            S.op("dve", (lambda g=g: V.tensor_reduce(out=NT[:, g:g + 1], in_=tmpN, axis=AX.X, op=ALU.add)), [b_tmpN], [b_NT])
        S.op("dve", lambda: V.tensor_scalar(out=EE[:, 0:1], in0=NT[:, 0:1], scalar1=float(TSZ), scalar2=None, op0=ALU.mult), [b_NT], [b_EE])
        for g in range(1, 4):
            S.op("dve", (lambda g=g: V.scalar_tensor_tensor(out=EE[:, g:g + 1], in0=NT[:, g:g + 1], scalar=float(TSZ), in1=EE[:, g - 1:g], op0=ALU.mult, op1=ALU.add)), [b_NT, b_EE], [b_EE])
        S.op("dve", lambda: V.memset(BASE[:, 0:1], 0.0), [], [b_BASE])
        S.op("dve", lambda: V.tensor_copy(out=BASE[:, 1:4], in_=EE[:, 0:3]), [b_EE], [b_BASE])
        S.op("dve", lambda: V.tensor_tensor(out=CUM, in0=CUM, in1=TOT, op=ALU.subtract), [b_CUM, b_TOT], [b_CUM])
        S.op("dve", lambda: V.tensor_tensor(out=CUM, in0=CUM, in1=WITH, op=ALU.add), [b_CUM, b_WITH], [b_CUM])
        S.op("dve", lambda: V.tensor_tensor(out=CUM, in0=CUM, in1=BASE.unsqueeze(1).broadcast_to([128, 64, 4]), op=ALU.add), [b_CUM, b_BASE], [b_CUM])
        S.op("dve", lambda: V.tensor_tensor(out=CUM, in0=CUM, in1=OH_all, op=ALU.mult), [b_CUM, b_OH], [b_CUM])
        S.op("dve", lambda: V.tensor_reduce(out=POSf, in_=CUM, axis=AX.X, op=ALU.add), [b_CUM], [b_POSf])
        S.op("dve", lambda: V.tensor_copy(out=posi[:, :], in_=POSf), [b_POSf], [b_posi])
        S.op("dve", lambda: V.tensor_scalar(out=GIDf, in0=thrf, scalar1=EE[:, 0:1], scalar2=None, op0=ALU.is_ge), [b_cst, b_EE], [b_GIDf])
        for g in range(1, 3):
            S.op("dve", (lambda g=g: V.tensor_scalar(out=tmpN, in0=thrf, scalar1=EE[:, g:g + 1], scalar2=None, op0=ALU.is_ge)), [b_cst, b_EE], [b_tmpN])
            S.op("dve", lambda: V.tensor_tensor(out=GIDf, in0=GIDf, in1=tmpN, op=ALU.add), [b_GIDf, b_tmpN], [b_GIDf])

        for Tt in range(64):
            i = Tt % 2
            rb_ = rowbuf[i]; brb_ = b_rowbuf[i]
            S.dma("sp", (lambda rb_=rb_, Tt=Tt: nc.sync.dma_start(out=rb_[:, 0:D], in_=h2_d[Tt * 128:(Tt + 1) * 128, :])), b_h2d, brb_)
            S.op("dve", (lambda rb_=rb_, Tt=Tt: V.tensor_copy(out=rb_[:, D:D + 4], in_=GI_all[:, Tt, :])), [b_GI, brb_], [brb_])
            S.dma("pool", (lambda rb_=rb_, Tt=Tt: P.indirect_dma_start(out=h2s_d[:, :], out_offset=bass.IndirectOffsetOnAxis(ap=posi[:, Tt:Tt + 1], axis=0), in_=rb_, in_offset=None)), [brb_, b_posi, b_h2z], b_h2ss[i])

        S.barrier()

        cb = Carver(ARB, ARB_N); cf = Carver(ARF, ARF_N); cf.o = f_persist
        rows8 = cb.take(4, ROWW); b_rows8 = S.buf("rows8")
        h2sT = [cb.take(8, TSZ), cb.take(8, TSZ)]
        b_h2sT = [[S.buf("h2sT%d_%d" % (p, i)) for i in range(4)] for p in range(2)]
        acc = cb.take(4, D); b_acc = [S.buf("acc_%d" % i) for i in range(4)]
        w1s = [cb.take(8, 512), cb.take(8, 512)]; w3s = [cb.take(8, 512), cb.take(8, 512)]; w2s = [cb.take(4, D), cb.take(4, D)]
        b_w1s = [S.buf("w1s0"), S.buf("w1s1")]; b_w3s = [S.buf("w3s0"), S.buf("w3s1")]; b_w2s = [S.buf("w2s0"), S.buf("w2s1")]
        heT = cb.take(4, 512); b_heT = S.buf("heT")
        sil = [cb.take(512), cb.take(512)]; b_sil = [S.buf("sil0"), S.buf("sil1")]
        w1rows = w1b_d.rearrange("e p k n -> (e p) (k n)")
        w3rows = w3b_d.rearrange("e p k n -> (e p) (k n)")
        w2rows = w2b_d.rearrange("e p k n -> (e p) (k n)")

        def load_w(s_, i_):
            wi = (s_ * 4 + i_) % 2
            S.op("dve", (lambda s_=s_, i_=i_, wi=wi: V.tensor_scalar(out=ef[:, wi:wi + 1], in0=GIDf[:, s_:s_ + 1], scalar1=512.0, scalar2=float(i_ * 128), op0=ALU.mult, op1=ALU.add)), [b_GIDf], [b_ef[wi]])
            S.op("dve", (lambda wi=wi: V.tensor_tensor(out=ef[:, wi:wi + 1], in0=ef[:, wi:wi + 1], in1=iotap, op=ALU.add)), [b_ef[wi], b_cst], [b_ef[wi]])
            S.op("dve", (lambda wi=wi: V.tensor_copy(out=widx[:, wi:wi + 1], in_=ef[:, wi:wi + 1])), [b_ef[wi]], [b_widx[wi]])
            S.dma("pool", (lambda wi=wi: P.indirect_dma_start(out=w1s[wi].rearrange("p k n -> p (k n)"), out_offset=None, in_=w1rows, in_offset=bass.IndirectOffsetOnAxis(ap=widx[:, wi:wi + 1], axis=0))), [b_widx[wi], b_wb], b_w1s[wi])
            S.dma("pool", (lambda wi=wi: P.indirect_dma_start(out=w3s[wi].rearrange("p k n -> p (k n)"), out_offset=None, in_=w3rows, in_offset=bass.IndirectOffsetOnAxis(ap=widx[:, wi:wi + 1], axis=0))), [b_widx[wi], b_wb], b_w3s[wi])
            S.dma("pool", (lambda wi=wi: P.indirect_dma_start(out=w2s[wi].rearrange("p k n -> p (k n)"), out_offset=None, in_=w2rows, in_offset=bass.IndirectOffsetOnAxis(ap=widx[:, wi:wi + 1], axis=0))), [b_widx[wi], b_wb], b_w2s[wi])

        def load_rows(s_):
            pp = s_ % 2
            S.dma("sp", (lambda s_=s_: nc.sync.dma_start(out=rows8, in_=h2s_d[s_ * TSZ:(s_ + 1) * TSZ, :].rearrange("(a p) c -> p a c", p=128))), b_h2ss, b_rows8)
            S.op("dve", (lambda pp=pp: V.tensor_copy(out=gsl2[pp][:, 0:4, :], in_=rows8[:, :, D:D + 4])), [b_rows8], [b_gsl2[pp]])
            for sub in range(4):
                for k in range(8):
                    S.op("pe", (lambda sub=sub, k=k: T.transpose(out=pT[:, k, :], in_=rows8[:, sub, k * 128:(k + 1) * 128], identity=identb[:])), [b_rows8, b_identb], [b_pT])
                if sub % 2 == 0:
                    S.op("act", (lambda sub=sub, pp=pp: A.copy(out=h2sT[pp][:, :, sub * 128:(sub + 1) * 128], in_=pT[:, :, :])), [b_pT], [b_h2sT[pp][sub]])
                else:
                    S.op("dve", (lambda sub=sub, pp=pp: V.tensor_copy(out=h2sT[pp][:, :, sub * 128:(sub + 1) * 128], in_=pT[:, :, :])), [b_pT], [b_h2sT[pp][sub]])

        def expert_tile(s_, i_):
            pp = s_ % 2
            wi = (s_ * 4 + i_) % 2
            for c in range(1):
                for dd in range(4):
                    p1, bp1 = zbank()
                    p3, bp3 = zbank()
                    for k in range(8):
                        S.op("pe", (lambda p1=p1, k=k, dd=dd, c=c, wi=wi, pp=pp: T.matmul(p1[:, :], lhsT=w1s[wi][:, k, dd * 128:(dd + 1) * 128], rhs=h2sT[pp][:, k, c * 512:(c + 1) * 512], start=(k == 0), stop=(k == 7))), [b_w1s[wi]] + b_h2sT[pp][c * 4:(c + 1) * 4], [bp1])
                    for k in range(8):
                        S.op("pe", (lambda p3=p3, k=k, dd=dd, c=c, wi=wi, pp=pp: T.matmul(p3[:, :], lhsT=w3s[wi][:, k, dd * 128:(dd + 1) * 128], rhs=h2sT[pp][:, k, c * 512:(c + 1) * 512], start=(k == 0), stop=(k == 7))), [b_w3s[wi]] + b_h2sT[pp][c * 4:(c + 1) * 4], [bp3])
                    si = dd % 2
                    S.op("act", (lambda p1=p1, si=si: A.activation(out=sil[si], in_=p1[:, :], func=AF.Silu)), [bp1], [b_sil[si]])
                    S.op("dve", (lambda p3=p3, si=si, dd=dd: V.tensor_tensor(out=heT[:, dd, :], in0=sil[si], in1=p3[:, :], op=ALU.mult)), [b_sil[si], bp3], [b_heT])
                for tt_ in range(4):
                    t = c * 4 + tt_
                    for hf in range(2):
                        po, bpo = zbank()
                        for dd in range(4):
                            S.op("pe", (lambda po=po, dd=dd, tt_=tt_, hf=hf, wi=wi: T.matmul(po[:, :], lhsT=heT[:, dd, tt_ * 128:(tt_ + 1) * 128], rhs=w2s[wi][:, dd, hf * 512:(hf + 1) * 512], start=(dd == 0), stop=(dd == 3))), [b_heT, b_w2s[wi]], [bpo])
                        if i_ == 0:
                            S.op("dve", (lambda po=po, t=t, hf=hf, i_=i_, pp=pp: V.tensor_scalar(out=acc[:, t, hf * 512:(hf + 1) * 512], in0=po[:, :], scalar1=gsl2[pp][:, t, i_:i_ + 1], scalar2=None, op0=ALU.mult)), [bpo, b_gsl2[pp]], [b_acc[t]])
                        else:
                            S.op("dve", (lambda po=po, t=t, hf=hf, i_=i_, pp=pp: V.scalar_tensor_tensor(out=acc[:, t, hf * 512:(hf + 1) * 512], in0=po[:, :], scalar=gsl2[pp][:, t, i_:i_ + 1], in1=acc[:, t, hf * 512:(hf + 1) * 512], op0=ALU.mult, op1=ALU.add)), [bpo, b_gsl2[pp], b_acc[t]], [b_acc[t]])

        load_w(0, 0)
        load_rows(0)
        for s_ in range(NST):
            for i_ in range(4):
                nxt = s_ * 4 + i_ + 1
                if nxt < NST * 4:
                    load_w(nxt // 4, nxt % 4)
                expert_tile(s_, i_)
                if i_ == 1 and s_ + 1 < NST:
                    load_rows(s_ + 1)
            S.dma("sp", (lambda s_=s_: nc.sync.dma_start(out=moe_d[s_ * TSZ:(s_ + 1) * TSZ, :].rearrange("(a p) c -> p a c", p=128), in_=acc)), b_acc, b_moe)

        S.barrier()

        cb = Carver(ARB, ARB_N); cf = Carver(ARF, ARF_N); cf.o = f_persist
        mrow = [cb.take(D), cb.take(D), cb.take(D)]; b_mrow = [S.buf("mrow0"), S.buf("mrow1"), S.buf("mrow2")]
        x3t = [cf.take(D), cf.take(D), cf.take(D)]; b_x3t = [S.buf("x3t0"), S.buf("x3t1"), S.buf("x3t2")]
        t1s = [cf.take(D), cf.take(D)]; b_t1s = [S.buf("t1_0"), S.buf("t1_1")]
        G2b = cf.take(D); b_G2b = S.buf("G2b")
        FGb = cf.take(D); b_FGb = S.buf("FGb")
        S.dma("sp", lambda: nc.sync.dma_start(out=FGb, in_=fg_d.partition_broadcast(128)), [], b_FGb)
        def c5_fetch(Tt):
            i = Tt % 3
            tok0 = Tt * 128
            S.dma("pool", (lambda i=i, Tt=Tt: P.indirect_dma_start(out=mrow[i], out_offset=None, in_=moe_d[:, :], in_offset=bass.IndirectOffsetOnAxis(ap=posi[:, Tt:Tt + 1], axis=0))), [b_moe, b_posi], b_mrow[i])
            S.dma("sp", (lambda i=i, tok0=tok0: nc.sync.dma_start(out=x3t[i], in_=x1_d[tok0:tok0 + 128, :])), b_x1ds, b_x3t[i])

        G2bs = [G2b, t1s[0]]
        c5_fetch(0)
        for Tt in range(64):
            b = Tt // 16
            if Tt % 16 == 0:
                S.dma("sp", (lambda b=b: nc.sync.dma_start(out=G2b, in_=modrow_d[b, 5 * D:6 * D].partition_broadcast(128))), [b_modrow], b_G2b)
            if Tt + 1 < 64:
                c5_fetch(Tt + 1)
            tok0 = Tt * 128
            i = Tt % 3
            t1 = t1s[Tt % 2]; b_t1 = b_t1s[Tt % 2]
            S.op("dve", (lambda i=i, t1=t1: V.tensor_tensor(out=t1, in0=mrow[i], in1=G2b, op=ALU.mult)), [b_mrow[i], b_G2b], [b_t1])
            S.op("dve", (lambda i=i, t1=t1: V.tensor_tensor(out=x3t[i], in0=t1, in1=x3t[i], op=ALU.add)), [b_t1, b_x3t[i]], [b_x3t[i]])
            j = cntC["s"] % 4; cntC["s"] += 1
            S.op("act", (lambda i=i, j=j, t1=t1: A.activation(out=t1, in_=x3t[i], func=AF.Square, accum_out=ssC[:, j:j + 1])), [b_x3t[i]], [b_t1, b_ssC[j]])
            S.op("act", (lambda j=j: A.activation(out=sdC[:, j:j + 1], in_=ssC[:, j:j + 1], func=AF.Sqrt, scale=1.0 / D, bias=epsc)), [b_ssC[j], b_cst], [b_sdC[j]])
            S.op("dve", (lambda j=j: V.reciprocal(out=sdC[:, j:j + 1], in_=sdC[:, j:j + 1])), [b_sdC[j]], [b_sdC[j]])
            S.op("act", (lambda i=i, j=j, t1=t1: A.activation(out=t1, in_=x3t[i], func=AF.Copy, scale=sdC[:, j:j + 1])), [b_x3t[i], b_sdC[j]], [b_t1])
            S.op("dve", (lambda i=i, t1=t1: V.tensor_tensor(out=x3t[i], in0=t1, in1=FGb, op=ALU.mult)), [b_t1, b_FGb], [b_x3t[i]])
            S.dma("sp", (lambda i=i, tok0=tok0: nc.sync.dma_start(out=out_d[tok0:tok0 + 128, :], in_=x3t[i])), [b_x3t[i]], b_outs[Tt % 4])

        S.final_waits.extend(b_outs)
        S.emit()
    return nc


_NC_CACHE = {}


def kernel(x, c, ctx, c_ctx, w_mod, b_mod, norm1_g, norm2_g, w_in, s5_lambda_re, s5_lambda_im,
           s5_log_dt, s5_b_re, s5_b_im, s5_c_re, s5_c_im, s5_d, w_glu, b_glu, conv_w, w_out,
           router_group_w, router_group_b, router_expert_w, router_expert_b,
           expert_w1, expert_w3, expert_w2, final_g):
    f = lambda a: np.ascontiguousarray(np.asarray(a, dtype=np.float32))
    if "nc" not in _NC_CACHE:
        _NC_CACHE["nc"] = build()
    nc = _NC_CACHE["nc"]
    x = f(x); ctx = f(ctx); c = f(c); c_ctx = f(c_ctx)
    shared = {
        "w_mod": f(w_mod[0]), "b_mod": f(b_mod[0]), "norm1_g": f(norm1_g[0]), "norm2_g": f(norm2_g[0]),
        "w_in": f(w_in[0]), "lam_re": f(s5_lambda_re[0]), "lam_im": f(s5_lambda_im[0]),
        "log_dt": f(s5_log_dt[0]).reshape(32), "b_re": f(s5_b_re[0]), "b_im": f(s5_b_im[0]),
        "c_re": f(s5_c_re[0]).reshape(512, 64), "c_im": f(s5_c_im[0]).reshape(512, 64),
        "s5_d": f(s5_d[0]), "w_glu": f(w_glu[0]), "b_glu": f(b_glu[0]), "conv_w": f(conv_w[0]),
        "w_out": f(w_out[0]), "rgw": f(router_group_w[0]), "rgb": f(router_group_b[0]),
        "rew": f(router_expert_w[0]), "reb": f(router_expert_b[0]),
        "w1": f(expert_w1[0]), "w3": f(expert_w3[0]), "w2": f(expert_w2[0]),
        "final_g": f(final_g), "consts": CONSTS, "gsel": GSEL,
    }
    in_maps = []
    for i in range(8):
        m = dict(shared)
        m["x"] = x[i * NB:(i + 1) * NB].reshape(NB * SEQ, D)
        m["ctx"] = ctx[i * NB:(i + 1) * NB].reshape(NB * NCTX, D)
        m["cond"] = np.concatenate([c[i * NB:(i + 1) * NB], c_ctx[None, :]], axis=0)
        in_maps.append(m)
    res = run_bass_kernel_spmd(nc, in_maps, core_ids=list(range(8)))
    outs = [np.asarray(r["out"]).reshape(NB, SEQ, D) for r in res.results]
    return np.concatenate(outs, axis=0).astype(np.float32)
```

```python
import contextlib
import math
import numpy as np
import concourse.bass as bass
import concourse.mybir as mybir
from concourse.bass_utils import run_bass_kernel_spmd

F32 = mybir.dt.float32
BF16 = mybir.dt.bfloat16
AF = mybir.ActivationFunctionType
ALU = mybir.AluOpType
AX = mybir.AxisListType
ENGS = ("pe", "act", "dve", "pool", "sp")
NB = 4
SEQ = 2048
NCTX = 256
D = 1024
LTOT = SEQ + NCTX
NLEV = 9
NBLK = 288
NX = 320
TAIL = 47104
TSZ = 512
NST = 19
NSLOT = NST * TSZ
ROWW = 1040
I32 = mybir.dt.int32
ARB_N = 65536
ARF_N = 9472


class Buf:
    __slots__ = ("name", "writers", "readers", "sem", "dma_total", "_par", "_epoch")

    def __init__(self, name):
        self.name = name
        self.writers = []
        self.readers = []
        self.sem = None
        self.dma_total = 0
        self._par = False
        self._epoch = []


class Op:
    __slots__ = ("eng", "fn", "deps", "needs_inc", "count", "dma_buf", "dma_count")

    def __init__(self, eng, fn):
        self.eng = eng
        self.fn = fn
        self.deps = []
        self.needs_inc = False
        self.count = None
        self.dma_buf = None
        self.dma_count = None


class Sched:
    def __init__(self, nc, stack):
        self.nc = nc
        self.stack = stack
        self.ops = {e: [] for e in ENGS}
        self.bufs = []
        self.final_waits = []

    def buf(self, name):
        b = Buf(name)
        self.bufs.append(b)
        return b

    def _add(self, eng, fn, reads, writes, dma_buf=None, par=False):
        op = Op(eng, fn)
        seen = set()
        for b in reads:
            for d in b.writers:
                if id(d) not in seen:
                    seen.add(id(d)); op.deps.append(d)
        for b in writes:
            if par:
                if b.readers or not getattr(b, "_par", False):
                    b._epoch = b.writers + b.readers
                    b.writers = []
                    b.readers = []
                    b._par = True
                dl = b._epoch
            else:
                dl = b.writers + b.readers
                b._par = False
            for d in dl:
                if id(d) not in seen:
                    seen.add(id(d)); op.deps.append(d)
        for b in reads:
            b.readers.append(op)
        for b in writes:
            if par:
                b.writers.append(op)
            else:
                b.writers = [op]
                b.readers = []
        if dma_buf is not None:
            op.dma_buf = dma_buf
            if dma_buf.sem is None:
                dma_buf.sem = self.stack.enter_context(self.nc.semaphore("s_" + dma_buf.name))
            dma_buf.dma_total += 16
            op.dma_count = dma_buf.dma_total
        self.ops[eng].append(op)
        return op

    def op(self, eng, fn, reads=(), writes=()):
        return self._add(eng, fn, list(reads), list(writes))

    def dma(self, eng, fn, reads, write, par=False):
        return self._add(eng, fn, list(reads), [write], dma_buf=write, par=par)

    def barrier(self):
        nc = self.nc
        pend = []
        seen = set()
        for b in self.bufs:
            if b.name.endswith("_d") or "_d" in b.name and b.name.split("_d")[-1].isdigit() or b.name in ("wb_scr", "h2s_zero"):
                continue
            for d in b.writers + b.readers:
                if id(d) not in seen:
                    seen.add(id(d)); pend.append(d)
        engobj = {"pe": nc.tensor, "act": nc.scalar, "dve": nc.vector, "pool": nc.gpsimd, "sp": nc.sync}
        for e in ENGS:
            op = Op(e, (lambda e=e: engobj[e].nop()))
            op.deps = list(pend)
            self.ops[e].append(op)

    def emit(self):
        nc = self.nc
        for e in ENGS:
            for op in self.ops[e]:
                for d in op.deps:
                    if d.dma_buf is None and not (d.eng == "pe" and op.eng == "pe" and op.dma_buf is None):
                        d.needs_inc = True
        sems = {e: self.stack.enter_context(nc.semaphore("eng_" + e)) for e in ENGS}
        for e in ENGS:
            c = 0
            for op in self.ops[e]:
                if op.dma_buf is None and op.needs_inc:
                    c += 1
                    op.count = c
        final_waits = self.final_waits

        def run(e, eng):
            waited = {}
            for op in self.ops[e]:
                need = {}
                for d in op.deps:
                    if d.dma_buf is not None:
                        key = ("d", id(d.dma_buf)); v = d.dma_count; sem = d.dma_buf.sem
                    else:
                        if d.eng == "pe" and e == "pe" and op.dma_buf is None:
                            continue
                        key = ("e", d.eng); v = d.count; sem = sems[d.eng]
                    if key not in need or need[key][1] < v:
                        need[key] = (sem, v)
                for key, (sem, v) in need.items():
                    if waited.get(key, 0) >= v:
                        continue
                    waited[key] = v
                    eng.wait_ge(sem, v)
                ins = op.fn()
                if op.dma_buf is not None:
                    ins.then_inc(op.dma_buf.sem, 16)
                elif op.needs_inc:
                    ins.then_inc(sems[e], 1)
            if e == "sp":
                for b in final_waits:
                    eng.wait_ge(b.sem, b.dma_total)

        with nc.Block() as block:
            @block.tensor
            def _(eng):
                run("pe", eng)

            @block.scalar
            def _(eng):
                run("act", eng)

            @block.vector
            def _(eng):
                run("dve", eng)

            @block.gpsimd
            def _(eng):
                run("pool", eng)

            @block.sync
            def _(eng):
                run("sp", eng)


def make_consts():
    c = {}
    c["ident"] = np.eye(128, dtype=np.float32)
    isw = np.zeros((128, 128), np.float32)
    for p in range(128):
        isw[p, (p + 64) % 128] = 1.0
    c["iswap"] = isw
    c["ones"] = np.ones((128, 128), np.float32)
    sg = np.ones((128, 1), np.float32); sg[64:] = -1.0
    c["sgn"] = sg
    rm = np.zeros((128, 8), np.float32)
    for p in range(128):
        rm[p, p // 16] = 1.0
    c["rowmask"] = rm
    s8i = np.arange(128)[:, None] // 16
    t8i = np.arange(128)[None, :] // 16
    c["maskf"] = (s8i <= t8i).astype(np.float32)
    c["maskb"] = (s8i >= t8i).astype(np.float32)
    c["eps"] = np.full((128, 1), 1e-6, np.float32)
    c["lst"] = (np.arange(128)[:, None] < np.arange(128)[None, :]).astype(np.float32)
    c["thr"] = np.tile((np.arange(NST, dtype=np.float32) * float(TSZ))[None, :], (128, 1))
    c["iotap"] = np.arange(128, dtype=np.float32)[:, None]
    order = ["ident", "iswap", "ones", "sgn", "rowmask", "maskf", "maskb", "eps", "lst", "thr", "iotap"]
    offs = {}
    o = 0
    for k in order:
        offs[k] = (o, c[k].shape[1]); o += c[k].shape[1]
    return np.concatenate([c[k] for k in order], axis=1), offs


CONSTS, COFF = make_consts()


def make_gsel():
    g = np.zeros((128, 8, 240), np.float32)
    for g8 in range(8):
        for h in range(16):
            g[16 * g8 + h, g8, 112 + h] = 1.0
    return g.reshape(128, 8 * 240)


GSEL = make_gsel()


def build():
    nc = bass.Bass("TRN2", target_bir_lowering=False, dynamic_dma_scratch_size=8192)

    def din(name, shape):
        return nc.dram_tensor(name, list(shape), F32, kind="ExternalInput").ap()

    x_d = din("x", [NB * SEQ, D]); ctx_d = din("ctx", [NB * NCTX, D]); cond_d = din("cond", [NB + 1, D])
    wmod_d = din("w_mod", [D, 6 * D]); bmod_d = din("b_mod", [6 * D])
    n1g_d = din("norm1_g", [D]); n2g_d = din("norm2_g", [D]); win_d = din("w_in", [D, 2560])
    lre_d = din("lam_re", [2, 16, 64]); lim_d = din("lam_im", [2, 16, 64]); ldt_d = din("log_dt", [32])
    bre_d = din("b_re", [2, 16, 64, 16]); bim_d = din("b_im", [2, 16, 64, 16])
    cre_d = din("c_re", [512, 64]); cim_d = din("c_im", [512, 64])
    s5d_d = din("s5_d", [256]); wglu_d = din("w_glu", [256, 256]); bglu_d = din("b_glu", [256])
    cw_d = din("conv_w", [3, 768]); wout_d = din("w_out", [D, D])
    rgw_d = din("rgw", [D, 4]); rgb_d = din("rgb", [4]); rew_d = din("rew", [D, 16]); reb_d = din("reb", [16])
    w1_d = din("w1", [16, D, 512]); w3_d = din("w3", [16, D, 512]); w2_d = din("w2", [16, 512, D])
    fg_d = din("final_g", [D]); consts_d = din("consts", list(CONSTS.shape)); gsel_d = din("gsel", [128, 1920])
    out_d = nc.dram_tensor("out", [NB * SEQ, D], F32, kind="ExternalOutput").ap()
    ycat_d = nc.dram_tensor("ycat_scr", [NB, 128, 8, SEQ], BF16, kind="Internal").ap()
    x1_d = nc.dram_tensor("x1_scr", [NB * SEQ, D], F32, kind="Internal").ap()
    modrow_d = nc.dram_tensor("modrow_scr", [NB + 1, 6 * D], F32, kind="Internal").ap()
    a2row_d = nc.dram_tensor("a2row_scr", [NB + 1, D], F32, kind="Internal").ap()
    rot_d = nc.dram_tensor("rot_scr", [16, 128, 2 * NLEV, 128], BF16, kind="Internal").ap()
    w1b_d = nc.dram_tensor("w1b_scr", [16, 128, 8, 512], BF16, kind="Internal").ap()
    w3b_d = nc.dram_tensor("w3b_scr", [16, 128, 8, 512], BF16, kind="Internal").ap()
    w2b_d = nc.dram_tensor("w2b_scr", [16, 128, 4, D], BF16, kind="Internal").ap()
    h2_d = nc.dram_tensor("h2_scr", [NB * SEQ, D], BF16, kind="Internal").ap()
    h2s_d = nc.dram_tensor("h2s_scr", [NSLOT, ROWW], BF16, kind="Internal").ap()
    moe_d = nc.dram_tensor("moe_scr", [NSLOT, D], BF16, kind="Internal").ap()

    with contextlib.ExitStack() as st:
        S = Sched(nc, st)

        def sb(name, shape, dt=F32):
            return st.enter_context(nc.sbuf_tensor(name, list(shape), dt))

        def ps(name, shape, dt=F32):
            return st.enter_context(nc.psum_tensor(name, list(shape), dt))

        V, A, P, T = nc.vector, nc.scalar, nc.gpsimd, nc.tensor
        NCQ = dict(allow_slow_non_contiguous=True)

        cst = sb("cst", list(CONSTS.shape)); b_cst = S.buf("cst")
        identf = cst[:, COFF["ident"][0]:COFF["ident"][0] + 128]
        iswapf = cst[:, COFF["iswap"][0]:COFF["iswap"][0] + 128]
        onesf = cst[:, COFF["ones"][0]:COFF["ones"][0] + 128]
        sgn = cst[:, COFF["sgn"][0]:COFF["sgn"][0] + 1]
        rowmask = cst[:, COFF["rowmask"][0]:COFF["rowmask"][0] + 8]
        maskf = cst[:, COFF["maskf"][0]:COFF["maskf"][0] + 128]
        maskb = cst[:, COFF["maskb"][0]:COFF["maskb"][0] + 128]
        epsc = cst[:, COFF["eps"][0]:COFF["eps"][0] + 1]
        lstf = cst[:, COFF["lst"][0]:COFF["lst"][0] + 128]
        thrf = cst[:, COFF["thr"][0]:COFF["thr"][0] + NST]
        iotap = cst[:, COFF["iotap"][0]:COFF["iotap"][0] + 1]
        posi = sb("posi", [128, 64], I32); b_posi = S.buf("posi")
        zres = sb("zres", [128, ROWW], BF16); b_zres = S.buf("zres")
        b_h2z = S.buf("h2s_zero")
        S.op("pool", lambda: P.memset(zres[:, :], 0.0), [], [b_zres])
        zfill = list(range(NSLOT // 128))
        widx = sb("widx", [128, 2], I32); b_widx = [S.buf("widx0"), S.buf("widx1")]
        Gsel = sb("Gsel", [128, 8, 240], BF16); b_Gsel = S.buf("Gsel")
        identb = sb("identb", [128, 128], BF16); b_identb = S.buf("identb")
        modT = sb("modT", [128, 48, 8]); b_modT = S.buf("modT")
        A1T = sb("A1T", [128, 8, 8]); b_A1T = S.buf("A1T")
        A2T = sb("A2T", [128, 8, 8]); b_A2T = S.buf("A2T")
        gT12 = sb("gT12", [128, 2, 8]); b_gT12 = S.buf("gT12")
        rbias = sb("rbias", [128, 20]); b_rbias = S.buf("rbias")
        wr = sb("wr", [128, 8, 20], BF16); b_wr = S.buf("wr")
        cw = sb("cw", [128, 6, 3]); b_cw = S.buf("cw")
        dcol = sb("dcol", [128, 2]); b_dcol = S.buf("dcol")
        bglu = sb("bglu", [128, 2]); b_bglu = S.buf("bglu")
        wglu = sb("wglu", [128, 2, 256], BF16); b_wglu = S.buf("wglu")
        c1t = sb("c1t", [128, 32, NLEV]); b_c1t = S.buf("c1t")
        c2t = sb("c2t", [128, 32, NLEV]); b_c2t = S.buf("c2t")
        ss = sb("ss", [128, 4]); rstd = sb("rstd", [128, 4])
        b_ss = [S.buf("ss%d" % i) for i in range(4)]; b_rstd = [S.buf("rstd%d" % i) for i in range(4)]

        ARB = sb("arb", [128, ARB_N], BF16)
        ARF = sb("arf", [128, ARF_N], F32)

        class Carver:
            def __init__(self, t, n):
                self.t = t; self.n = n; self.o = 0

            def take(self, *shape):
                n = int(np.prod(shape))
                assert self.o + n <= self.n, ("arena overflow", self.o + n, self.n)
                ap = self.t[:, self.o:self.o + n]
                self.o += n
                if len(shape) == 2:
                    ap = ap.rearrange("p (a b) -> p a b", a=shape[0])
                elif len(shape) == 3:
                    ap = ap.rearrange("p (a b c) -> p a b c", a=shape[0], b=shape[1])
                return ap

        pbank = [ps("pb%d" % i, [128, 512]) for i in range(7)]
        b_pb = [S.buf("pb%d" % i) for i in range(7)]
        pT = ps("pT", [128, 8, 128], BF16); b_pT = S.buf("pT")

        S.dma("sp", lambda: nc.sync.dma_start(out=cst[:], in_=consts_d), [], b_cst)
        S.dma("pool", lambda: P.dma_start(out=identb[:], in_=consts_d[:, 0:128]), [], b_identb)
        S.dma("pool", lambda: P.dma_start(out=Gsel[:], in_=gsel_d.rearrange("p (a b) -> p a b", a=8)), [], b_Gsel)
        S.dma("sp", lambda: nc.sync.dma_start(out=rbias[:, 0:4], in_=rgb_d.partition_broadcast(128)), [], b_rbias)
        S.dma("sp", lambda: nc.sync.dma_start(out=rbias[:, 4:20], in_=reb_d.partition_broadcast(128)), [], b_rbias)
        S.dma("pool", lambda: P.dma_start(out=wr[:, :, 0:4], in_=rgw_d.rearrange("(k p) n -> p k n", p=128)), [], b_wr)
        S.dma("pool", lambda: P.dma_start(out=wr[:, :, 4:20], in_=rew_d.rearrange("(k p) n -> p k n", p=128)), [], b_wr)
        for tp in range(3):
            S.dma("sp", (lambda tp=tp: nc.sync.dma_start(out=cw[:, :, tp], in_=cw_d[tp].rearrange("(i p) -> p i", p=128), **NCQ)), [], b_cw)
        S.dma("sp", lambda: nc.sync.dma_start(out=dcol[:], in_=s5d_d.rearrange("(f p) -> p f", p=128), **NCQ), [], b_dcol)
        S.dma("sp", lambda: nc.sync.dma_start(out=bglu[:], in_=bglu_d.rearrange("(f p) -> p f", p=128), **NCQ), [], b_bglu)
        S.dma("pool", lambda: P.dma_start(out=wglu[:], in_=wglu_d.rearrange("(k p) n -> p k n", p=128)), [], b_wglu)
        S.dma("sp", lambda: nc.sync.dma_start(out=gT12[:, 0, :], in_=n1g_d.rearrange("(k p) -> p k", p=128), **NCQ), [], b_gT12)
        S.dma("sp", lambda: nc.sync.dma_start(out=gT12[:, 1, :], in_=n2g_d.rearrange("(k p) -> p k", p=128), **NCQ), [], b_gT12)

        cb = Carver(ARB, ARB_N); cf = Carver(ARF, ARF_N)
        win = cb.take(8, 2560); b_win = S.buf("win")
        S.dma("pool", lambda: P.dma_start(out=win[:, :, 0:1280], in_=win_d[:, 0:1280].rearrange("(k p) n -> p k n", p=128)), [], b_win, par=True)
        S.dma("pool", lambda: P.dma_start(out=win[:, :, 1280:2560], in_=win_d[:, 1280:2560].rearrange("(k p) n -> p k n", p=128)), [], b_win, par=True)
        wm = [cb.take(8, 512).rearrange("p k n -> p (k n)").bitcast(F32).rearrange("p (k n) -> p k n", k=8), cb.take(8, 512).rearrange("p k n -> p (k n)").bitcast(F32).rearrange("p (k n) -> p k n", k=8)]
        b_wm = [S.buf("wm0"), S.buf("wm1")]
        LFa = cb.take(16, 8, 16); LFb = cb.take(16, 8, 16); b_LFa = S.buf("LFa"); b_LFb = S.buf("LFb")
        LF = cb.take(32, 128); b_LF = S.buf("LF")
        LF4 = LF.rearrange("p a (b c) -> p a b c", b=8)
        RCa = cb.take(16, 16, 16); RCb = cb.take(16, 16, 16); b_RCa = S.buf("RCa"); b_RCb = S.buf("RCb")
        assert cb.o <= TAIL
        cb.o = TAIL
        Wt = cb.take(32, 128); b_Wt = S.buf("Wt")
        RC = cb.take(32, 256); b_RC = S.buf("RC")
        RC4 = RC.rearrange("p a (b c) -> p a b c", b=16)
        M0 = cb.take(16, 128); b_M0 = S.buf("M0")
        condT = cf.take(8, 8); b_condT = S.buf("condT")
        scT = cf.take(8, 8); b_scT = S.buf("scT")
        bmodT = cf.take(48); b_bmodT = S.buf("bmodT")
        NSM = 30
        sm = cf.take(NSM, 32); b_sm = [S.buf("sm%d" % i) for i in range(NSM)]
        Bm1 = cf.take(32, 16); Bm2 = cf.take(32, 16); b_Bm1 = S.buf("Bm1"); b_Bm2 = S.buf("Bm2")
        CIN = cf.take(4, 128); b_CIN = S.buf("CIN")
        CIN2 = cf.take(4, 128); b_CIN2 = S.buf("CIN2")
        Cm1 = cf.take(512); b_Cm1 = S.buf("Cm1")
        Cm2 = cf.take(512); b_Cm2 = S.buf("Cm2")
        Pre = cf.take(32, 16); Pim = cf.take(32, 16); b_P = [S.buf("P%d" % i) for i in range(16)]
        PEre = cf.take(32, 8); PEim = cf.take(32, 8); b_PE = S.buf("PE")
        PQre = cf.take(32, 8); PQim = cf.take(32, 8); PQt = cf.take(32, 8)
        b_PQ = S.buf("PQ"); b_PQi = S.buf("PQi"); b_PQt = S.buf("PQt")
        PRre = cf.take(32, 16); PRim = cf.take(32, 16); b_PR = S.buf("PR")
        Mt1 = cf.take(4, 128); Mt2 = cf.take(4, 128); b_Mt1 = S.buf("Mt1"); b_Mt2 = S.buf("Mt2")

        (LRE, LIM, DT, XR, XI, MAG, SN, CS, ARE, AIM, T1, T2, T3, NR, D2, RD, QRE, QIM, QB, T4) = range(20)

        def smv(i):
            return sm[:, i, :]

        for half in range(2):
            S.dma("sp", (lambda half=half: nc.sync.dma_start(out=sm[half * 64:(half + 1) * 64, LRE, :], in_=lre_d.rearrange("d g p -> p (d g)"), **NCQ)), [], b_sm[LRE])
            S.dma("sp", (lambda half=half: nc.sync.dma_start(out=sm[half * 64:(half + 1) * 64, LIM, :], in_=lim_d.rearrange("d g p -> p (d g)"), **NCQ)), [], b_sm[LIM])
        S.dma("sp", lambda: nc.sync.dma_start(out=smv(DT), in_=ldt_d.partition_broadcast(128)), [], b_sm[DT])
        S.dma("sp", lambda: nc.sync.dma_start(out=Bm1[0:64], in_=bre_d.rearrange("d g p h -> p (d g) h")), [], b_Bm1)
        S.dma("sp", lambda: nc.sync.dma_start(out=Bm1[64:128], in_=bim_d.rearrange("d g p h -> p (d g) h")), [], b_Bm1)
        S.dma("sp", lambda: nc.sync.dma_start(out=Bm2[0:64], in_=bim_d.rearrange("d g p h -> p (d g) h")), [], b_Bm2)
        S.dma("sp", lambda: nc.sync.dma_start(out=Bm2[64:128], in_=bre_d.rearrange("d g p h -> p (d g) h")), [], b_Bm2)
        S.dma("sp", lambda: nc.sync.dma_start(out=CIN[:, :, 0:64], in_=cre_d.rearrange("(r q) p -> q r p", q=128)), [], b_CIN)
        S.dma("sp", lambda: nc.sync.dma_start(out=CIN[:, :, 64:128], in_=cim_d.rearrange("(r q) p -> q r p", q=128)), [], b_CIN)
        S.dma("sp", lambda: nc.sync.dma_start(out=CIN2[:, :, 0:64], in_=cim_d.rearrange("(r q) p -> q r p", q=128)), [], b_CIN2)
        S.dma("sp", lambda: nc.sync.dma_start(out=CIN2[:, :, 64:128], in_=cre_d.rearrange("(r q) p -> q r p", q=128)), [], b_CIN2)
        for bb in range(5):
            S.dma("sp", (lambda bb=bb: nc.sync.dma_start(out=condT[:, :, bb], in_=cond_d[bb].rearrange("(k p) -> p k", p=128), **NCQ)), [], b_condT)
        S.op("act", lambda: A.activation(out=scT[:, :, 0:5], in_=condT[:, :, 0:5], func=AF.Silu), [b_condT], [b_scT])
        S.dma("sp", lambda: nc.sync.dma_start(out=bmodT, in_=bmod_d.rearrange("(t p) -> p t", p=128), **NCQ), [], b_bmodT)
        pmod = pbank[0][:, 0:384].rearrange("p (t e) -> p t e", e=8)
        for j in range(24):
            s_ = j % 2
            S.dma("sp", (lambda j=j, s_=s_: nc.sync.dma_start(out=wm[s_], in_=wmod_d[:, j * 256:(j + 1) * 256].rearrange("(k p) n -> p k n", p=128))), [], b_wm[s_])
            for f in range(2):
                t = j * 2 + f
                for k in range(8):
                    S.op("pe", (lambda t=t, f=f, k=k, s_=s_: T.matmul(pmod[:, t, 0:5], lhsT=wm[s_][:, k, f * 128:(f + 1) * 128], rhs=scT[:, k, 0:5], start=(k == 0), stop=(k == 7))), [b_wm[s_], b_scT], [b_pb[0]])
        for b in range(5):
            S.op("dve", (lambda b=b: V.tensor_tensor(out=modT[:, :, b], in0=pmod[:, :, b], in1=bmodT, op=ALU.add)), [b_pb[0], b_bmodT], [b_modT])
        S.op("dve", lambda: V.tensor_scalar(out=A1T[:, :, 0:5], in0=modT[:, 8:16, 0:5], scalar1=1.0, scalar2=None, op0=ALU.add), [b_modT], [b_A1T])
        S.op("dve", lambda: V.tensor_scalar(out=A2T[:, :, 0:5], in0=modT[:, 32:40, 0:5], scalar1=1.0, scalar2=None, op0=ALU.add), [b_modT], [b_A2T])
        for b in range(5):
            S.op("dve", (lambda b=b: V.tensor_tensor(out=A1T[:, :, b], in0=A1T[:, :, b], in1=gT12[:, 0, :], op=ALU.mult)), [b_A1T, b_gT12], [b_A1T])
            S.op("dve", (lambda b=b: V.tensor_tensor(out=A2T[:, :, b], in0=A2T[:, :, b], in1=gT12[:, 1, :], op=ALU.mult)), [b_A2T, b_gT12], [b_A2T])

        b_modrow = S.buf("modrow_d")
        for bb in range(NB):
            S.dma("sp", (lambda bb=bb: nc.sync.dma_start(out=modrow_d[bb].rearrange("(t p) -> p t", p=128), in_=modT[:, :, bb], **NCQ)), [b_modT], b_modrow, par=True)
            S.dma("sp", (lambda bb=bb: nc.sync.dma_start(out=a2row_d[bb].rearrange("(k p) -> p k", p=128), in_=A2T[:, :, bb], **NCQ)), [b_A2T], b_modrow, par=True)
        (LRE, LIM, DT, XR, XI, MAG, SN, CS, ARE, AIM, T1, T2, T3, NR, D2, RD, QRE, QIM, QB, T4) = range(20)

        def smv(i):
            return sm[:, i, :]

        def dv(fn, reads, writes):
            S.op("dve", fn, [b_sm[i] for i in reads], [b_sm[i] for i in writes])

        def tt(o, a, b, op):
            dv((lambda: V.tensor_tensor(out=smv(o), in0=smv(a), in1=smv(b), op=op)), [a, b], [o])

        S.op("act", lambda: A.activation(out=smv(DT), in_=smv(DT), func=AF.Exp), [b_sm[DT]], [b_sm[DT]])
        tt(XR, LRE, DT, ALU.mult); tt(XI, LIM, DT, ALU.mult)
        S.op("act", lambda: A.activation(out=smv(MAG), in_=smv(XR), func=AF.Exp, scale=1.0 / 16), [b_sm[XR]], [b_sm[MAG]])
        S.op("act", lambda: A.activation(out=smv(SN), in_=smv(XI), func=AF.Sin, scale=1.0 / 16), [b_sm[XI]], [b_sm[SN]])
        dv((lambda: V.tensor_scalar(out=smv(T4), in0=smv(XI), scalar1=1.0 / 16, scalar2=math.pi / 2, op0=ALU.mult, op1=ALU.add)), [XI], [T4])
        S.op("act", lambda: A.activation(out=smv(CS), in_=smv(T4), func=AF.Sin), [b_sm[T4]], [b_sm[CS]])
        tt(ARE, MAG, CS, ALU.mult); tt(AIM, MAG, SN, ALU.mult)

        def csq(re, im):
            tt(T1, re, re, ALU.mult); tt(T2, im, im, ALU.mult); tt(T3, re, im, ALU.mult)
            tt(re, T1, T2, ALU.subtract)
            dv((lambda: V.tensor_scalar(out=smv(im), in0=smv(T3), scalar1=2.0, scalar2=None, op0=ALU.mult)), [T3], [im])

        for _ in range(4):
            csq(ARE, AIM)
        dv((lambda: V.tensor_scalar(out=smv(NR), in0=smv(ARE), scalar1=-1.0, scalar2=None, op0=ALU.add)), [ARE], [NR])
        tt(T1, LRE, LRE, ALU.mult); tt(T2, LIM, LIM, ALU.mult); tt(D2, T1, T2, ALU.add)
        dv((lambda: V.reciprocal(out=smv(RD), in_=smv(D2))), [D2], [RD])
        tt(T1, NR, LRE, ALU.mult); tt(T2, AIM, LIM, ALU.mult); tt(T3, T1, T2, ALU.add); tt(QRE, T3, RD, ALU.mult)
        tt(T1, AIM, LRE, ALU.mult); tt(T2, NR, LIM, ALU.mult); tt(T3, T1, T2, ALU.subtract); tt(QIM, T3, RD, ALU.mult)
        S.op("dve", lambda: V.tensor_scalar(out=smv(QB), in0=smv(QIM), scalar1=sgn, scalar2=-1.0, op0=ALU.mult, op1=ALU.mult), [b_sm[QIM], b_cst], [b_sm[QB]])
        def cmulP(o_re, o_im, bo, x_re, x_im, bx, y_re, y_im, by):
            bxy = list(bx) + list(by)
            S.op("dve", (lambda: V.tensor_tensor(out=smv(T1), in0=x_re, in1=y_re, op=ALU.mult)), bxy, [b_sm[T1]])
            S.op("dve", (lambda: V.tensor_tensor(out=smv(T2), in0=x_im, in1=y_im, op=ALU.mult)), bxy, [b_sm[T2]])
            S.op("dve", (lambda: V.tensor_tensor(out=o_re, in0=smv(T1), in1=smv(T2), op=ALU.subtract)), [b_sm[T1], b_sm[T2]], [bo])
            S.op("dve", (lambda: V.tensor_tensor(out=smv(T1), in0=x_re, in1=y_im, op=ALU.mult)), bxy, [b_sm[T1]])
            S.op("dve", (lambda: V.tensor_tensor(out=smv(T2), in0=x_im, in1=y_re, op=ALU.mult)), bxy, [b_sm[T2]])
            S.op("dve", (lambda: V.tensor_tensor(out=o_im, in0=smv(T1), in1=smv(T2), op=ALU.add)), [b_sm[T1], b_sm[T2]], [bo])

        AIRE, AIIM, A8RE, A8IM = 20, 21, 22, 23
        b_A = S.buf("a_pair")
        S.op("dve", lambda: V.memset(Pre[:, :, 7], 1.0), [], [b_P[7]])
        S.op("dve", lambda: V.memset(Pim[:, :, 7], 0.0), [], [b_P[7]])
        S.op("dve", lambda: V.tensor_copy(out=Pre[:, :, 8], in_=smv(ARE)), [b_sm[ARE], b_sm[AIM]], [b_P[8]])
        S.op("dve", lambda: V.tensor_copy(out=Pim[:, :, 8], in_=smv(AIM)), [b_sm[AIM]], [b_P[8]])
        for m in range(8, 15):
            cmulP(Pre[:, :, m + 1], Pim[:, :, m + 1], b_P[m + 1], Pre[:, :, m], Pim[:, :, m], [b_P[m]], smv(ARE), smv(AIM), [b_sm[ARE], b_sm[AIM]])
        tt(T1, ARE, ARE, ALU.mult); tt(T2, AIM, AIM, ALU.mult); tt(T3, T1, T2, ALU.add)
        dv((lambda: V.reciprocal(out=smv(T4), in_=smv(T3))), [T3], [T4])
        tt(AIRE, ARE, T4, ALU.mult)
        dv((lambda: V.scalar_tensor_tensor(out=smv(AIIM), in0=smv(AIM), scalar=-1.0, in1=smv(T4), op0=ALU.mult, op1=ALU.mult)), [AIM, T4], [AIIM])
        b_AI = S.buf("ainv_pair")
        for m in range(7, 0, -1):
            cmulP(Pre[:, :, m - 1], Pim[:, :, m - 1], b_P[m - 1], Pre[:, :, m], Pim[:, :, m], [b_P[m]], smv(AIRE), smv(AIIM), [b_sm[AIRE], b_sm[AIIM]])
        S.op("dve", lambda: V.tensor_copy(out=smv(A8RE), in_=Pre[:, :, 15]), [b_P[15]], [b_sm[A8RE]])
        S.op("dve", lambda: V.tensor_copy(out=smv(A8IM), in_=Pim[:, :, 15]), [b_P[15]], [b_sm[A8IM]])
        for m in range(NLEV):
            S.op("dve", (lambda m=m: V.tensor_copy(out=c1t[:, :, m], in_=smv(A8RE))), [b_sm[A8RE]], [b_c1t])
            S.op("dve", (lambda m=m: V.tensor_scalar(out=c2t[:, :, m], in0=smv(A8IM), scalar1=sgn, scalar2=None, op0=ALU.mult)), [b_sm[A8IM], b_cst], [b_c2t])
            if m < NLEV - 1:
                csq(A8RE, A8IM)
        b_rotds = [S.buf("rot_d0"), S.buf("rot_d1")]
        b_Pall = b_P
        for s8 in range(8):
            S.op("dve", (lambda s8=s8: V.tensor_copy(out=PEre[:, 0:16, s8], in_=Pre[:, 0:16, 14 - s8])), [b_P[14 - s8]], [b_PE])
            S.op("dve", (lambda s8=s8: V.tensor_copy(out=PEim[:, 0:16, s8], in_=Pim[:, 0:16, 14 - s8])), [b_P[14 - s8]], [b_PE])
            S.op("dve", (lambda s8=s8: V.tensor_copy(out=PEre[:, 16:32, s8], in_=Pre[:, 16:32, 7 + s8])), [b_P[7 + s8]], [b_PE])
            S.op("dve", (lambda s8=s8: V.tensor_copy(out=PEim[:, 16:32, s8], in_=Pim[:, 16:32, 7 + s8])), [b_P[7 + s8]], [b_PE])
        qre_b = smv(QRE).unsqueeze(2).broadcast_to([128, 32, 8])
        qim_b = smv(QIM).unsqueeze(2).broadcast_to([128, 32, 8])
        S.op("dve", lambda: V.tensor_tensor(out=PQre, in0=PEre, in1=qre_b, op=ALU.mult), [b_PE, b_sm[QRE]], [b_PQ])
        S.op("dve", lambda: V.tensor_tensor(out=PQt, in0=PEim, in1=qim_b, op=ALU.mult), [b_PE, b_sm[QIM]], [b_PQt])
        S.op("dve", lambda: V.tensor_tensor(out=PQre, in0=PQre, in1=PQt, op=ALU.subtract), [b_PQ, b_PQt], [b_PQ])
        S.op("dve", lambda: V.tensor_tensor(out=PQim, in0=PEre, in1=qim_b, op=ALU.mult), [b_PE, b_sm[QIM]], [b_PQi])
        S.op("dve", lambda: V.tensor_tensor(out=PQt, in0=PEim, in1=qre_b, op=ALU.mult), [b_PE, b_sm[QRE], b_PQ], [b_PQt])
        S.op("dve", lambda: V.tensor_tensor(out=PQim, in0=PQim, in1=PQt, op=ALU.add), [b_PQi, b_PQt], [b_PQi])
        S.op("dve", lambda: V.tensor_scalar(out=PQim, in0=PQim, scalar1=sgn, scalar2=-1.0, op0=ALU.mult, op1=ALU.mult), [b_PQi, b_cst], [b_PQi])
        for d in range(2):
            dsl = slice(d * 16, (d + 1) * 16)
            S.op("dve", (lambda dsl=dsl: V.tensor_tensor(out=LFa, in0=PQre[:, dsl, :].unsqueeze(3).broadcast_to([128, 16, 8, 16]), in1=Bm1[:, dsl, :].unsqueeze(2).broadcast_to([128, 16, 8, 16]), op=ALU.mult)), [b_PQ, b_Bm1], [b_LFa])
            S.op("dve", (lambda dsl=dsl: V.tensor_tensor(out=LFb, in0=PQim[:, dsl, :].unsqueeze(3).broadcast_to([128, 16, 8, 16]), in1=Bm2[:, dsl, :].unsqueeze(2).broadcast_to([128, 16, 8, 16]), op=ALU.mult)), [b_PQi, b_Bm2], [b_LFb])
            S.op("dve", (lambda dsl=dsl: V.tensor_tensor(out=LF4[:, dsl], in0=LFa, in1=LFb, op=ALU.add)), [b_LFa, b_LFb], [b_LF])
        for r in range(4):
            for q8 in range(8):
                dg = r * 8 + q8
                S.op("pe", (lambda dg=dg, q8=q8: T.transpose(out=pT[:, q8, :], in_=LF[:, dg, :], identity=identb[:])), [b_LF, b_identb], [b_pT])
            S.op("act", (lambda r=r: A.copy(out=Wt[:, r * 8:(r + 1) * 8, :], in_=pT[:, :, :])), [b_pT], [b_Wt])
        for r in range(4):
            S.op("pe", (lambda r=r: T.transpose(out=pbank[1][:, r * 128:(r + 1) * 128], in_=CIN[:, r, :], identity=identf)), [b_CIN, b_cst], [b_pb[1]])
        S.op("dve", lambda: V.tensor_scalar(out=Cm1, in0=pbank[1][:, :], scalar1=sgn, scalar2=None, op0=ALU.mult), [b_pb[1], b_cst], [b_Cm1])
        for r in range(4):
            S.op("pe", (lambda r=r: T.transpose(out=pbank[2][:, r * 128:(r + 1) * 128], in_=CIN2[:, r, :], identity=identf)), [b_CIN2, b_cst], [b_pb[2]])
        S.op("dve", lambda: V.tensor_scalar(out=Cm2, in0=pbank[2][:, :], scalar1=sgn, scalar2=None, op0=ALU.mult), [b_pb[2], b_cst], [b_Cm2])
        S.op("dve", lambda: V.tensor_copy(out=PRre[:, 0:16, :], in_=Pre[:, 0:16, :]), b_P, [b_PR])
        S.op("dve", lambda: V.tensor_copy(out=PRim[:, 0:16, :], in_=Pim[:, 0:16, :]), b_P, [b_PR])
        for m in range(16):
            S.op("dve", (lambda m=m: V.tensor_copy(out=PRre[:, 16:32, m], in_=Pre[:, 16:32, 15 - m])), [b_P[15 - m]], [b_PR])
            S.op("dve", (lambda m=m: V.tensor_copy(out=PRim[:, 16:32, m], in_=Pim[:, 16:32, 15 - m])), [b_P[15 - m]], [b_PR])
        S.op("dve", lambda: V.tensor_scalar(out=PRim, in0=PRim, scalar1=sgn, scalar2=-1.0, op0=ALU.mult, op1=ALU.mult), [b_PR, b_cst], [b_PR])
        Cm1v = Cm1.rearrange("p (a b) -> p a b", a=32)
        Cm2v = Cm2.rearrange("p (a b) -> p a b", a=32)
        for d in range(2):
            dsl = slice(d * 16, (d + 1) * 16)
            S.op("dve", (lambda dsl=dsl: V.tensor_tensor(out=RCa, in0=PRre[:, dsl, :].unsqueeze(3).broadcast_to([128, 16, 16, 16]), in1=Cm1v[:, dsl, :].unsqueeze(2).broadcast_to([128, 16, 16, 16]), op=ALU.mult)), [b_PR, b_Cm1], [b_RCa])
            S.op("dve", (lambda dsl=dsl: V.tensor_tensor(out=RCb, in0=PRim[:, dsl, :].unsqueeze(3).broadcast_to([128, 16, 16, 16]), in1=Cm2v[:, dsl, :].unsqueeze(2).broadcast_to([128, 16, 16, 16]), op=ALU.mult)), [b_PR, b_Cm2], [b_RCb])
            S.op("dve", (lambda dsl=dsl: V.tensor_tensor(out=RC4[:, dsl], in0=RCa, in1=RCb, op=ALU.add)), [b_RCa, b_RCb], [b_RC])
        for g4 in range(4):
            for gi in range(4):
                g = g4 * 4 + gi
                S.op("pe", (lambda g=g, gi=gi: T.matmul(pbank[3][:, gi * 128:(gi + 1) * 128], lhsT=LF[:, g, :], rhs=RC[:, g, 0:128], start=True, stop=True)), [b_LF, b_RC], [b_pb[3]])
                S.op("pe", (lambda g=g, gi=gi: T.matmul(pbank[4][:, gi * 128:(gi + 1) * 128], lhsT=LF[:, 16 + g, :], rhs=RC[:, 16 + g, 128:256], start=True, stop=True)), [b_LF, b_RC], [b_pb[4]])
            S.op("dve", lambda: V.tensor_tensor(out=Mt1, in0=pbank[3][:, :].rearrange("p (a b) -> p a b", a=4), in1=maskf.unsqueeze(1).broadcast_to([128, 4, 128]), op=ALU.mult), [b_pb[3], b_cst], [b_Mt1])
            S.op("dve", lambda: V.tensor_tensor(out=Mt2, in0=pbank[4][:, :].rearrange("p (a b) -> p a b", a=4), in1=maskb.unsqueeze(1).broadcast_to([128, 4, 128]), op=ALU.mult), [b_pb[4], b_cst], [b_Mt2])
            S.op("dve", (lambda g4=g4: V.tensor_tensor(out=M0[:, g4 * 4:(g4 + 1) * 4, :], in0=Mt1, in1=Mt2, op=ALU.add)), [b_Mt1, b_Mt2], [b_M0])

        S.barrier()

        cb = Carver(ARB, ARB_N); cf = Carver(ARF, ARF_N)
        cb.take(8, 2560)
        UT = cb.take(2, 2560); b_UT = [S.buf("UT0"), S.buf("UT1")]
        o_alias = cb.o
        cvcol = cb.take(3, SEQ); b_cvcol = [S.buf("cvcol%d" % i) for i in range(3)]
        bcol = cb.take(3, SEQ); b_bcol = [S.buf("bcol%d" % i) for i in range(3)]
        o_end = cb.o
        cb.o = o_alias
        Hs2 = [[cb.take(NBLK), cb.take(NBLK)], [cb.take(NBLK), cb.take(NBLK)]]
        b_Hs2 = [[S.buf("Hs00"), S.buf("Hs01")], [S.buf("Hs10"), S.buf("Hs11")]]
        rot = [cb.take(2 * NLEV, 128), cb.take(2 * NLEV, 128)]; b_rot = [S.buf("rot0"), S.buf("rot1")]
        gTt = cb.take(2, SEQ); b_gTt = [S.buf("gT0"), S.buf("gT1")]
        Yall = cb.take(8, 256); b_Yall = S.buf("Yall")
        assert cb.o <= o_end
        cb.o = o_end
        hT0 = cb.take(8, 512)
        junk = cb.take(D); b_junk = S.buf("junk")
        xn = cb.take(D); b_xn = S.buf("xn")
        yst = [cb.take(512), cb.take(512)]; b_yst = [S.buf("yst0"), S.buf("yst1")]
        ycolst = cb.take(SEQ); b_ycolst = S.buf("ycolst")
        Xg = [ycolst[:, 0:NX], ycolst[:, 1024:1024 + NX]]; b_Xg = [S.buf("Xg0"), S.buf("Xg1")]
        assert cb.o <= TAIL, cb.o
        cb.o = TAIL + 14336
        hT1 = cb.take(8, 512)
        hTs = [hT0, hT1]; b_hTs = [S.buf("hT0"), S.buf("hT1")]
        hcnt = {"i": 0}
        xt = [cf.take(D), cf.take(D)]; b_xt = [S.buf("xt0"), S.buf("xt1")]
        ysacc = cf.take(SEQ); b_ysacc = S.buf("ysacc")
        Csb2 = [cf.take(512), cf.take(512)]; b_Csb2 = [S.buf("Csb0"), S.buf("Csb1")]
        cvt2 = [cf.take(512), cf.take(512)]; b_cvt2 = [S.buf("cvt0"), S.buf("cvt1")]
        cacc2 = [cf.take(512), cf.take(512)]; b_cacc2 = [S.buf("cacc0"), S.buf("cacc1")]
        ge1 = cf.take(512); ge2 = cf.take(512); b_ge1 = S.buf("ge1"); b_ge2 = S.buf("ge2")
        rt1 = cf.take(128); rt2 = cf.take(128); b_rt1 = S.buf("rt1"); b_rt2 = S.buf("rt2")
        colacc = cf.take(SEQ) if False else None


        b_wb = S.buf("wb_scr")
        precast = []
        for e in range(16):
            precast.append(lambda e=e: S.dma("pool", (lambda: P.dma_start(out=w1b_d[e], in_=w1_d[e].rearrange("(k p) n -> p k n", p=128))), [], b_wb))
            precast.append(lambda e=e: S.dma("pool", (lambda: P.dma_start(out=w3b_d[e], in_=w3_d[e].rearrange("(k p) n -> p k n", p=128))), [], b_wb))
            precast.append(lambda e=e: S.dma("pool", (lambda: P.dma_start(out=w2b_d[e], in_=w2_d[e].rearrange("(k p) n -> p k n", p=128))), [], b_wb))
        b_ycats = [S.buf("ycat_d%d" % i) for i in range(4)]
        b_x1ds = [S.buf("x1_d%d" % i) for i in range(4)]
        b_outs = [S.buf("out_d%d" % i) for i in range(4)]
        rr = {"yc": 0, "out": 0}

        def nyc():
            rr["yc"] += 1
            return b_ycats[rr["yc"] % 4]
        cnt = {"xt": 0, "ev": 0, "ss": 0}

        def norm_transpose(nb, src_rows, AT, ST, bA, bS, idx, dst, b_dst, eps=1e-6, src_sb=None, b_src=None):
            junk, xn, b_junk, b_xn, xt, b_xt = nb
            if src_sb is None:
                i = cnt["xt"] % 2; cnt["xt"] += 1
                xs = xt[i]; bx = b_xt[i]
                S.dma("sp", (lambda: nc.sync.dma_start(out=xs, in_=src_rows)), [], bx)
            else:
                xs = src_sb; bx = b_src
            j = cnt["ss"] % 4; cnt["ss"] += 1
            S.op("act", (lambda: A.activation(out=junk, in_=xs, func=AF.Square, accum_out=ss[:, j:j + 1])), [bx], [b_junk, b_ss[j]])
            S.op("act", (lambda: A.activation(out=rstd[:, j:j + 1], in_=ss[:, j:j + 1], func=AF.Sqrt, scale=1.0 / D, bias=epsc)), [b_ss[j], b_cst], [b_rstd[j]])
            S.op("dve", (lambda: V.reciprocal(out=rstd[:, j:j + 1], in_=rstd[:, j:j + 1])), [b_rstd[j]], [b_rstd[j]])
            S.op("act", (lambda: A.activation(out=xn, in_=xs, func=AF.Copy, scale=rstd[:, j:j + 1])), [bx, b_rstd[j]], [b_xn])
            for k in range(8):
                S.op("pe", (lambda k=k: T.transpose(out=pT[:, k, :], in_=xn[:, k * 128:(k + 1) * 128], identity=identb[:])), [b_xn, b_identb], [b_pT])
            for k in range(8):
                if k % 2 == 0:
                    S.op("dve", (lambda k=k: V.tensor_scalar(out=dst[:, k, :], in0=pT[:, k, :], scalar1=AT[:, k, idx:idx + 1], scalar2=ST[:, k, idx:idx + 1], op0=ALU.mult, op1=ALU.add)), [b_pT, bA, bS], [b_dst])
                else:
                    S.op("act", (lambda k=k: A.activation(out=dst[:, k, :], in_=pT[:, k, :], func=AF.Identity, scale=AT[:, k, idx:idx + 1], bias=ST[:, k, idx:idx + 1])), [b_pT, bA, bS], [b_dst])
            return j

        S1T = modT[:, 0:8, :]
        nbB = (junk, xn, b_junk, b_xn, xt, b_xt)
        S2T = modT[:, 24:32, :]
        pz = {"i": 0}

        def zbank():
            i = 1 + (pz["i"] % 6); pz["i"] += 1
            return pbank[i], b_pb[i]

        def ctx_norms(bb):
            hT_ = hTs[hcnt["i"] % 2]; b_hT_ = b_hTs[hcnt["i"] % 2]; hcnt["i"] += 1
            for ti in range(2):
                norm_transpose(nbB, ctx_d[bb * NCTX + ti * 128: bb * NCTX + (ti + 1) * 128, :], A1T, S1T, b_A1T, b_modT, 4, hT_[:, :, ti * 128:(ti + 1) * 128], b_hT_)
            return hT_, b_hT_

        def chunk_norms(bb, c_):
            t0_ = bb * SEQ + c_ * 512
            hT_ = hTs[hcnt["i"] % 2]; b_hT_ = b_hTs[hcnt["i"] % 2]; hcnt["i"] += 1
            for ti in range(4):
                norm_transpose(nbB, x_d[t0_ + ti * 128: t0_ + (ti + 1) * 128, :], A1T, S1T, b_A1T, b_modT, bb, hT_[:, :, ti * 128:(ti + 1) * 128], b_hT_)
            return hT_, b_hT_

        def chunk_norm_alloc():
            hT_ = hTs[hcnt["i"] % 2]; b_hT_ = b_hTs[hcnt["i"] % 2]; hcnt["i"] += 1
            return hT_, b_hT_

        def chunk_norm_tile(bb, c_, ti, hT_, b_hT_):
            t0_ = bb * SEQ + c_ * 512
            norm_transpose(nbB, x_d[t0_ + ti * 128: t0_ + (ti + 1) * 128, :], A1T, S1T, b_A1T, b_modT, bb, hT_[:, :, ti * 128:(ti + 1) * 128], b_hT_)

        pref = {}
        for b in range(NB):
            if b in pref:
                (hT, b_hT), pre_c0 = pref[b]
            else:
                hT, b_hT = ctx_norms(b)
                pre_c0 = None
            for ft in range(2):
                pb_, bpb_ = zbank()
                for k in range(8):
                    S.op("pe", (lambda k=k, ft=ft, pb_=pb_, hT=hT: T.matmul(pb_[:, 0:256], lhsT=win[:, k, ft * 128:(ft + 1) * 128], rhs=hT[:, k, 0:256], start=(k == 0), stop=(k == 7))), [b_win, b_hT], [bpb_])
                S.op("act", (lambda ft=ft, pb_=pb_: A.copy(out=UT[:, ft, 0:256], in_=pb_[:, 0:256])), [bpb_], [b_UT[ft]])
                S.op("dve", (lambda ft=ft, pb_=pb_: V.tensor_copy(out=UT[:, ft, 2304:2560], in_=pb_[:, 0:256])), [bpb_], [b_UT[ft]])
            def do_norms(c_):
                return chunk_norms(b, c_)

            nxt_h = pre_c0 if pre_c0 is not None else do_norms(0)
            for c in range(4):
                t0 = b * SEQ + c * 512
                hT, b_hT = nxt_h

                def zx(col0, hT=hT, b_hT=b_hT):
                    pb_, bpb_ = zbank()
                    for k in range(8):
                        S.op("pe", (lambda k=k, pb_=pb_: T.matmul(pb_[:, :], lhsT=win[:, k, col0:col0 + 128], rhs=hT[:, k, :], start=(k == 0), stop=(k == 7))), [b_win, b_hT], [bpb_])
                    return pb_, bpb_

                for ft in range(2):
                    pb_, bpb_ = zx(ft * 128)
                    S.op("act", (lambda ft=ft, pb_=pb_, c=c: A.copy(out=UT[:, ft, 256 + c * 512: 256 + (c + 1) * 512], in_=pb_[:, :])), [bpb_], [b_UT[ft]])
                if c + 1 < 4:
                    nxt_h = chunk_norm_alloc()
                for i in range(6):
                    if c + 1 < 4 and 1 <= i <= 4:
                        chunk_norm_tile(b, c + 1, i - 1, nxt_h[0], nxt_h[1])
                    q = i % 2
                    Csb_, cvt_, cacc_ = Csb2[q], cvt2[q], cacc2[q]
                    bCsb_, bcvt_, bcacc_ = b_Csb2[q], b_cvt2[q], b_cacc2[q]
                    pC, bC = zx(1024 + i * 128)
                    S.op("act", (lambda pC=pC, Csb_=Csb_: A.copy(out=Csb_, in_=pC[:, :])), [bC], [bCsb_])
                    pV, bV = zx(1792 + i * 128)
                    if i < 3:
                        S.op("dve", (lambda pV=pV, Csb_=Csb_, cvt_=cvt_: V.tensor_tensor(out=cvt_, in0=Csb_, in1=pV[:, :], op=ALU.mult)), [bCsb_, bV], [bcvt_])
                        pB, bB = zx(256 + i * 128)
                        S.op("act", (lambda i=i, cvt_=cvt_, cacc_=cacc_: A.activation(out=cacc_, in_=cvt_, func=AF.Copy, scale=cw[:, i, 1:2])), [bcvt_, b_cw], [bcacc_])
                        c3 = cvt_.rearrange("p (r w) -> p r w", w=64)
                        a3 = cacc_.rearrange("p (r w) -> p r w", w=64)
                        S.op("dve", (lambda i=i, c3=c3, a3=a3: V.scalar_tensor_tensor(out=a3[:, :, 1:64], in0=c3[:, :, 0:63], scalar=cw[:, i, 0:1], in1=a3[:, :, 1:64], op0=ALU.mult, op1=ALU.add)), [bcvt_, bcacc_, b_cw], [bcacc_])
                        S.op("dve", (lambda i=i, c3=c3, a3=a3: V.scalar_tensor_tensor(out=a3[:, :, 0:63], in0=c3[:, :, 1:64], scalar=cw[:, i, 2:3], in1=a3[:, :, 0:63], op0=ALU.mult, op1=ALU.add)), [bcvt_, bcacc_, b_cw], [bcacc_])
                        yi = (c * 6 + i) % 2
                        S.op("dve", (lambda pB=pB, yi=yi, cacc_=cacc_: V.tensor_tensor(out=yst[yi], in0=cacc_, in1=pB[:, :], op=ALU.mult)), [bcacc_, bB], [b_yst[yi]])
                        S.dma("pool", (lambda b=b, i=i, c=c, yi=yi: P.dma_start(out=ycat_d[b, :, 2 + i, c * 512:(c + 1) * 512], in_=yst[yi])), [b_yst[yi]], nyc())
                    else:
                        j = i - 3
                        S.op("dve", (lambda pV=pV, j=j, c=c, Csb_=Csb_: V.tensor_tensor(out=cvcol[:, j, c * 512:(c + 1) * 512], in0=Csb_, in1=pV[:, :], op=ALU.mult)), [bCsb_, bV], [b_cvcol[j]])
                        pB, bB = zx(256 + i * 128)
                        S.op("act", (lambda pB=pB, j=j, c=c: A.copy(out=bcol[:, j, c * 512:(c + 1) * 512], in_=pB[:, :])), [bB], [b_bcol[j]])
            for j in range(3):
                i = 3 + j
                S.op("act", (lambda j=j, i=i: A.activation(out=ysacc, in_=cvcol[:, j, :], func=AF.Copy, scale=cw[:, i, 1:2])), [b_cvcol[j], b_cw], [b_ysacc])
                S.op("dve", (lambda j=j, i=i: V.scalar_tensor_tensor(out=ysacc[:, 64:SEQ], in0=cvcol[:, j, 0:SEQ - 64], scalar=cw[:, i, 0:1], in1=ysacc[:, 64:SEQ], op0=ALU.mult, op1=ALU.add)), [b_cvcol[j], b_ysacc, b_cw], [b_ysacc])
                S.op("dve", (lambda j=j, i=i: V.scalar_tensor_tensor(out=ysacc[:, 0:SEQ - 64], in0=cvcol[:, j, 64:SEQ], scalar=cw[:, i, 2:3], in1=ysacc[:, 0:SEQ - 64], op0=ALU.mult, op1=ALU.add)), [b_cvcol[j], b_ysacc, b_cw], [b_ysacc])
                S.op("pool", (lambda j=j: P.tensor_tensor(out=ycolst, in0=ysacc, in1=bcol[:, j, :], op=ALU.mult)), [b_ysacc, b_bcol[j]], [b_ycolst])
                S.dma("pool", (lambda b=b, j=j: P.dma_start(out=ycat_d[b, :, 5 + j, :], in_=ycolst)), [b_ycolst], nyc())
            S.barrier()
            for _ in range(16):
                if precast:
                    precast.pop(0)()
            for _ in range(26):
                if zfill:
                    r_ = zfill.pop(0)
                    S.dma("pool", (lambda r_=r_: P.dma_start(out=h2s_d[r_ * 128:(r_ + 1) * 128, :], in_=zres[:, :])), [b_zres], b_h2z, par=True)
            if b + 1 < NB:
                pref[b + 1] = (ctx_norms(b + 1), chunk_norms(b + 1, 0))
            for ft in range(2):
                for gp in range(4):
                    gl = [(ft * 8 + gp * 2 + u, gp * 2 + u, u) for u in range(2)]
                    for (g, g8, ri) in gl:
                        if b == 0:
                            for d in range(2):
                                dg = d * 16 + g
                                for m in range(NLEV):
                                    S.op("dve", (lambda dg=dg, m=m: V.tensor_scalar(out=rt1, in0=identf, scalar1=c1t[:, dg, m:m + 1], scalar2=None, op0=ALU.mult)), [b_cst, b_c1t], [b_rt1])
                                    S.op("dve", (lambda d=d, dg=dg, m=m, ri=ri: V.scalar_tensor_tensor(out=rot[ri][:, d * NLEV + m, :], in0=iswapf, scalar=c2t[:, dg, m:m + 1], in1=rt1, op0=ALU.mult, op1=ALU.add)), [b_cst, b_c2t, b_rt1], [b_rot[ri]])
                            S.dma("pool", (lambda g=g, ri=ri: P.dma_start(out=rot_d[g], in_=rot[ri])), [b_rot[ri]], b_rotds[ri])
                        else:
                            S.dma("sp", (lambda g=g, ri=ri: nc.sync.dma_start(out=rot[ri], in_=rot_d[g])), b_rotds, b_rot[ri])
                        pb_, bpb_ = zbank()
                        for s8 in range(8):
                            S.op("pe", (lambda pb_=pb_, s8=s8, g8=g8, ft=ft: T.matmul(pb_[:, 0:NX], lhsT=Gsel[:, g8, 112 - 16 * s8: 240 - 16 * s8], rhs=UT[:, ft, s8:2560:8], start=(s8 == 0), stop=(s8 == 7))), [b_Gsel, b_UT[ft]], [bpb_])
                        S.op("act", (lambda pb_=pb_, ri=ri: A.copy(out=Xg[ri], in_=pb_[:, 0:NX])), [bpb_], [b_Xg[ri]])
                    for (g, g8, ri) in gl:
                        for d in range(2):
                            dg = d * 16 + g
                            off = 0 if d == 0 else 32
                            pb_, bpb_ = zbank()
                            S.op("pe", (lambda pb_=pb_, dg=dg, off=off, ri=ri: T.matmul(pb_[:, 0:NBLK], lhsT=Wt[:, dg, :], rhs=Xg[ri][:, off:off + NBLK], start=True, stop=True)), [b_Wt, b_Xg[ri]], [bpb_])
                            if d == 0:
                                S.op("act", (lambda pb_=pb_, d=d, ri=ri: A.copy(out=Hs2[ri][d], in_=pb_[:, 0:NBLK])), [bpb_], [b_Hs2[ri][d]])
                            else:
                                S.op("dve", (lambda pb_=pb_, d=d, ri=ri: V.tensor_copy(out=Hs2[ri][d], in_=pb_[:, 0:NBLK])), [bpb_], [b_Hs2[ri][d]])
                    for m in range(NLEV):
                        s_ = 1 << m
                        n = NBLK - s_
                        for (g, g8, ri) in gl:
                            for d in range(2):
                                lo, rlo = (s_, 0) if d == 0 else (0, s_)
                                pb_, bpb_ = zbank()
                                S.op("pe", (lambda pb_=pb_, d=d, m=m, rlo=rlo, n=n, ri=ri: T.matmul(pb_[:, 0:n], lhsT=rot[ri][:, d * NLEV + m, :], rhs=Hs2[ri][d][:, rlo:rlo + n], start=True, stop=True)), [b_rot[ri], b_Hs2[ri][d]], [bpb_])
                                S.op("dve", (lambda pb_=pb_, d=d, lo=lo, n=n, ri=ri: V.tensor_tensor(out=Hs2[ri][d][:, lo:lo + n], in0=pb_[:, 0:n], in1=Hs2[ri][d][:, lo:lo + n], op=ALU.add)), [bpb_, b_Hs2[ri][d]], [b_Hs2[ri][d]])
                    for (g, g8, ri) in gl:
                        pb_, bpb_ = zbank()
                        S.op("pe", (lambda pb_=pb_, g=g, ri=ri: T.matmul(pb_[:, 0:256], lhsT=M0[:, g, :], rhs=Xg[ri][:, 32:288], start=True, stop=False)), [b_M0, b_Xg[ri]], [bpb_])
                        S.op("pe", (lambda pb_=pb_, g=g, ri=ri: T.matmul(pb_[:, 0:256], lhsT=RC[:, g, 128:256], rhs=Hs2[ri][0][:, 31:287], start=False, stop=False)), [b_RC, b_Hs2[ri][0]], [bpb_])
                        S.op("pe", (lambda pb_=pb_, g=g, ri=ri: T.matmul(pb_[:, 0:256], lhsT=RC[:, 16 + g, 0:128], rhs=Hs2[ri][1][:, 1:257], start=False, stop=True)), [b_RC, b_Hs2[ri][1]], [bpb_])
                        S.op("act", (lambda pb_=pb_, g8=g8: A.copy(out=Yall[:, g8, :], in_=pb_[:, 0:256])), [bpb_], [b_Yall])
                for q in range(4):
                    pb_, bpb_ = zbank()
                    for t8 in range(8):
                        for g8 in range(8):
                            S.op("pe", (lambda pb_=pb_, t8=t8, g8=g8, q=q: T.matmul(pb_[:, t8:512:8], lhsT=Gsel[:, t8, 112 - 16 * g8: 240 - 16 * g8], rhs=Yall[:, g8, q * 64:(q + 1) * 64], start=(g8 == 0), stop=(g8 == 7))), [b_Gsel, b_Yall], [bpb_])
                    S.op("dve", (lambda pb_=pb_, q=q, ft=ft: V.scalar_tensor_tensor(out=ysacc[:, q * 512:(q + 1) * 512], in0=UT[:, ft, 256 + q * 512: 256 + (q + 1) * 512], scalar=dcol[:, ft:ft + 1], in1=pb_[:, :], op0=ALU.mult, op1=ALU.add)), [b_UT[ft], b_dcol, bpb_], [b_ysacc])
                for c in range(4):
                    xs_ = ysacc[:, c * 512:(c + 1) * 512]
                    S.op("act", (lambda xs_=xs_: A.activation(out=ge1, in_=xs_, func=AF.Square)), [b_ysacc], [b_ge1])
                    S.op("dve", (lambda: V.tensor_scalar(out=ge1, in0=ge1, scalar1=0.044715, scalar2=1.0, op0=ALU.mult, op1=ALU.add)), [b_ge1], [b_ge1])
                    S.op("dve", (lambda xs_=xs_: V.tensor_tensor(out=ge2, in0=ge1, in1=xs_, op=ALU.mult)), [b_ge1, b_ysacc], [b_ge2])
                    S.op("act", (lambda: A.activation(out=ge1, in_=ge2, func=AF.Sigmoid, scale=1.5957691216057308)), [b_ge2, b_ge1], [b_ge1])
                    S.op("dve", (lambda xs_=xs_, ft=ft, c=c: V.tensor_tensor(out=gTt[:, ft, c * 512:(c + 1) * 512], in0=ge1, in1=xs_, op=ALU.mult)), [b_ge1, b_ysacc], [b_gTt[ft]])
            for c in range(4):
                for f2 in range(2):
                    pb_, bpb_ = zbank()
                    for k in range(2):
                        S.op("pe", (lambda pb_=pb_, k=k, f2=f2, c=c: T.matmul(pb_[:, :], lhsT=wglu[:, k, f2 * 128:(f2 + 1) * 128], rhs=gTt[:, k, c * 512:(c + 1) * 512], start=(k == 0), stop=(k == 1))), [b_wglu, b_gTt[k]], [bpb_])
                    S.op("act", (lambda pb_=pb_, f2=f2: A.activation(out=ge1, in_=pb_[:, :], func=AF.Sigmoid, bias=bglu[:, f2:f2 + 1])), [bpb_, b_bglu], [b_ge1])
                    yi = (c * 2 + f2) % 2
                    S.op("dve", (lambda f2=f2, c=c, yi=yi: V.tensor_tensor(out=yst[yi], in0=ge1, in1=gTt[:, f2, c * 512:(c + 1) * 512], op=ALU.mult)), [b_ge1, b_gTt[f2]], [b_yst[yi]])
                    S.dma("pool", (lambda b=b, f2=f2, c=c, yi=yi: P.dma_start(out=ycat_d[b, :, f2, c * 512:(c + 1) * 512], in_=yst[yi])), [b_yst[yi]], nyc())
            S.barrier()

        S.barrier()

        while precast:
            precast.pop(0)()
        cf = Carver(ARF, ARF_N)
        OH_all = cf.take(64, 4); b_OH = S.buf("OH_all")
        GI_all = cf.take(64, 4); b_GI = S.buf("GI_all")
        POSf = cf.take(64); b_POSf = S.buf("POSf")
        GIDf = cf.take(NST); b_GIDf = S.buf("GIDf")
        ssC = cf.take(4); sdC = cf.take(4); b_ssC = [S.buf("ssC%d" % i) for i in range(4)]; b_sdC = [S.buf("sdC%d" % i) for i in range(4)]
        ef = cf.take(2); b_ef = [S.buf("ef0"), S.buf("ef1")]
        rdiag = cf.take(128); b_rdiag = S.buf("rdiag")
        gsl2 = [cf.take(8, 4), cf.take(8, 4)]; b_gsl2 = [S.buf("gsl0"), S.buf("gsl1")]
        f_persist = cf.o
        cntC = {"x": 0, "s": 0, "y": 0, "x3": 0, "r": 0}
        b_h2d = [S.buf("h2_d%d" % i) for i in range(4)]
        b_h2ss = [S.buf("h2s_d0"), S.buf("h2s_d1")]
        b_moe = S.buf("moe_d")

        cb = Carver(ARB, ARB_N)
        wout = cb.take(8, D); b_wout = S.buf("wout")
        yc = [cb.take(8, 512), cb.take(8, 512)]; b_yc = [S.buf("yc0"), S.buf("yc1")]
        xnC = [cb.take(D), cb.take(D)]; b_xnC = [S.buf("xnC0"), S.buf("xnC1")]
        xnT = [cb.take(8, 128), cb.take(8, 128)]; b_xnT = [S.buf("xnT0"), S.buf("xnT1")]
        rowbuf = [cb.take(ROWW), cb.take(ROWW)]; b_rowbuf = [S.buf("rowbuf0"), S.buf("rowbuf1")]
        wrb = cb.take(8, 20); b_wrb = S.buf("wrb")
        swr = cb.take(8, 20); b_swr = S.buf("swr")
        onesb = cb.take(128); b_onesb = S.buf("onesb")
        zrow = cb.take(8, ROWW); b_zrow = S.buf("zrow")
        xtC = [cf.take(D), cf.take(D)]; b_xtC = [S.buf("xtC0"), S.buf("xtC1")]
        A2b = cf.take(D); b_A2b = S.buf("A2b")
        S2b = cf.take(D); b_S2b = S.buf("S2b")
        G1b = cf.take(D); b_G1b = S.buf("G1b")
        rbb = cf.take(20); b_rbb = S.buf("rbb")
        lg = cf.take(8, 20); b_lg = S.buf("lg")
        NR_ = 18
        rsm = cf.take(NR_, 8, 4); b_rsm = [S.buf("rsm%d" % i) for i in range(NR_)]
        lgp = pbank[0][:, 0:160].rearrange("p (t e) -> p t e", e=20)
        b_lgp = b_pb[0]

        while zfill:
            r_ = zfill.pop(0)
            S.dma("pool", (lambda r_=r_: P.dma_start(out=h2s_d[r_ * 128:(r_ + 1) * 128, :], in_=zres[:, :])), [b_zres], b_h2z, par=True)
        S.op("dve", lambda: V.tensor_copy(out=onesb, in_=onesf), [b_cst], [b_onesb])

        (GM, GE, GS, GP, OHG, EIN, ET, M1, MK1, E2, M2, MK2, DL, W1, W2, GI, GT) = range(17)

        def rs(i, n=4):
            return rsm[:, i, :, 0:n]

        def rop(fn, reads, writes, extra_r=(), extra_w=()):
            S.op("dve", fn, [b_rsm[i] for i in reads] + list(extra_r), [b_rsm[i] for i in writes] + list(extra_w))

        def bc4(ap1):
            return ap1.broadcast_to([128, 8, 4])

        def router_batch(sc):
            T0 = sc * 8
            S.op("dve", (lambda: V.tensor_tensor(out=lg, in0=lgp, in1=rbb.unsqueeze(1).broadcast_to([128, 8, 20]), op=ALU.add)), [b_lgp, b_rbb], [b_lg])
            rop((lambda: V.tensor_reduce(out=rs(GM, 1), in_=lg[:, :, 0:4], axis=AX.X, op=ALU.max)), [], [GM], [b_lg])
            rop((lambda: V.tensor_tensor(out=rs(GE), in0=lg[:, :, 0:4], in1=bc4(rs(GM, 1)), op=ALU.subtract)), [GM], [GE], [b_lg])
            S.op("act", (lambda: A.activation(out=rs(GE), in_=rs(GE), func=AF.Exp)), [b_rsm[GE]], [b_rsm[GE]])
            rop((lambda: V.tensor_reduce(out=rs(GS, 1), in_=rs(GE), axis=AX.X, op=ALU.add)), [GE], [GS])
            rop((lambda: V.reciprocal(out=rs(GP, 1), in_=rs(GS, 1))), [GS], [GP])
            rop((lambda: V.tensor_tensor(out=OH_all[:, T0:T0 + 8, :], in0=lg[:, :, 0:4], in1=bc4(rs(GM, 1)), op=ALU.is_equal)), [GM], [], [b_lg], [b_OH])
            ohv = OH_all[:, T0:T0 + 8, :]
            rop((lambda: V.tensor_tensor(out=rs(EIN), in0=lg[:, :, 4:8], in1=bc4(ohv[:, :, 0:1]), op=ALU.mult)), [], [EIN], [b_lg, b_OH])
            for g in range(1, 4):
                rop((lambda g=g: V.tensor_tensor(out=rs(ET), in0=lg[:, :, 4 + 4 * g: 8 + 4 * g], in1=bc4(ohv[:, :, g:g + 1]), op=ALU.mult)), [], [ET], [b_lg, b_OH])
                rop((lambda: V.tensor_tensor(out=rs(EIN), in0=rs(EIN), in1=rs(ET), op=ALU.add)), [EIN, ET], [EIN])
            rop((lambda: V.tensor_reduce(out=rs(M1, 1), in_=rs(EIN), axis=AX.X, op=ALU.max)), [EIN], [M1])
            rop((lambda: V.tensor_tensor(out=rs(MK1), in0=rs(EIN), in1=bc4(rs(M1, 1)), op=ALU.is_equal)), [EIN, M1], [MK1])
            rop((lambda: V.scalar_tensor_tensor(out=rs(E2), in0=rs(MK1), scalar=-1e30, in1=rs(EIN), op0=ALU.mult, op1=ALU.add)), [MK1, EIN], [E2])
            rop((lambda: V.tensor_reduce(out=rs(M2, 1), in_=rs(E2), axis=AX.X, op=ALU.max)), [E2], [M2])
            rop((lambda: V.tensor_tensor(out=rs(MK2), in0=rs(E2), in1=bc4(rs(M2, 1)), op=ALU.is_equal)), [E2, M2], [MK2])
            rop((lambda: V.tensor_tensor(out=rs(DL, 1), in0=rs(M2, 1), in1=rs(M1, 1), op=ALU.subtract)), [M1, M2], [DL])
            S.op("act", (lambda: A.activation(out=rs(DL, 1), in_=rs(DL, 1), func=AF.Exp)), [b_rsm[DL]], [b_rsm[DL]])
            rop((lambda: V.tensor_scalar(out=rs(W1, 1), in0=rs(DL, 1), scalar1=1.0, scalar2=None, op0=ALU.add)), [DL], [W1])
            rop((lambda: V.reciprocal(out=rs(W1, 1), in_=rs(W1, 1))), [W1], [W1])
            rop((lambda: V.tensor_tensor(out=rs(W2, 1), in0=rs(DL, 1), in1=rs(W1, 1), op=ALU.mult)), [DL, W1], [W2])
            rop((lambda: V.tensor_tensor(out=rs(W1, 1), in0=rs(W1, 1), in1=rs(GP, 1), op=ALU.mult)), [W1, GP], [W1])
            rop((lambda: V.tensor_tensor(out=rs(W2, 1), in0=rs(W2, 1), in1=rs(GP, 1), op=ALU.mult)), [W2, GP], [W2])
            rop((lambda: V.tensor_tensor(out=rs(GI), in0=rs(MK1), in1=bc4(rs(W1, 1)), op=ALU.mult)), [MK1, W1], [GI])
            rop((lambda: V.tensor_tensor(out=rs(GT), in0=rs(MK2), in1=bc4(rs(W2, 1)), op=ALU.mult)), [MK2, W2], [GT])
            rop((lambda: V.tensor_tensor(out=GI_all[:, T0:T0 + 8, :], in0=rs(GI), in1=rs(GT), op=ALU.add)), [GI, GT], [], (), [b_GI])

        def bcast_rows(gt0, b, Gd, bG):
            for k in range(8):
                S.op("dve", (lambda gt0=gt0, k=k, b=b: V.tensor_scalar(out=rdiag, in0=identf, scalar1=modT[:, gt0 + k, b:b + 1], scalar2=None, op0=ALU.mult)), [b_cst, b_modT], [b_rdiag])
                pb_, bpb_ = zbank()
                S.op("pe", (lambda pb_=pb_: T.matmul(pb_[:, 0:128], lhsT=onesf, rhs=rdiag, start=True, stop=True)), [b_cst, b_rdiag], [bpb_])
                S.op("act", (lambda pb_=pb_, Gd=Gd, k=k: A.copy(out=Gd[:, k * 128:(k + 1) * 128], in_=pb_[:, 0:128])), [bpb_], [bG])

        def bcast_rows_tab(tab, b_tab, b, Gd, bG):
            for k in range(8):
                S.op("dve", (lambda k=k, b=b: V.tensor_scalar(out=rdiag, in0=identf, scalar1=tab[:, k, b:b + 1], scalar2=None, op0=ALU.mult)), [b_cst, b_tab], [b_rdiag])
                pb_, bpb_ = zbank()
                S.op("pe", (lambda pb_=pb_: T.matmul(pb_[:, 0:128], lhsT=onesf, rhs=rdiag, start=True, stop=True)), [b_cst, b_rdiag], [bpb_])
                S.op("act", (lambda pb_=pb_, Gd=Gd, k=k: A.copy(out=Gd[:, k * 128:(k + 1) * 128], in_=pb_[:, 0:128])), [bpb_], [bG])

        def batch_prep(b):
            S.dma("sp", (lambda b=b: nc.sync.dma_start(out=G1b, in_=modrow_d[b, 2 * D:3 * D].partition_broadcast(128))), [b_modrow], b_G1b)
            S.dma("sp", (lambda b=b: nc.sync.dma_start(out=A2b, in_=a2row_d[b].partition_broadcast(128))), [b_modrow], b_A2b)
            S.dma("sp", (lambda b=b: nc.sync.dma_start(out=S2b, in_=modrow_d[b, 3 * D:4 * D].partition_broadcast(128))), [b_modrow], b_S2b)
            S.dma("pool", lambda: P.dma_start(out=wout, in_=wout_d.rearrange("(k p) n -> p k n", p=128)), [], b_wout)
            for k in range(8):
                S.op("dve", (lambda k=k: V.tensor_tensor(out=wout[:, k, :], in0=wout[:, k, :], in1=G1b, op=ALU.mult)), [b_wout, b_G1b], [b_wout])
            for k in range(8):
                S.op("dve", (lambda k=k, b=b: V.tensor_scalar(out=wrb[:, k, :], in0=wr[:, k, :], scalar1=A2T[:, k, b:b + 1], scalar2=None, op0=ALU.mult)), [b_wr, b_A2T], [b_wrb])
                S.op("dve", (lambda k=k, b=b: V.tensor_scalar(out=swr[:, k, :], in0=wr[:, k, :], scalar1=S2T[:, k, b:b + 1], scalar2=None, op0=ALU.mult)), [b_wr, b_modT], [b_swr])
            pb_, bpb_ = zbank()
            for k in range(8):
                S.op("pe", (lambda pb_=pb_, k=k: T.matmul(pb_[:, 0:20], lhsT=onesb, rhs=swr[:, k, :], start=(k == 0), stop=(k == 7))), [b_onesb, b_swr], [bpb_])
            S.op("dve", (lambda pb_=pb_: V.tensor_tensor(out=rbb, in0=pb_[:, 0:20], in1=rbias[:, :], op=ALU.add)), [bpb_, b_rbias], [b_rbb])

        def part1A(sc, t):
            b = sc // 2; half = sc % 2
            tok0 = sc * 1024 + t * 128
            col = half * 1024 + t * 128
            if t % 4 == 0:
                cntC["y"] += 1
                yi = cntC["y"] % 2
                S.dma("sp", (lambda b=b, col=col, yi=yi: nc.sync.dma_start(out=yc[yi], in_=ycat_d[b, :, :, col:col + 512])), b_ycats, b_yc[yi])
            yi = cntC["y"] % 2
            tc_ = (t % 4) * 128
            i = cntC["x"] % 2; cntC["x"] += 1
            S.dma("sp", (lambda i=i, tok0=tok0: nc.sync.dma_start(out=xtC[i], in_=x_d[tok0:tok0 + 128, :])), [], b_xtC[i])
            for hf in range(2):
                pb_, bpb_ = zbank()
                for k in range(8):
                    S.op("pe", (lambda pb_=pb_, k=k, hf=hf, yi=yi, tc_=tc_: T.matmul(pb_[:, :], lhsT=yc[yi][:, k, tc_:tc_ + 128], rhs=wout[:, k, hf * 512:(hf + 1) * 512], start=(k == 0), stop=(k == 7))), [b_yc[yi], b_wout], [bpb_])
                S.op("dve", (lambda pb_=pb_, hf=hf, i=i: V.tensor_tensor(out=xtC[i][:, hf * 512:(hf + 1) * 512], in0=pb_[:, :], in1=xtC[i][:, hf * 512:(hf + 1) * 512], op=ALU.add)), [bpb_, b_xtC[i]], [b_xtC[i]])
            S.dma("pool", (lambda i=i, tok0=tok0: P.dma_start(out=x1_d[tok0:tok0 + 128, :], in_=xtC[i])), [b_xtC[i]], b_x1ds[t % 4])
            return (i, tok0)

        def part1A2(sc, t, st1):
            i, tok0 = st1
            j = cntC["s"] % 4; cntC["s"] += 1
            xq = xnC[i]; bxq = b_xnC[i]
            S.op("act", (lambda i=i, j=j, xq=xq: A.activation(out=xq, in_=xtC[i], func=AF.Square, accum_out=ssC[:, j:j + 1])), [b_xtC[i]], [bxq, b_ssC[j]])
            S.op("act", (lambda j=j: A.activation(out=sdC[:, j:j + 1], in_=ssC[:, j:j + 1], func=AF.Sqrt, scale=1.0 / D, bias=epsc)), [b_ssC[j], b_cst], [b_sdC[j]])
            S.op("dve", (lambda j=j: V.reciprocal(out=sdC[:, j:j + 1], in_=sdC[:, j:j + 1])), [b_sdC[j]], [b_sdC[j]])
            S.op("act", (lambda i=i, j=j, xq=xq: A.activation(out=xq, in_=xtC[i], func=AF.Copy, scale=sdC[:, j:j + 1])), [b_xtC[i], b_sdC[j]], [bxq])
            rb_ = rowbuf[i]; brb_ = b_rowbuf[i]
            S.op("dve", (lambda xq=xq, rb_=rb_: V.tensor_tensor(out=rb_[:, 0:D], in0=xq, in1=A2b, op=ALU.mult)), [bxq, b_A2b], [brb_])
            S.op("pool", (lambda rb_=rb_: P.tensor_tensor(out=rb_[:, 0:D], in0=rb_[:, 0:D], in1=S2b, op=ALU.add)), [brb_, b_S2b], [brb_])
            S.dma("pool", (lambda rb_=rb_, tok0=tok0: P.dma_start(out=h2_d[tok0:tok0 + 128, :], in_=rb_[:, 0:D])), [brb_], b_h2d[t % 4])
            return (i, xq, bxq)

        def part1B(sc, t, st_):
            i, xq, bxq = st_
            for k in range(8):
                S.op("pe", (lambda k=k, xq=xq: T.transpose(out=pT[:, k, :], in_=xq[:, k * 128:(k + 1) * 128], identity=identb[:])), [bxq, b_identb], [b_pT])
            S.op("act", (lambda i=i: A.copy(out=xnT[i], in_=pT[:, :, :])), [b_pT], [b_xnT[i]])
            for k in range(8):
                S.op("pe", (lambda k=k, t=t, i=i: T.matmul(lgp[:, t, :], lhsT=xnT[i][:, k, :], rhs=wrb[:, k, :], start=(k == 0), stop=(k == 7))), [b_xnT[i], b_wrb], [b_lgp])

        NSC = 2 * NB
        for sc in range(NSC):
            if sc % 2 == 0:
                batch_prep(sc // 2)
            prev = None
            for t in range(8):
                st1 = part1A(sc, t)
                if prev is not None:
                    part1B(sc, t - 1, prev)
                prev = part1A2(sc, t, st1)
            part1B(sc, 7, prev)
            router_batch(sc)

        WITH = cf.take(64, 4); b_WITH = S.buf("WITH")
        TOT = cf.take(64, 4); b_TOT = S.buf("TOT")
        CUM = cf.take(64, 4); b_CUM = S.buf("CUM")
        ones64 = cf.take(64); b_ones64 = S.buf("ones64")
        tmpN = cf.take(NST); b_tmpN = S.buf("tmpN")
        NT = cf.take(4); b_NT = S.buf("NT")
        EE = cf.take(4); b_EE = S.buf("EE")
        BASE = cf.take(4); b_BASE = S.buf("BASE")
        OHf = OH_all.rearrange("p a b -> p (a b)")
        S.op("pe", lambda: T.matmul(pbank[1][:, 0:256], lhsT=lstf, rhs=OHf, start=True, stop=True), [b_cst, b_OH], [b_pb[1]])
        S.op("pe", lambda: T.matmul(pbank[2][:, 0:256], lhsT=onesf, rhs=OHf, start=True, stop=True), [b_cst, b_OH], [b_pb[2]])
        S.op("dve", lambda: V.tensor_copy(out=WITH.rearrange("p a b -> p (a b)"), in_=pbank[1][:, 0:256]), [b_pb[1]], [b_WITH])
        S.op("dve", lambda: V.tensor_copy(out=TOT.rearrange("p a b -> p (a b)"), in_=pbank[2][:, 0:256]), [b_pb[2]], [b_TOT])
        S.op("dve", lambda: V.memset(ones64, 1.0), [], [b_ones64])
        for g in range(4):
            S.op("dve", (lambda g=g: V.tensor_tensor_scan(out=CUM[:, :, g], data0=ones64, data1=TOT[:, :, g], initial=0.0, op0=ALU.mult, op1=ALU.add)), [b_ones64, b_TOT], [b_CUM])
        for g in range(4):
            S.op("dve", (lambda g=g: V.tensor_scalar(out=tmpN, in0=thrf, scalar1=CUM[:, 63, g:g + 1], scalar2=None, op0=ALU.is_lt)), [b_cst, b_CUM], [b_tmpN])
            S.op("dve", (lambda g=g: V.tensor_reduce(out=NT[:, g:g + 1], in_=tmpN, axis=AX.X, op=ALU.add)), [b_tmpN], [b_NT])
        S.op("dve", lambda: V.tensor_scalar(out=EE[:, 0:1], in0=NT[:, 0:1], scalar1=float(TSZ), scalar2=None, op0=ALU.mult), [b_NT], [b_EE])
        for g in range(1, 4):
            S.op("dve", (lambda g=g: V.scalar_tensor_tensor(out=EE[:, g:g + 1], in0=NT[:, g:g + 1], scalar=float(TSZ), in1=EE[:, g - 1:g], op0=ALU.mult, op1=ALU.add)), [b_NT, b_EE], [b_EE])
        S.op("dve", lambda: V.memset(BASE[:, 0:1], 0.0), [], [b_BASE])
        S.op("dve", lambda: V.tensor_copy(out=BASE[:, 1:4], in_=EE[:, 0:3]), [b_EE], [b_BASE])
        S.op("dve", lambda: V.tensor_tensor(out=CUM, in0=CUM, in1=TOT, op=ALU.subtract), [b_CUM, b_TOT], [b_CUM])
        S.op("dve", lambda: V.tensor_tensor(out=CUM, in0=CUM, in1=WITH, op=ALU.add), [b_CUM, b_WITH], [b_CUM])
        S.op("dve", lambda: V.tensor_tensor(out=CUM, in0=CUM, in1=BASE.unsqueeze(1).broadcast_to([128, 64, 4]), op=ALU.add), [b_CUM, b_BASE], [b_CUM])
        S.op("dve", lambda: V.tensor_tensor(out=CUM, in0=CUM, in1=OH_all, op=ALU.mult), [b_CUM, b_OH], [b_CUM])
        S.op("dve", lambda: V.tensor_reduce(out=POSf, in_=CUM, axis=AX.X, op=ALU.add), [b_CUM], [b_POSf])
        S.op("dve", lambda: V.tensor_copy(out=posi[:, :], in_=POSf), [b_POSf], [b_posi])
        S.op("dve", lambda: V.tensor_scalar(out=GIDf, in0=thrf, scalar1=EE[:, 0:1], scalar2=None, op0=ALU.is_ge), [b_cst, b_EE], [b_GIDf])
        for g in range(1, 3):
            S.op("dve", (lambda g=g: V.tensor_scalar(out=tmpN, in0=thrf, scalar1=EE[:, g:g + 1], scalar2=None, op0=ALU.is_ge)), [b_cst, b_EE], [b_tmpN])
            S.op("dve", lambda: V.tensor_tensor(out=GIDf, in0=GIDf, in1=tmpN, op=ALU.add), [b_GIDf, b_tmpN], [b_GIDf])

        for Tt in range(64):
            i = Tt % 2
            rb_ = rowbuf[i]; brb_ = b_rowbuf[i]
            S.dma("sp", (lambda rb_=rb_, Tt=Tt: nc.sync.dma_start(out=rb_[:, 0:D], in_=h2_d[Tt * 128:(Tt + 1) * 128, :])), b_h2d, brb_)
            S.op("dve", (lambda rb_=rb_, Tt=Tt: V.tensor_copy(out=rb_[:, D:D + 4], in_=GI_all[:, Tt, :])), [b_GI, brb_], [brb_])
            S.dma("pool", (lambda rb_=rb_, Tt=Tt: P.indirect_dma_start(out=h2s_d[:, :], out_offset=bass.IndirectOffsetOnAxis(ap=posi[:, Tt:Tt + 1], axis=0), in_=rb_, in_offset=None)), [brb_, b_posi, b_h2z], b_h2ss[i])

        S.barrier()

        cb = Carver(ARB, ARB_N); cf = Carver(ARF, ARF_N); cf.o = f_persist
        rows8 = cb.take(4, ROWW); b_rows8 = S.buf("rows8")
        h2sT = [cb.take(8, TSZ), cb.take(8, TSZ)]
        b_h2sT = [[S.buf("h2sT%d_%d" % (p, i)) for i in range(4)] for p in range(2)]
        acc = cb.take(4, D); b_acc = [S.buf("acc_%d" % i) for i in range(4)]
        w1s = [cb.take(8, 512), cb.take(8, 512)]; w3s = [cb.take(8, 512), cb.take(8, 512)]; w2s = [cb.take(4, D), cb.take(4, D)]
        b_w1s = [S.buf("w1s0"), S.buf("w1s1")]; b_w3s = [S.buf("w3s0"), S.buf("w3s1")]; b_w2s = [S.buf("w2s0"), S.buf("w2s1")]
        heT = cb.take(4, 512); b_heT = S.buf("heT")
        sil = [cb.take(512), cb.take(512)]; b_sil = [S.buf("sil0"), S.buf("sil1")]
        w1rows = w1b_d.rearrange("e p k n -> (e p) (k n)")
        w3rows = w3b_d.rearrange("e p k n -> (e p) (k n)")
        w2rows = w2b_d.rearrange("e p k n -> (e p) (k n)")

        def load_w(s_, i_):
            wi = (s_ * 4 + i_) % 2
            S.op("dve", (lambda s_=s_, i_=i_, wi=wi: V.tensor_scalar(out=ef[:, wi:wi + 1], in0=GIDf[:, s_:s_ + 1], scalar1=512.0, scalar2=float(i_ * 128), op0=ALU.mult, op1=ALU.add)), [b_GIDf], [b_ef[wi]])
            S.op("dve", (lambda wi=wi: V.tensor_tensor(out=ef[:, wi:wi + 1], in0=ef[:, wi:wi + 1], in1=iotap, op=ALU.add)), [b_ef[wi], b_cst], [b_ef[wi]])
            S.op("dve", (lambda wi=wi: V.tensor_copy(out=widx[:, wi:wi + 1], in_=ef[:, wi:wi + 1])), [b_ef[wi]], [b_widx[wi]])
            S.dma("pool", (lambda wi=wi: P.indirect_dma_start(out=w1s[wi].rearrange("p k n -> p (k n)"), out_offset=None, in_=w1rows, in_offset=bass.IndirectOffsetOnAxis(ap=widx[:, wi:wi + 1], axis=0))), [b_widx[wi], b_wb], b_w1s[wi])
            S.dma("pool", (lambda wi=wi: P.indirect_dma_start(out=w3s[wi].rearrange("p k n -> p (k n)"), out_offset=None, in_=w3rows, in_offset=bass.IndirectOffsetOnAxis(ap=widx[:, wi:wi + 1], axis=0))), [b_widx[wi], b_wb], b_w3s[wi])
            S.dma("pool", (lambda wi=wi: P.indirect_dma_start(out=w2s[wi].rearrange("p k n -> p (k n)"), out_offset=None, in_=w2rows, in_offset=bass.IndirectOffsetOnAxis(ap=widx[:, wi:wi + 1], axis=0))), [b_widx[wi], b_wb], b_w2s[wi])

        def load_rows(s_):
            pp = s_ % 2
            S.dma("sp", (lambda s_=s_: nc.sync.dma_start(out=rows8, in_=h2s_d[s_ * TSZ:(s_ + 1) * TSZ, :].rearrange("(a p) c -> p a c", p=128))), b_h2ss, b_rows8)
            S.op("dve", (lambda pp=pp: V.tensor_copy(out=gsl2[pp][:, 0:4, :], in_=rows8[:, :, D:D + 4])), [b_rows8], [b_gsl2[pp]])
            for sub in range(4):
                for k in range(8):
                    S.op("pe", (lambda sub=sub, k=k: T.transpose(out=pT[:, k, :], in_=rows8[:, sub, k * 128:(k + 1) * 128], identity=identb[:])), [b_rows8, b_identb], [b_pT])
                if sub % 2 == 0:
                    S.op("act", (lambda sub=sub, pp=pp: A.copy(out=h2sT[pp][:, :, sub * 128:(sub + 1) * 128], in_=pT[:, :, :])), [b_pT], [b_h2sT[pp][sub]])
                else:
                    S.op("dve", (lambda sub=sub, pp=pp: V.tensor_copy(out=h2sT[pp][:, :, sub * 128:(sub + 1) * 128], in_=pT[:, :, :])), [b_pT], [b_h2sT[pp][sub]])

        def expert_tile(s_, i_):
            pp = s_ % 2
            wi = (s_ * 4 + i_) % 2
            for c in range(1):
                for dd in range(4):
                    p1, bp1 = zbank()
                    p3, bp3 = zbank()
                    for k in range(8):
                        S.op("pe", (lambda p1=p1, k=k, dd=dd, c=c, wi=wi, pp=pp: T.matmul(p1[:, :], lhsT=w1s[wi][:, k, dd * 128:(dd + 1) * 128], rhs=h2sT[pp][:, k, c * 512:(c + 1) * 512], start=(k == 0), stop=(k == 7))), [b_w1s[wi]] + b_h2sT[pp][c * 4:(c + 1) * 4], [bp1])
                    for k in range(8):
                        S.op("pe", (lambda p3=p3, k=k, dd=dd, c=c, wi=wi, pp=pp: T.matmul(p3[:, :], lhsT=w3s[wi][:, k, dd * 128:(dd + 1) * 128], rhs=h2sT[pp][:, k, c * 512:(c + 1) * 512], start=(k == 0), stop=(k == 7))), [b_w3s[wi]] + b_h2sT[pp][c * 4:(c + 1) * 4], [bp3])
                    si = dd % 2
                    S.op("act", (lambda p1=p1, si=si: A.activation(out=sil[si], in_=p1[:, :], func=AF.Silu)), [bp1], [b_sil[si]])
                    S.op("dve", (lambda p3=p3, si=si, dd=dd: V.tensor_tensor(out=heT[:, dd, :], in0=sil[si], in1=p3[:, :], op=ALU.mult)), [b_sil[si], bp3], [b_heT])
                for tt_ in range(4):
                    t = c * 4 + tt_
                    for hf in range(2):
                        po, bpo = zbank()
                        for dd in range(4):
                            S.op("pe", (lambda po=po, dd=dd, tt_=tt_, hf=hf, wi=wi: T.matmul(po[:, :], lhsT=heT[:, dd, tt_ * 128:(tt_ + 1) * 128], rhs=w2s[wi][:, dd, hf * 512:(hf + 1) * 512], start=(dd == 0), stop=(dd == 3))), [b_heT, b_w2s[wi]], [bpo])
                        if i_ == 0:
                            S.op("dve", (lambda po=po, t=t, hf=hf, i_=i_, pp=pp: V.tensor_scalar(out=acc[:, t, hf * 512:(hf + 1) * 512], in0=po[:, :], scalar1=gsl2[pp][:, t, i_:i_ + 1], scalar2=None, op0=ALU.mult)), [bpo, b_gsl2[pp]], [b_acc[t]])
                        else:
                            S.op("dve", (lambda po=po, t=t, hf=hf, i_=i_, pp=pp: V.scalar_tensor_tensor(out=acc[:, t, hf * 512:(hf + 1) * 512], in0=po[:, :], scalar=gsl2[pp][:, t, i_:i_ + 1], in1=acc[:, t, hf * 512:(hf + 1) * 512], op0=ALU.mult, op1=ALU.add)), [bpo, b_gsl2[pp], b_acc[t]], [b_acc[t]])

        load_w(0, 0)
        load_rows(0)
        for s_ in range(NST):
            for i_ in range(4):
                nxt = s_ * 4 + i_ + 1
                if nxt < NST * 4:
                    load_w(nxt // 4, nxt % 4)
                expert_tile(s_, i_)
                if i_ == 1 and s_ + 1 < NST:
                    load_rows(s_ + 1)
            S.dma("sp", (lambda s_=s_: nc.sync.dma_start(out=moe_d[s_ * TSZ:(s_ + 1) * TSZ, :].rearrange("(a p) c -> p a c", p=128), in_=acc)), b_acc, b_moe)

        S.barrier()

        cb = Carver(ARB, ARB_N); cf = Carver(ARF, ARF_N); cf.o = f_persist
        mrow = [cb.take(D), cb.take(D), cb.take(D)]; b_mrow = [S.buf("mrow0"), S.buf("mrow1"), S.buf("mrow2")]
        x3t = [cf.take(D), cf.take(D), cf.take(D)]; b_x3t = [S.buf("x3t0"), S.buf("x3t1"), S.buf("x3t2")]
        t1s = [cf.take(D), cf.take(D)]; b_t1s = [S.buf("t1_0"), S.buf("t1_1")]
        G2b = cf.take(D); b_G2b = S.buf("G2b")
        FGb = cf.take(D); b_FGb = S.buf("FGb")
        S.dma("sp", lambda: nc.sync.dma_start(out=FGb, in_=fg_d.partition_broadcast(128)), [], b_FGb)
        def c5_fetch(Tt):
            i = Tt % 3
            tok0 = Tt * 128
            S.dma("pool", (lambda i=i, Tt=Tt: P.indirect_dma_start(out=mrow[i], out_offset=None, in_=moe_d[:, :], in_offset=bass.IndirectOffsetOnAxis(ap=posi[:, Tt:Tt + 1], axis=0))), [b_moe, b_posi], b_mrow[i])
            S.dma("sp", (lambda i=i, tok0=tok0: nc.sync.dma_start(out=x3t[i], in_=x1_d[tok0:tok0 + 128, :])), b_x1ds, b_x3t[i])

        G2bs = [G2b, t1s[0]]
        c5_fetch(0)
        for Tt in range(64):
            b = Tt // 16
            if Tt % 16 == 0:
                S.dma("sp", (lambda b=b: nc.sync.dma_start(out=G2b, in_=modrow_d[b, 5 * D:6 * D].partition_broadcast(128))), [b_modrow], b_G2b)
            if Tt + 1 < 64:
                c5_fetch(Tt + 1)
            tok0 = Tt * 128
            i = Tt % 3
            t1 = t1s[Tt % 2]; b_t1 = b_t1s[Tt % 2]
            S.op("dve", (lambda i=i, t1=t1: V.tensor_tensor(out=t1, in0=mrow[i], in1=G2b, op=ALU.mult)), [b_mrow[i], b_G2b], [b_t1])
            S.op("dve", (lambda i=i, t1=t1: V.tensor_tensor(out=x3t[i], in0=t1, in1=x3t[i], op=ALU.add)), [b_t1, b_x3t[i]], [b_x3t[i]])
            j = cntC["s"] % 4; cntC["s"] += 1
            S.op("act", (lambda i=i, j=j, t1=t1: A.activation(out=t1, in_=x3t[i], func=AF.Square, accum_out=ssC[:, j:j + 1])), [b_x3t[i]], [b_t1, b_ssC[j]])
            S.op("act", (lambda j=j: A.activation(out=sdC[:, j:j + 1], in_=ssC[:, j:j + 1], func=AF.Sqrt, scale=1.0 / D, bias=epsc)), [b_ssC[j], b_cst], [b_sdC[j]])
            S.op("dve", (lambda j=j: V.reciprocal(out=sdC[:, j:j + 1], in_=sdC[:, j:j + 1])), [b_sdC[j]], [b_sdC[j]])
            S.op("act", (lambda i=i, j=j, t1=t1: A.activation(out=t1, in_=x3t[i], func=AF.Copy, scale=sdC[:, j:j + 1])), [b_x3t[i], b_sdC[j]], [b_t1])
            S.op("dve", (lambda i=i, t1=t1: V.tensor_tensor(out=x3t[i], in0=t1, in1=FGb, op=ALU.mult)), [b_t1, b_FGb], [b_x3t[i]])
            S.dma("sp", (lambda i=i, tok0=tok0: nc.sync.dma_start(out=out_d[tok0:tok0 + 128, :], in_=x3t[i])), [b_x3t[i]], b_outs[Tt % 4])

        S.final_waits.extend(b_outs)
        S.emit()
    return nc


_NC_CACHE = {}


def kernel(x, c, ctx, c_ctx, w_mod, b_mod, norm1_g, norm2_g, w_in, s5_lambda_re, s5_lambda_im,
           s5_log_dt, s5_b_re, s5_b_im, s5_c_re, s5_c_im, s5_d, w_glu, b_glu, conv_w, w_out,
           router_group_w, router_group_b, router_expert_w, router_expert_b,
           expert_w1, expert_w3, expert_w2, final_g):
    f = lambda a: np.ascontiguousarray(np.asarray(a, dtype=np.float32))
    if "nc" not in _NC_CACHE:
        _NC_CACHE["nc"] = build()
    nc = _NC_CACHE["nc"]
    x = f(x); ctx = f(ctx); c = f(c); c_ctx = f(c_ctx)
    shared = {
        "w_mod": f(w_mod[0]), "b_mod": f(b_mod[0]), "norm1_g": f(norm1_g[0]), "norm2_g": f(norm2_g[0]),
        "w_in": f(w_in[0]), "lam_re": f(s5_lambda_re[0]), "lam_im": f(s5_lambda_im[0]),
        "log_dt": f(s5_log_dt[0]).reshape(32), "b_re": f(s5_b_re[0]), "b_im": f(s5_b_im[0]),
        "c_re": f(s5_c_re[0]).reshape(512, 64), "c_im": f(s5_c_im[0]).reshape(512, 64),
        "s5_d": f(s5_d[0]), "w_glu": f(w_glu[0]), "b_glu": f(b_glu[0]), "conv_w": f(conv_w[0]),
        "w_out": f(w_out[0]), "rgw": f(router_group_w[0]), "rgb": f(router_group_b[0]),
        "rew": f(router_expert_w[0]), "reb": f(router_expert_b[0]),
        "w1": f(expert_w1[0]), "w3": f(expert_w3[0]), "w2": f(expert_w2[0]),
        "final_g": f(final_g), "consts": CONSTS, "gsel": GSEL,
    }
    in_maps = []
    for i in range(8):
        m = dict(shared)
        m["x"] = x[i * NB:(i + 1) * NB].reshape(NB * SEQ, D)
        m["ctx"] = ctx[i * NB:(i + 1) * NB].reshape(NB * NCTX, D)
        m["cond"] = np.concatenate([c[i * NB:(i + 1) * NB], c_ctx[None, :]], axis=0)
        in_maps.append(m)
    res = run_bass_kernel_spmd(nc, in_maps, core_ids=list(range(8)))
    outs = [np.asarray(r["out"]).reshape(NB, SEQ, D) for r in res.results]
    return np.concatenate(outs, axis=0).astype(np.float32)
```
